# Optimizing a Trainium2 kernel written in Bass

```python
import jax, jax.numpy as jnp
from jax import lax
import numpy as np

D_MODEL = 1024
BATCH = 8
SEQ = 4096
DEPTH = 4
DEC_BATCH = 32
DEC_SEQ = 2048
PAST_LEN = 128

GRID_W = 64
HEAD_DIM = 64
D_MIX = D_MODEL
GROUP_W = D_MIX // 4
N_RWKV_HEADS = GROUP_W // HEAD_DIM
N_Q_HEADS = GROUP_W // HEAD_DIM
N_KV_HEADS = N_Q_HEADS // 2
N_NAT_HEADS = GROUP_W // HEAD_DIM
POOL_WINDOWS = (2, 4, 8, 16)
N_POOL_GROUPS = len(POOL_WINDOWS)
POOL_GROUP_DIM = GROUP_W // N_POOL_GROUPS
DECAY_LORA = 64
AAA_LORA = 64
GATE_LORA = 128
A_COLS = 3 * GROUP_W + 2 * DECAY_LORA + 2 * AAA_LORA + GATE_LORA
B_COLS = GROUP_W + 2 * N_KV_HEADS * HEAD_DIM
C_COLS = 3 * GROUP_W
D_COLS = GROUP_W
IN_COLS = A_COLS + B_COLS + C_COLS + D_COLS
Q_BLOCK = 128
ROPE_THETA = 10000.0
NAT_WIN_H = 8
NAT_WIN_W = 16
D_FF_DENSE = 2816
N_EXPERTS = 8
TOP_K = 2
D_FF_EXPERT = 3584
N_DENSE = (DEPTH + 1) // 2
N_MOE = DEPTH // 2
N_MOD = 6
NORM_EPS = 1e-6
GN_EPS = 64e-5

kernel_name = 'hybrid_bidir_encoder_parallel_groups'


def rmsnorm(x, g):
    xf = x.astype(jnp.float32)
    y = xf * lax.rsqrt(jnp.mean(xf * xf, axis=-1, keepdims=True) + NORM_EPS)
    return (y * g.astype(jnp.float32)).astype(x.dtype)


def centred_shift(p):
    zero = jnp.zeros_like(p[:, :1])
    prev = jnp.concatenate([zero, p[:, :-1]], axis=1)
    nxt = jnp.concatenate([p[:, 1:], zero], axis=1)
    return 0.5 * (prev + nxt)


def rwkv7_scan(r, w, k, v, kk, a, reverse):
    B, T, H, K = r.shape
    xs = tuple(jnp.moveaxis(t, 1, 0) for t in (r, w, k, v, kk, a))

    def step(S, inp):
        r_t, w_t, k_t, v_t, kk_t, a_t = inp
        s_kk = jnp.einsum('bhvk,bhk->bhv', S, kk_t)
        S = (S * w_t[:, :, None, :] - s_kk[..., None] * (kk_t * a_t)[:, :, None, :]
             + v_t[..., None] * k_t[:, :, None, :])
        return S, jnp.einsum('bhvk,bhk->bhv', S, r_t)

    S0 = jnp.zeros((B, H, K, K), jnp.float32)
    _, ys = lax.scan(step, S0, xs, reverse=reverse)
    return jnp.moveaxis(ys, 0, 1)


def rwkv7_mixer(p, mu, w0, w2, a0, a2, g2, k_k, k_a, r_k, lnx_w, lnx_b):
    B, T, _ = p.shape
    H, K = N_RWKV_HEADS, HEAD_DIM
    p = p.astype(jnp.float32)
    p = p + mu * (centred_shift(p) - p)
    r, k, v, wl, al, gl = jnp.split(
        p, [GROUP_W, 2 * GROUP_W, 3 * GROUP_W, 3 * GROUP_W + 2 * DECAY_LORA,
            3 * GROUP_W + 2 * DECAY_LORA + 2 * AAA_LORA], axis=-1)
    wl = wl.reshape(B, T, 2, DECAY_LORA)
    al = al.reshape(B, T, 2, AAA_LORA)
    w_log = -jax.nn.softplus(-(w0 + jnp.einsum('btdr,drc->btdc', jnp.tanh(wl), w2))) - 0.5
    decay = jnp.exp(-jnp.exp(w_log))
    a = jax.nn.sigmoid(a0 + jnp.einsum('btdr,drc->btdc', al, a2))
    g = jax.nn.sigmoid(gl) @ g2
    heads = lambda t: t.reshape(B, T, H, K)
    kk = heads(k * k_k)
    kk = kk * lax.rsqrt(jnp.maximum(jnp.sum(kk * kk, axis=-1, keepdims=True), 1e-24))
    k_dir = k[:, :, None, :] * (1.0 + (a - 1.0) * k_a)
    r_h, v_h = heads(r), heads(v)
    y = (rwkv7_scan(r_h, heads(decay[:, :, 0]), heads(k_dir[:, :, 0]), v_h, kk, heads(a[:, :, 0]), False)
         + rwkv7_scan(r_h, heads(decay[:, :, 1]), heads(k_dir[:, :, 1]), v_h, kk, heads(a[:, :, 1]), True))
    mean = jnp.mean(y, axis=-1, keepdims=True)
    var = jnp.mean(jnp.square(y - mean), axis=-1, keepdims=True)
    y = ((y - mean) * lax.rsqrt(var + GN_EPS)).reshape(B, T, GROUP_W) * lnx_w + lnx_b
    k_dir_h = k_dir.reshape(B, T, 2, H, K)
    bonus = jnp.sum(r_h[:, :, None] * k_dir_h * r_k, axis=(2, 4))[..., None] * v_h
    return (y + bonus.reshape(B, T, GROUP_W)) * g


def axial_rope(T):
    t = jnp.arange(T)
    row = (t // GRID_W).astype(jnp.float32)
    col = (t % GRID_W).astype(jnp.float32)
    n_freq = HEAD_DIM // 4
    inv = ROPE_THETA ** (-jnp.arange(n_freq, dtype=jnp.float32) / n_freq)
    ang = jnp.concatenate([row[:, None] * inv, col[:, None] * inv], axis=-1)
    return jnp.cos(ang), jnp.sin(ang)


def apply_rope(x, cos, sin):
    B, T, H, D = x.shape
    xp = x.astype(jnp.float32).reshape(B, T, H, D // 2, 2)
    x0, x1 = xp[..., 0], xp[..., 1]
    c = cos[None, :, None, :]
    s = sin[None, :, None, :]
    return jnp.stack([x0 * c - x1 * s, x0 * s + x1 * c], axis=-1).reshape(B, T, H, D)


def gqa_mixer(p, q_norm, k_norm):
    B, T, _ = p.shape
    D = HEAD_DIM
    G = N_Q_HEADS // N_KV_HEADS
    q, k, v = jnp.split(p, [GROUP_W, GROUP_W + N_KV_HEADS * D], axis=-1)
    q = rmsnorm(q.reshape(B, T, N_Q_HEADS, D), q_norm)
    k = rmsnorm(k.reshape(B, T, N_KV_HEADS, D), k_norm)
    cos, sin = axial_rope(T)
    q = apply_rope(q, cos, sin)
    k = apply_rope(k, cos, sin)
    nb = T // Q_BLOCK
    qb = q.reshape(B, nb, Q_BLOCK, N_KV_HEADS, G, D).transpose(1, 0, 3, 4, 2, 5)
    kt = k.transpose(0, 2, 1, 3)
    vt = v.reshape(B, T, N_KV_HEADS, D).transpose(0, 2, 1, 3)
    scale = D ** -0.5

    def block(qblk):
        s = jnp.einsum('bkgqd,bkjd->bkgqj', qblk, kt).astype(jnp.float32) * scale
        pr = jax.nn.softmax(s, axis=-1).astype(vt.dtype)
        return jnp.einsum('bkgqj,bkjd->bkgqd', pr, vt)

    o = lax.map(block, qb)
    return o.transpose(1, 0, 4, 2, 3, 5).reshape(B, T, GROUP_W)


def natten_mixer(p, rpb):
    B, T, _ = p.shape
    H, D = N_NAT_HEADS, HEAD_DIM
    rows = T // GRID_W
    wh = min(NAT_WIN_H, rows)
    q, k, v = jnp.split(p, [GROUP_W, 2 * GROUP_W], axis=-1)
    grid = lambda t: t.reshape(B, rows, GRID_W, H, D).transpose(0, 3, 1, 2, 4)
    qg, kg, vg = grid(q), grid(k), grid(v)
    cols = np.arange(GRID_W)
    col_start = np.clip(cols - NAT_WIN_W // 2, 0, GRID_W - NAT_WIN_W)
    col_idx = col_start[:, None] + np.arange(NAT_WIN_W)[None, :]
    dcol_idx = col_idx - cols[:, None] + (NAT_WIN_W - 1)
    rpb_cols = rpb[:, :, dcol_idx]
    scale = D ** -0.5

    def row_block(r):
        rs = jnp.clip(r - wh // 2, 0, rows - wh)
        k_rows = lax.dynamic_slice_in_dim(kg, rs, wh, axis=2)
        v_rows = lax.dynamic_slice_in_dim(vg, rs, wh, axis=2)
        k_win = k_rows[:, :, :, col_idx]
        v_win = v_rows[:, :, :, col_idx]
        q_row = lax.dynamic_index_in_dim(qg, r, axis=2, keepdims=False)
        drow_idx = rs + jnp.arange(wh) - r + (NAT_WIN_H - 1)
        bias = jnp.take(rpb_cols, drow_idx, axis=1).transpose(0, 2, 1, 3)
        s = jnp.einsum('bhqd,bhwqjd->bhqwj', q_row, k_win).astype(jnp.float32) * scale
        s = s + bias[None].astype(jnp.float32)
        pr = jax.nn.softmax(s.reshape(B, H, GRID_W, wh * NAT_WIN_W), axis=-1)
        pr = pr.reshape(B, H, GRID_W, wh, NAT_WIN_W).astype(v_win.dtype)
        return jnp.einsum('bhqwj,bhwqjd->bhqd', pr, v_win)

    o = lax.map(row_block, jnp.arange(rows))
    return o.transpose(1, 0, 3, 2, 4).reshape(B, T, GROUP_W)


def pool_mixer(p, pool_w, pool_scale):
    B, T, _ = p.shape
    xg = p.astype(jnp.float32).reshape(B, T, N_POOL_GROUPS, POOL_GROUP_DIM)
    cs = jnp.concatenate([jnp.zeros_like(xg[:, :1]), jnp.cumsum(xg, axis=1)], axis=1)
    t = np.arange(T)
    outs = []
    for gi, win in enumerate(POOL_WINDOWS):
        lo = np.clip(t - win // 2, 0, T)
        hi = np.clip(t + win - win // 2, 0, T)
        cnt = (hi - lo).astype(np.float32)[None, :, None]
        mean = (cs[:, hi, gi] - cs[:, lo, gi]) / cnt
        outs.append(mean - xg[:, :, gi])
    d = jnp.stack(outs, axis=2)
    y = jnp.einsum('btgc,gce->btge', d, pool_w).reshape(B, T, GROUP_W)
    return y * pool_scale


def mixing_sublayer(h, w_in, w_out, rwkv_mu, rwkv_w0, rwkv_w2, rwkv_a0, rwkv_a2, rwkv_g2,
                    rwkv_k_k, rwkv_k_a, rwkv_r_k, rwkv_lnx_w, rwkv_lnx_b,
                    gqa_q_norm, gqa_k_norm, nat_rpb, pool_w, pool_scale):
    p = h @ w_in
    p_a, p_b, p_c, p_d = jnp.split(p, [A_COLS, A_COLS + B_COLS, A_COLS + B_COLS + C_COLS], axis=-1)
    y_a = rwkv7_mixer(p_a, rwkv_mu, rwkv_w0, rwkv_w2, rwkv_a0, rwkv_a2, rwkv_g2,
                      rwkv_k_k, rwkv_k_a, rwkv_r_k, rwkv_lnx_w, rwkv_lnx_b)
    y_b = gqa_mixer(p_b, gqa_q_norm, gqa_k_norm)
    y_c = natten_mixer(p_c, nat_rpb)
    y_d = pool_mixer(p_d, pool_w, pool_scale)
    y = jnp.concatenate([y_a.astype(h.dtype), y_b.astype(h.dtype), y_c.astype(h.dtype),
                         y_d.astype(h.dtype)], axis=-1)
    return y @ w_out


def swiglu(h, w_gate, w_up, w_down):
    return (jax.nn.silu(h @ w_gate) * (h @ w_up)) @ w_down


def moe_ffn(h, router, w_gate, w_up, w_down):
    logits = (h @ router).astype(jnp.float32)
    top_v, top_i = lax.top_k(logits, TOP_K)
    gates = jax.nn.softmax(top_v, axis=-1)
    combine = jnp.sum(jax.nn.one_hot(top_i, N_EXPERTS, dtype=jnp.float32) * gates[..., None], axis=-2)
    combine = combine.astype(h.dtype)
    y = jnp.zeros_like(h)
    for e in range(N_EXPERTS):
        y = y + combine[..., e:e + 1] * swiglu(h, w_gate[e], w_up[e], w_down[e])
    return y


def trunk(x, c, weights):
    (w_ada, b_ada, norm_mix_g, norm_ffn_g, w_in, w_out, rwkv_mu, rwkv_w0, rwkv_w2, rwkv_a0,
     rwkv_a2, rwkv_g2, rwkv_k_k, rwkv_k_a, rwkv_r_k, rwkv_lnx_w, rwkv_lnx_b, gqa_q_norm,
     gqa_k_norm, nat_rpb, pool_w, pool_scale, ffn_w_gate, ffn_w_up, ffn_w_down, moe_router,
     moe_w_gate, moe_w_up, moe_w_down, final_norm_g) = weights
    nb = c.shape[0]
    for l in range(DEPTH):
        mod = (jax.nn.silu(c) @ w_ada[l] + b_ada[l]).reshape(nb, N_MOD, 1, D_MODEL)
        shift1, scale1, gate1, shift2, scale2, gate2 = (mod[:, i] for i in range(N_MOD))
        h = rmsnorm(x, norm_mix_g[l]) * (1.0 + scale1) + shift1
        y = mixing_sublayer(h, w_in[l], w_out[l], rwkv_mu[l], rwkv_w0[l], rwkv_w2[l], rwkv_a0[l],
                            rwkv_a2[l], rwkv_g2[l], rwkv_k_k[l], rwkv_k_a[l], rwkv_r_k[l],
                            rwkv_lnx_w[l], rwkv_lnx_b[l], gqa_q_norm[l], gqa_k_norm[l],
                            nat_rpb[l], pool_w[l], pool_scale[l])
        x = x + gate1 * y
        h = rmsnorm(x, norm_ffn_g[l]) * (1.0 + scale2) + shift2
        i = l // 2
        if l % 2 == 0:
            f = swiglu(h, ffn_w_gate[i], ffn_w_up[i], ffn_w_down[i])
        else:
            f = moe_ffn(h, moe_router[i], moe_w_gate[i], moe_w_up[i], moe_w_down[i])
        x = x + gate2 * f
    return rmsnorm(x, final_norm_g)


def setup_inputs(seed: int = 0) -> dict:
    key = jax.random.key(seed)
    ks = iter(jax.random.split(key, 48))
    nrm = lambda shape, s: jax.random.normal(next(ks), shape, jnp.float32) * s
    L = DEPTH
    return {
        'x_prompt': nrm((BATCH, SEQ, D_MODEL), 1.0),
        'x_sample': nrm((DEC_BATCH, DEC_SEQ, D_MODEL), 1.0),
        'c_prompt': nrm((BATCH, D_MODEL), 1.0),
        'c_sample': nrm((DEC_BATCH, D_MODEL), 1.0),
        'w_ada': nrm((L, D_MODEL, N_MOD * D_MODEL), 0.5 * D_MODEL ** -0.5),
        'b_ada': nrm((L, N_MOD * D_MODEL), 0.02),
        'norm_mix_g': 1.0 + nrm((L, D_MODEL), 0.02),
        'norm_ffn_g': 1.0 + nrm((L, D_MODEL), 0.02),
        'w_in': nrm((L, D_MODEL, IN_COLS), D_MODEL ** -0.5),
        'w_out': nrm((L, D_MIX, D_MODEL), D_MIX ** -0.5),
        'rwkv_mu': jax.random.uniform(next(ks), (L, A_COLS), jnp.float32),
        'rwkv_w0': -1.5 + nrm((L, 2, GROUP_W), 0.5),
        'rwkv_w2': nrm((L, 2, DECAY_LORA, GROUP_W), 0.5 * DECAY_LORA ** -0.5),
        'rwkv_a0': nrm((L, 2, GROUP_W), 0.3),
        'rwkv_a2': nrm((L, 2, AAA_LORA, GROUP_W), AAA_LORA ** -0.5),
        'rwkv_g2': nrm((L, GATE_LORA, GROUP_W), GATE_LORA ** -0.5),
        'rwkv_k_k': 0.85 + nrm((L, GROUP_W), 0.05),
        'rwkv_k_a': 1.0 + nrm((L, GROUP_W), 0.05),
        'rwkv_r_k': nrm((L, N_RWKV_HEADS, HEAD_DIM), 0.1),
        'rwkv_lnx_w': 1.0 + nrm((L, GROUP_W), 0.02),
        'rwkv_lnx_b': nrm((L, GROUP_W), 0.02),
        'gqa_q_norm': 1.0 + nrm((L, HEAD_DIM), 0.02),
        'gqa_k_norm': 1.0 + nrm((L, HEAD_DIM), 0.02),
        'nat_rpb': nrm((L, N_NAT_HEADS, 2 * NAT_WIN_H - 1, 2 * NAT_WIN_W - 1), 0.1),
        'pool_w': nrm((L, N_POOL_GROUPS, POOL_GROUP_DIM, POOL_GROUP_DIM), POOL_GROUP_DIM ** -0.5),
        'pool_scale': 1.0 + nrm((L, GROUP_W), 0.1),
        'ffn_w_gate': nrm((N_DENSE, D_MODEL, D_FF_DENSE), D_MODEL ** -0.5),
        'ffn_w_up': nrm((N_DENSE, D_MODEL, D_FF_DENSE), D_MODEL ** -0.5),
        'ffn_w_down': nrm((N_DENSE, D_FF_DENSE, D_MODEL), D_FF_DENSE ** -0.5),
        'moe_router': nrm((N_MOE, D_MODEL, N_EXPERTS), D_MODEL ** -0.5),
        'moe_w_gate': nrm((N_MOE, N_EXPERTS, D_MODEL, D_FF_EXPERT), D_MODEL ** -0.5),
        'moe_w_up': nrm((N_MOE, N_EXPERTS, D_MODEL, D_FF_EXPERT), D_MODEL ** -0.5),
        'moe_w_down': nrm((N_MOE, N_EXPERTS, D_FF_EXPERT, D_MODEL), D_FF_EXPERT ** -0.5),
        'final_norm_g': 1.0 + nrm((D_MODEL,), 0.02),
    }


def reference(x_prompt, x_sample, c_prompt, c_sample, w_ada, b_ada, norm_mix_g, norm_ffn_g,
              w_in, w_out, rwkv_mu, rwkv_w0, rwkv_w2, rwkv_a0, rwkv_a2, rwkv_g2, rwkv_k_k,
              rwkv_k_a, rwkv_r_k, rwkv_lnx_w, rwkv_lnx_b, gqa_q_norm, gqa_k_norm, nat_rpb,
              pool_w, pool_scale, ffn_w_gate, ffn_w_up, ffn_w_down, moe_router, moe_w_gate,
              moe_w_up, moe_w_down, final_norm_g):
    weights = (w_ada, b_ada, norm_mix_g, norm_ffn_g, w_in, w_out, rwkv_mu, rwkv_w0, rwkv_w2,
               rwkv_a0, rwkv_a2, rwkv_g2, rwkv_k_k, rwkv_k_a, rwkv_r_k, rwkv_lnx_w, rwkv_lnx_b,
               gqa_q_norm, gqa_k_norm, nat_rpb, pool_w, pool_scale, ffn_w_gate, ffn_w_up,
               ffn_w_down, moe_router, moe_w_gate, moe_w_up, moe_w_down, final_norm_g)
    y_prompt = trunk(x_prompt, c_prompt, weights)
    y_sample = trunk(x_sample, c_sample, weights)
    return (y_prompt, y_sample)
```

```python
import contextlib
import numpy as np
import concourse.bass as bass
import concourse.mybir as mybir

F32 = mybir.dt.float32
BF16 = mybir.dt.bfloat16
I32 = mybir.dt.int32
AF = mybir.ActivationFunctionType
ALU = mybir.AluOpType
AX = mybir.AxisListType

ENGS = ("pe", "dve", "act", "pool", "sp")
MAXC = 30000
DMA_SLOTS = 8
MAXD_USES = 1800


class Res:
    __slots__ = ("name", "writers", "readers", "excl")

    def __init__(self, name="", excl=False):
        self.name = name
        self.writers = {}
        self.readers = {}
        self.excl = excl


class Op:
    __slots__ = ("eng", "idx", "fn", "deps", "is_dma", "signal", "sig", "slot", "use")

    def __init__(self, eng, fn, is_dma):
        self.eng = eng
        self.fn = fn
        self.is_dma = is_dma
        self.deps = {}
        self.signal = False
        self.sig = None
        self.slot = None
        self.use = None


class Prog:
    def __init__(self, nc, same_engine_sync=True):
        self.nc = nc
        self.same_engine_sync = same_engine_sync
        self.gstack = contextlib.ExitStack()
        self.pstack = None
        self.ops = None
        self.ndma = {e: 0 for e in ENGS}
        self.ccount = {e: 0 for e in ENGS}
        self.sems = {}
        self.all_res = []
        self.pending_barrier = {}
        self.n_inst = 0
        self.n_wait = 0
        self.eng_obj = {"pe": nc.tensor, "dve": nc.vector, "act": nc.scalar,
                        "pool": nc.gpsimd, "sp": nc.sync}

    def gsbuf(self, name, shape, dtype):
        return self.gstack.enter_context(self.nc.sbuf_tensor(name, list(shape), dtype))

    def sbuf(self, name, shape, dtype):
        return self.pstack.enter_context(self.nc.sbuf_tensor(f"{name}_p{self.phase_id}", list(shape), dtype))

    def psum(self, name, shape, dtype=F32):
        return self.pstack.enter_context(self.nc.psum_tensor(f"{name}_p{self.phase_id}", list(shape), dtype))

    def res(self, name="", excl=False):
        r = Res(name, excl)
        self.all_res.append(r)
        return r

    def pres(self, name=""):
        return self.res(name, True)

    def get_sem(self, key):
        if key not in self.sems:
            nm = "s_" + "_".join(str(x) for x in key)
            self.sems[key] = self.gstack.enter_context(self.nc.semaphore(nm))
        return self.sems[key]

    def begin(self):
        assert self.ops is None
        self.phase_id = getattr(self, "phase_id", 0) + 1
        self.pstack = contextlib.ExitStack()
        self.ops = {e: [] for e in ENGS}

    def _key(self, op):
        if op.is_dma:
            return ("dma", op.eng, op.slot)
        return ("c", op.eng)

    def _record(self, op, reads, writes):
        deps = op.deps

        def add(p):
            k = self._key(p)
            q = deps.get(k)
            if q is None or p.idx > q.idx:
                deps[k] = p

        for r in reads:
            for p in r.writers.values():
                add(p)
            if r.excl:
                for p in r.readers.values():
                    if p.eng != op.eng:
                        add(p)
        for w in writes:
            for p in w.writers.values():
                add(p)
            for p in w.readers.values():
                add(p)
        for k in list(deps.keys()):
            p = deps[k]
            if p is op:
                del deps[k]
                continue
            if (not p.is_dma) and p.eng == op.eng and not op.is_dma:
                if (not self.same_engine_sync) or op.eng == "pe":
                    del deps[k]
                    continue
            p.signal = True
        me = self._key(op)
        for r in reads:
            r.readers[me] = op
        for w in writes:
            w.writers = {me: op}
            w.readers = {}

    def op(self, eng, fn, R=(), W=()):
        o = Op(eng, fn, False)
        o.idx = len(self.ops[eng])
        self.ops[eng].append(o)
        self._record(o, R, W)
        return o

    def dma(self, q, out, in_, R=(), W=(), **kw):
        o = Op(q, (lambda e: e.dma_start(out=out, in_=in_, **kw)), True)
        o.idx = len(self.ops[q])
        n = self.ndma[q]
        self.ndma[q] = n + 1
        o.slot = n % DMA_SLOTS
        o.use = n // DMA_SLOTS
        o.signal = True
        self.ops[q].append(o)
        self._record(o, R, W)
        return o

    def pe(self, fn, R=(), W=()):
        return self.op("pe", fn, R, W)

    def dve(self, fn, R=(), W=()):
        return self.op("dve", fn, R, W)

    def act(self, fn, R=(), W=()):
        return self.op("act", fn, R, W)

    def pool(self, fn, R=(), W=()):
        return self.op("pool", fn, R, W)

    def end(self, final=False):
        nc = self.nc
        bar = {}
        for e in ENGS:
            last = None
            for o in self.ops[e]:
                if not o.is_dma:
                    last = o
            if last is not None:
                last.signal = True
                bar[e] = last
        for e in ENGS:
            for o in self.ops[e]:
                if o.is_dma:
                    ep = o.use // MAXD_USES
                    u = o.use % MAXD_USES
                    o.sig = (("d", e, o.slot, ep), 16 * (u + 1), 16)
                elif o.signal:
                    cnt = self.ccount[e]
                    self.ccount[e] = cnt + 1
                    o.sig = (("c", e, cnt // MAXC), cnt % MAXC + 1, 1)
                if o.sig is not None:
                    self.get_sem(o.sig[0])
        new_barrier = {}
        for e in ENGS:
            if e in bar:
                new_barrier[bar[e].sig[0]] = bar[e].sig[1]
            for o in self.ops[e]:
                if o.is_dma:
                    k, v, _ = o.sig
                    if new_barrier.get(k, 0) < v:
                        new_barrier[k] = v
        start_waits = dict(self.pending_barrier)
        sems = self.sems
        ops = self.ops
        stats = self

        with nc.Block() as block:
            def run(e):
                def body(eng):
                    waited = {}
                    for (k, v) in start_waits.items():
                        if k[0] == "c" and k[1] == e:
                            continue
                        eng.wait_ge(sems[k], v)
                        waited[k] = v
                        stats.n_wait += 1
                    for o in ops[e]:
                        waits = []
                        for p in o.deps.values():
                            waits.append((p.sig[0], p.sig[1]))
                        if o.is_dma:
                            k = o.sig[0]
                            if o.sig[1] > 16:
                                waits.append((k, o.sig[1] - 16))
                            elif k[3] > 0:
                                waits.append(((k[0], k[1], k[2], k[3] - 1), 16 * MAXD_USES))
                        for (k, v) in waits:
                            if waited.get(k, 0) >= v:
                                continue
                            waited[k] = v
                            eng.wait_ge(sems[k], v)
                            stats.n_wait += 1
                        inst = o.fn(eng)
                        stats.n_inst += 1
                        if o.sig is not None:
                            inst.then_inc(sems[o.sig[0]], o.sig[2])
                    if final:
                        for (k, v) in new_barrier.items():
                            if waited.get(k, 0) >= v:
                                continue
                            if k[0] == "c" and k[1] == e:
                                continue
                            eng.wait_ge(sems[k], v)
                return body
            block.tensor(run("pe"))
            block.vector(run("dve"))
            block.scalar(run("act"))
            block.gpsimd(run("pool"))
            block.sync(run("sp"))
        self.pending_barrier = new_barrier
        for r in self.all_res:
            r.writers = {}
            r.readers = {}
        self.ops = None
        self.pstack.close()
        self.pstack = None

    def finish(self):
        self.gstack.close()


from concourse.bass_utils import run_bass_kernel_spmd

D = 1024
IN_COLS = 2688
NCH_IN = 21
DFF_D = 2816
DFF_E = 3584
NEXP = 8
EPS = 1e-6
GN_EPS = 64e-5
TT = 512
PV_GMIX, PV_GFFN, PV_BADA, PV_MU, PV_W0, PV_A0 = 0, 8, 16, 64, 73, 77
PV_KK, PV_KA, PV_RK, PV_LNW, PV_LNB, PV_QN, PV_KN, PV_PS = 81, 83, 85, 87, 89, 91, 92, 93
PV_N = 95
C_ID, C_BLK, C_SWAP, C_ANTI, C_CMASK, C_EXPO, C_EPS, C_MU, C_ML, C_GFIN = 0, 128, 256, 384, 448, 512, 513, 516, 644, 772
C_RST, C_IP = 780, 1036
NCONST = 1100


def bcast(ap, shape):
    return ap.to_broadcast(list(shape))


class Ctx:
    pass


def build_program(seq_lens, depth, dbg=()):
    nc = bass.Bass("TRN2", target_bir_lowering=False)
    P = Prog(nc)
    C = Ctx()
    C.nc, C.P = nc, P
    C.seq_lens = list(seq_lens)
    C.NS = len(seq_lens)
    C.NT = sum(seq_lens)
    C.offs = [sum(seq_lens[:i]) for i in range(len(seq_lens))]
    C.L = depth
    C.dbg = set(dbg)
    NT, NS, L = C.NT, C.NS, C.L
    Tmax = max(seq_lens)
    C.Tmax = Tmax
    n_dense = (depth + 1) // 2
    n_moe = depth // 2

    def din(name, shape, dt=F32):
        return nc.dram_tensor(name, list(shape), dt, kind="ExternalInput").ap()

    def dscr(name, shape, dt=F32):
        kind = "ExternalOutput" if name in C.dbg else "Internal"
        return nc.dram_tensor(name, list(shape), dt, kind=kind).ap()

    C.xT = din("xT", [D, NT])
    C.cT = din("cT", [128, 8, NS])
    C.w_ada = din("w_ada", [L, D, 6 * D])
    C.pvec = din("pvec", [L, 128, PV_N])
    C.w_in = din("w_in", [L, D, IN_COLS])
    C.w_out = din("w_out", [L, D, D])
    C.w2 = din("rwkv_w2", [L, 128, 256])
    C.a2 = din("rwkv_a2", [L, 128, 256])
    C.g2 = din("rwkv_g2", [L, 128, 256])
    C.rpb = din("rpb_pad", [L, 2048])
    C.pool_w = din("pool_w", [L, 4, 64, 64])
    C.full = "stopS" not in C.dbg and "stopA" not in C.dbg and "stopM" not in C.dbg
    if C.full:
        C.ffn_g = din("ffn_w_gate", [n_dense, D, DFF_D])
        C.ffn_u = din("ffn_w_up", [n_dense, D, DFF_D])
        C.ffn_d = din("ffn_w_down", [n_dense, DFF_D, D])
        if n_moe:
            C.router = din("moe_router", [n_moe, D, NEXP])
            C.moe_g = din("moe_w_gate", [n_moe, NEXP, D, DFF_E])
            C.moe_u = din("moe_w_up", [n_moe, NEXP, D, DFF_E])
            C.moe_d = din("moe_w_down", [n_moe, NEXP, DFF_E, D])
    C.consts = din("consts", [128, NCONST])
    C.Tsel = sorted(set(seq_lens), reverse=True)
    C.invc = din("invc", [len(C.Tsel), 128, 2, 4096])
    C.yT = nc.dram_tensor("yT", [D, NT], F32, kind="ExternalOutput").ap()

    C.pa = dscr("s_pa", [1152, NT])
    C.gqqk = dscr("s_gqqk", [384, NT], BF16)
    C.gqv = dscr("s_gqv", [NT, 128], BF16)
    C.ntqk = dscr("s_ntqk", [512, NT], BF16)
    C.ntv = dscr("s_ntv", [NT, 256], BF16)
    C.pd = dscr("s_pd", [256, NT])
    C.ymix = dscr("s_ymix", [D, NT], BF16)
    C.cs = din("cs_tab", [2, 128, 4096])
    C.g23 = dscr("s_g23", [2, 256, NT])
    C.yscan = dscr("s_yscan", [2, NT, 256])
    C.rkv = dscr("s_rkv", [10, 256, NT])
    C.h2s = dscr("s_h2", [D, NT], BF16)
    C.comb = dscr("s_comb", [NT, NEXP])
    C.dscr = dscr

    C.cst = P.gsbuf("cst", [128, NCONST], F32)
    C.cstb = P.gsbuf("cstb", [128, 512], BF16)
    C.pv = P.gsbuf("pv", [128, L, PV_N], F32)
    C.modT = P.gsbuf("modT", [128, L, 48, NS], F32)
    C.modA = P.gsbuf("modA", [128, L, 2, 8, NS], F32)
    C.qkg = P.gsbuf("qkg", [128, L, 2], F32)
    C.rnull = P.res("null")

    ph_setup(C)
    for l in range(L):
        if "stopS" in C.dbg:
            break
        ph_A(C, l)
        if "stopA" in C.dbg:
            break
        ph_pool(C, l)
        ph_nat(C, l)
        ph_gqa(C, l)
        ph_rwkv(C, l)
        if "stopM" in C.dbg:
            break
        ph_C(C, l)
    P.begin()
    P.end(final=True)
    P.finish()
    return nc


def ph_setup(C):
    P, nc = C.P, C.nc
    L, NS = C.L, C.NS
    rn = C.rnull
    P.begin()
    r_c = P.res()
    P.dma("sp", C.cst[:], C.consts, W=[r_c])
    r_cb = P.res()
    P.dma("pool", C.cstb[:, 0:384], C.consts[:, 0:384], W=[r_cb])
    P.pool(lambda e: e.memset(C.cstb[:, 384:512], 1.0), W=[r_cb])
    r_pv = P.res()
    P.dma("sp", C.pv[:], C.pvec.rearrange("l p n -> p l n"), W=[r_pv])
    P.dve(lambda e: e.tensor_scalar(out=C.qkg[:, :, 0:1], in0=C.pv[:, :, PV_QN:PV_QN + 1], scalar1=0.125,
                                    scalar2=None, op0=ALU.mult), R=[r_pv], W=[rn])
    P.dve(lambda e: e.tensor_copy(out=C.qkg[:, :, 1:2], in_=C.pv[:, :, PV_KN:PV_KN + 1]), R=[r_pv], W=[rn])
    C.r_csd = [P.res(), P.res()]
    ct = P.sbuf("ct", [128, 8, NS], F32)
    r_ct = P.res()
    P.dma("sp", ct[:], C.cT, W=[r_ct])
    sc = P.sbuf("sc", [128, 8, NS], BF16)
    r_sc = P.res()
    P.act(lambda e: e.activation(out=sc[:], in_=ct[:], func=AF.Silu), R=[r_ct], W=[r_sc])
    wts = [P.sbuf(f"wada{i}", [128, 8, 768], BF16) for i in range(2)]
    r_w = [P.res(), P.res()]
    pss = [P.psum(f"psada{i}", [128, 6, NS], F32) for i in range(2)]
    r_ps = [P.res(), P.res()]
    it = 0
    for l in range(L):
        for j in range(8):
            b = it % 2
            it += 1
            wt, ps = wts[b], pss[b]
            P.dma("pool", wt[:], C.w_ada[l, :, j * 768:(j + 1) * 768].rearrange("(c p) f -> p c f", p=128),
                  W=[r_w[b]])
            for f in range(6):
                for kc in range(8):
                    P.pe(lambda e, ps=ps, wt=wt, f=f, kc=kc: e.matmul(
                        ps[:, f, :], wt[:, kc, f * 128:(f + 1) * 128], sc[:, kc, :], start=(kc == 0), stop=(kc == 7)),
                        R=[r_w[b], r_sc], W=[r_ps[b]])
            P.dve(lambda e, ps=ps, l=l, j=j: e.tensor_tensor(
                out=C.modT[:, l, j * 6:(j + 1) * 6, :], in0=ps[:],
                in1=bcast(C.pv[:, l, PV_BADA + j * 6:PV_BADA + (j + 1) * 6].unsqueeze(2), [128, 6, NS]), op=ALU.add),
                R=[r_ps[b], r_pv], W=[rn])
    for l in range(L):
        for i, (gcol, sidx) in enumerate(((PV_GMIX, 1), (PV_GFFN, 4))):
            P.dve(lambda e, l=l, i=i, gcol=gcol, sidx=sidx: e.scalar_tensor_tensor(
                out=C.modA[:, l, i], in0=C.modT[:, l, sidx * 8:(sidx + 1) * 8, :], scalar=1.0,
                in1=bcast(C.pv[:, l, gcol:gcol + 8].unsqueeze(2), [128, 8, NS]), op0=ALU.add, op1=ALU.mult),
                R=[rn, r_pv], W=[rn])
    P.end()


def ph_A(C, l):
    P = C.P
    NT = C.NT
    rn = C.rnull
    X = C.xT if l == 0 else C.yT
    P.begin()
    wi = P.sbuf("wi", [128, 8, IN_COLS], BF16)
    r_wi = [P.res() for _ in range(8)]
    for kc in range(8):
        P.dma("pool", wi[:, kc, :], C.w_in[l, kc * 128:(kc + 1) * 128, :], W=[r_wi[kc]])
    xt = [P.sbuf(f"xt{i}", [128, 8, TT], F32) for i in range(2)]
    r_xt = [P.res() for _ in range(2)]
    hb = [P.sbuf(f"hb{i}", [128, 8, TT], BF16) for i in range(2)]
    r_hb = [P.res() for _ in range(2)]
    sq = P.sbuf("sq", [128, 8, TT], BF16)
    r_sq = P.res()
    rt = P.sbuf("rt", [128, TT], F32)
    r_rt = P.res()
    cst = [P.sbuf(f"cosb{i}", [128, 2, TT], F32) for i in range(2)]
    r_cst = [P.res() for _ in range(2)]
    st_pa = [P.sbuf(f"st_pa{i}", [128, 9, TT], F32) for i in range(2)]
    st_gq = [P.sbuf(f"st_gq{i}", [128, 3, TT], BF16) for i in range(2)]
    st_gv = [P.sbuf(f"st_gv{i}", [128, 4, 128], BF16) for i in range(2)]
    st_nq = [P.sbuf(f"st_nq{i}", [128, 4, TT], BF16) for i in range(2)]
    st_nv = [P.sbuf(f"st_nv{i}", [128, 4, 256], BF16) for i in range(2)]
    st_pd = [P.sbuf(f"st_pd{i}", [128, 2, TT], F32) for i in range(2)]
    r_st = [[P.res() for _ in range(6)] for _ in range(2)]
    sq2 = P.sbuf("sq2", [128, TT], BF16); r_sq2 = P.res()
    qf = P.sbuf("qf", [128, TT], F32); r_qf = P.res()
    rq = P.sbuf("rq", [128, TT], F32); r_rq = P.res()
    qnb = P.sbuf("qnb", [128, TT], BF16); r_qnb = P.res()
    t1 = P.sbuf("t1", [128, TT], F32); r_t1 = P.res()
    t2 = P.sbuf("t2", [128, TT], F32); r_t2 = P.res()
    ps_ss = P.psum("ps_ss", [128, TT]); r_pss = P.res()
    ps_o = [P.psum(f"ps_o{i}", [128, TT]) for i in range(4)]
    r_pso = [P.res() for _ in range(4)]
    ps_n = P.psum("ps_n", [128, TT]); r_psn = P.res()
    ps_w = P.psum("ps_w", [128, TT]); r_psw = P.res()
    ones_b = C.cstb[:, 384:512]
    blk_b = C.cstb[:, 128:256]
    swap_b = C.cstb[:, 256:384]
    eps_ap = C.cst[:, C_EPS:C_EPS + 1]

    tiles = []
    for s, T in enumerate(C.seq_lens):
        for i in range(T // TT):
            tiles.append((s, i * TT, C.offs[s] + i * TT))

    def prep(ti):
        s, t0, g0 = tiles[ti]
        b = ti % 2
        P.dma("sp", xt[b][:], X[:, g0:g0 + TT].rearrange("(c p) t -> p c t", p=128), W=[r_xt[b]])
        P.dma("sp", cst[b][:, 0, :], C.cs[0, :, t0:t0 + TT], R=[C.r_csd[0]], W=[r_cst[b]])
        P.dma("sp", cst[b][:, 1, :], C.cs[1, :, t0:t0 + TT], R=[C.r_csd[1]], W=[r_cst[b]])
        P.act(lambda e: e.activation(out=sq[:], in_=xt[b][:], func=AF.Square), R=[r_xt[b]], W=[r_sq])
        for c in range(8):
            P.pe(lambda e, c=c: e.matmul(ps_ss[:], ones_b, sq[:, c, :], start=(c == 0), stop=(c == 7)),
                 R=[r_sq], W=[r_pss])
        P.act(lambda e: e.activation(out=rt[:], in_=ps_ss[:], func=AF.Sqrt, bias=eps_ap, scale=1.0 / D),
              R=[r_pss], W=[r_rt])
        P.dve(lambda e: e.reciprocal(out=rt[:], in_=rt[:]), R=[r_rt], W=[r_rt])
        P.dve(lambda e: e.tensor_tensor(out=xt[b][:], in0=xt[b][:], in1=bcast(rt[:].unsqueeze(1), [128, 8, TT]),
                                        op=ALU.mult), R=[r_xt[b], r_rt], W=[r_xt[b]])
        for c in range(8):
            if c % 2 == 0:
                P.act(lambda e, c=c: e.activation(out=hb[b][:, c, :], in_=xt[b][:, c, :], func=AF.Identity,
                                                  bias=C.modT[:, l, 0 * 8 + c, s:s + 1],
                                                  scale=C.modA[:, l, 0, c, s:s + 1]), R=[r_xt[b]], W=[r_hb[b]])
            else:
                P.dve(lambda e, c=c: e.tensor_scalar(out=hb[b][:, c, :], in0=xt[b][:, c, :],
                                                     scalar1=C.modA[:, l, 0, c, s:s + 1],
                                                     scalar2=C.modT[:, l, 0 * 8 + c, s:s + 1],
                                                     op0=ALU.mult, op1=ALU.add), R=[r_xt[b]], W=[r_hb[b]])

    state = {"pi": 0}

    def mm_fm(ch, b):
        pi = state["pi"] % 4
        state["pi"] += 1
        for kc in range(8):
            P.pe(lambda e, kc=kc: e.matmul(ps_o[pi][:], wi[:, kc, ch * 128:(ch + 1) * 128], hb[b][:, kc, :],
                                           start=(kc == 0), stop=(kc == 7)),
                 R=[r_wi[kc], r_hb[b]], W=[r_pso[pi]])
        return pi

    import os
    ASTOP = int(os.environ.get("ASTOP", "99"))

    def main(ti):
        s, t0, g0 = tiles[ti]
        b = ti % 2
        rs = r_st[b]
        if ASTOP < 2:
            return
        for ch in range(9):
            pi = mm_fm(ch, b)
            if ch % 2 == 0:
                P.act(lambda e, pi=pi, ch=ch: e.activation(out=st_pa[b][:, ch, :], in_=ps_o[pi][:], func=AF.Copy),
                      R=[r_pso[pi]], W=[rs[0]])
            else:
                P.dve(lambda e, pi=pi, ch=ch: e.tensor_copy(out=st_pa[b][:, ch, :], in_=ps_o[pi][:]),
                      R=[r_pso[pi]], W=[rs[0]])
        P.dma("sp", C.pa[:, g0:g0 + TT].rearrange("(c p) t -> p c t", p=128), st_pa[b][:], R=[rs[0]])
        if ASTOP < 3:
            return
        GS = int(os.environ.get("GSTOP", "99"))
        for j in range(3):
            ch = 9 + j
            pi = mm_fm(ch, b)
            gi = 0 if j < 2 else 1
            P.dve(lambda e, pi=pi: e.tensor_copy(out=qf[:], in_=ps_o[pi][:]), R=[r_pso[pi]], W=[r_qf])
            P.act(lambda e: e.activation(out=sq2[:], in_=qf[:], func=AF.Square), R=[r_qf], W=[r_sq2])
            if GS < 1: continue
            P.pe(lambda e: e.matmul(ps_n[:], blk_b, sq2[:], start=True, stop=True), R=[r_sq2], W=[r_psn])
            P.act(lambda e: e.activation(out=rq[:], in_=ps_n[:], func=AF.Sqrt, bias=eps_ap, scale=1.0 / 64),
                  R=[r_psn], W=[r_rq])
            P.dve(lambda e: e.reciprocal(out=rq[:], in_=rq[:]), R=[r_rq], W=[r_rq])
            if GS < 2: continue
            P.dve(lambda e, gi=gi: e.scalar_tensor_tensor(out=qnb[:], in0=qf[:], scalar=C.qkg[:, l, gi:gi + 1],
                                                          in1=rq[:], op0=ALU.mult, op1=ALU.mult),
                  R=[r_qf, r_rq], W=[r_qnb])
            if GS < 3: continue
            P.pe(lambda e: e.matmul(ps_w[:], swap_b, qnb[:], start=True, stop=True), R=[r_qnb], W=[r_psw])
            P.dve(lambda e: e.tensor_tensor(out=t1[:], in0=qnb[:], in1=cst[b][:, 0, :], op=ALU.mult),
                  R=[r_qnb, r_cst[b]], W=[r_t1])
            if GS < 4: continue
            P.dve(lambda e: e.tensor_tensor(out=t2[:], in0=ps_w[:], in1=cst[b][:, 1, :], op=ALU.mult),
                  R=[r_psw, r_cst[b]], W=[r_t2])
            if GS < 5: continue
            P.dve(lambda e, j=j: e.tensor_tensor(out=st_gq[b][:, j, :], in0=t1[:], in1=t2[:], op=ALU.add),
                  R=[r_t1, r_t2], W=[rs[1]])
        if GS < 6:
            return
        P.dma("sp", C.gqqk[:, g0:g0 + TT].rearrange("(c p) t -> p c t", p=128), st_gq[b][:], R=[rs[1]])
        if ASTOP < 4:
            return
        pi = state["pi"] % 4
        state["pi"] += 1
        for tb in range(4):
            for kc in range(8):
                P.pe(lambda e, tb=tb, kc=kc, pi=pi: e.matmul(ps_o[pi][:, tb * 128:(tb + 1) * 128],
                                                      hb[b][:, kc, tb * 128:(tb + 1) * 128],
                                                      wi[:, kc, 12 * 128:13 * 128], start=(kc == 0), stop=(kc == 7)),
                     R=[r_wi[kc], r_hb[b]], W=[r_pso[pi]])
        P.dve(lambda e, pi=pi: e.tensor_copy(out=st_gv[b][:], in_=ps_o[pi][:].rearrange("p (b c) -> p b c", c=128)),
              R=[r_pso[pi]], W=[rs[2]])
        P.dma("sp", C.gqv[g0:g0 + TT, :].rearrange("(b p) c -> p b c", p=128), st_gv[b][:], R=[rs[2]])
        if ASTOP < 5:
            return
        for j in range(4):
            ch = 13 + j
            pi = mm_fm(ch, b)
            if j < 2:
                P.act(lambda e, pi=pi, j=j: e.activation(out=st_nq[b][:, j, :], in_=ps_o[pi][:], func=AF.Copy,
                                                         scale=0.125), R=[r_pso[pi]], W=[rs[3]])
            else:
                P.dve(lambda e, pi=pi, j=j: e.tensor_copy(out=st_nq[b][:, j, :], in_=ps_o[pi][:]),
                      R=[r_pso[pi]], W=[rs[3]])
        P.dma("sp", C.ntqk[:, g0:g0 + TT].rearrange("(c p) t -> p c t", p=128), st_nq[b][:], R=[rs[3]])
        if ASTOP < 6:
            return
        for half in range(2):
            pi = state["pi"] % 4
            state["pi"] += 1
            for tb2 in range(2):
                tb = half * 2 + tb2
                for kc in range(8):
                    P.pe(lambda e, tb=tb, tb2=tb2, kc=kc, pi=pi: e.matmul(ps_o[pi][:, tb2 * 256:(tb2 + 1) * 256],
                                                                   hb[b][:, kc, tb * 128:(tb + 1) * 128],
                                                                   wi[:, kc, 17 * 128:19 * 128],
                                                                   start=(kc == 0), stop=(kc == 7)),
                         R=[r_wi[kc], r_hb[b]], W=[r_pso[pi]])
            P.dve(lambda e, pi=pi, half=half: e.tensor_copy(
                out=st_nv[b][:, half * 2:half * 2 + 2, :], in_=ps_o[pi][:].rearrange("p (b c) -> p b c", c=256)),
                R=[r_pso[pi]], W=[rs[4]])
        P.dma("sp", C.ntv[g0:g0 + TT, :].rearrange("(b p) c -> p b c", p=128), st_nv[b][:], R=[rs[4]])
        if ASTOP < 7:
            return
        for j in range(2):
            pi = mm_fm(19 + j, b)
            P.dve(lambda e, pi=pi, j=j: e.tensor_copy(out=st_pd[b][:, j, :], in_=ps_o[pi][:]),
                  R=[r_pso[pi]], W=[rs[5]])
        P.dma("sp", C.pd[:, g0:g0 + TT].rearrange("(c p) t -> p c t", p=128), st_pd[b][:], R=[rs[5]])

    prep(0)
    for ti in range(len(tiles)):
        if ti + 1 < len(tiles):
            prep(ti + 1)
        main(ti)
    P.end()


GQ_PERM = np.concatenate([np.arange(0, 64), np.arange(128, 192), np.arange(64, 128), np.arange(192, 256)])


def make_consts():
    c = np.zeros((128, NCONST), np.float32)
    c[:, C_ID:C_ID + 128] = np.eye(128)
    blk = np.zeros((128, 128), np.float32)
    blk[:64, :64] = 1.0
    blk[64:, 64:] = 1.0
    c[:, C_BLK:C_BLK + 128] = blk
    sw = np.zeros((128, 128), np.float32)
    for i in range(64):
        sw[2 * i + 1, 2 * i] = -1.0
        sw[2 * i, 2 * i + 1] = 1.0
    c[:, C_SWAP:C_SWAP + 128] = sw
    anti = np.zeros((64, 64), np.float32)
    for i in range(64):
        anti[i, 63 - i] = 1.0
    c[0:64, C_ANTI:C_ANTI + 64] = anti
    c[64:128, C_ANTI:C_ANTI + 64] = anti
    cols = np.arange(64)
    cs = np.clip(cols - 8, 0, 48)
    cm = ((cols[:, None] >= cs[None, :]) & (cols[:, None] < cs[None, :] + 16)).astype(np.float32)
    c[0:64, C_CMASK:C_CMASK + 64] = cm
    c[64:128, C_CMASK:C_CMASK + 64] = cm
    p = np.arange(128)
    c[:, C_EXPO] = (((p % 64) // 2) % 16) / 16.0
    c[:, C_EPS] = EPS
    c[:, C_EPS + 1] = GN_EPS
    c[:, C_EPS + 2] = 1e-24
    s_ = np.arange(64)[:, None]
    t_ = np.arange(64)[None, :]
    mu = np.zeros((128, 128), np.float32)
    ml = np.zeros((128, 128), np.float32)
    for h in range(2):
        mu[h * 64:(h + 1) * 64, 0:64] = (s_ < t_)
        mu[h * 64:(h + 1) * 64, 64:128] = (s_ <= t_)
        ml[h * 64:(h + 1) * 64, 0:64] = (s_ > t_)
        ml[h * 64:(h + 1) * 64, 64:128] = (s_ >= t_)
    c[:, C_MU:C_MU + 128] = mu
    c[:, C_ML:C_ML + 128] = ml
    rst = np.ones((128, 256), np.float32)
    rst[:, ::64] = 0.0
    c[:, C_RST:C_RST + 256] = rst
    c[:, C_IP:C_IP + 64] = np.concatenate([np.eye(64), np.eye(64)], axis=0)
    return c


def make_rope_tab():
    t = np.arange(4096)
    row = (t // 64).astype(np.float32)
    col = (t % 64).astype(np.float32)
    p = np.arange(128)
    i = (p % 64) // 2
    inv = (10000.0 ** (-((i % 16).astype(np.float32)) / 16.0)).astype(np.float32)
    pos = np.where((i < 16)[:, None], row[None, :], col[None, :]).astype(np.float32)
    ang = pos * inv[:, None]
    return np.stack([np.cos(ang), np.sin(ang)]).astype(np.float32)


def make_invc(Ts):
    out = np.zeros((len(Ts), 128, 2, 4096), np.float32)
    wins = (2, 4, 8, 16)
    for ti, T in enumerate(Ts):
        t = np.arange(T)
        for g, w in enumerate(wins):
            lo = np.clip(t - w // 2, 0, T)
            hi = np.clip(t + w - w // 2, 0, T)
            inv = (1.0 / (hi - lo).astype(np.float32)).astype(np.float32)
            j, hp = g // 2, g % 2
            out[ti, hp * 64:(hp + 1) * 64, j, :T] = inv[None, :]
    return out


def pack_weights(inp, L, seq_lens, full=True):
    f = lambda a: np.ascontiguousarray(np.asarray(a, dtype=np.float32))
    w = {}
    w["w_ada"] = f(inp["w_ada"])[:L]
    pv = np.zeros((L, 128, PV_N), np.float32)

    def fm(v, n):
        return np.asarray(v, np.float32).reshape(L, n, 128).transpose(0, 2, 1)

    pv[:, :, PV_GMIX:PV_GMIX + 8] = fm(inp["norm_mix_g"][:L], 8)
    pv[:, :, PV_GFFN:PV_GFFN + 8] = fm(inp["norm_ffn_g"][:L], 8)
    pv[:, :, PV_BADA:PV_BADA + 48] = fm(inp["b_ada"][:L], 48)
    pv[:, :, PV_MU:PV_MU + 9] = fm(inp["rwkv_mu"][:L], 9)
    w0 = np.asarray(inp["rwkv_w0"][:L], np.float32).reshape(L, 2, 2, 128)
    pv[:, :, PV_W0:PV_W0 + 4] = w0.transpose(0, 3, 1, 2).reshape(L, 128, 4)
    a0 = np.asarray(inp["rwkv_a0"][:L], np.float32).reshape(L, 2, 2, 128)
    pv[:, :, PV_A0:PV_A0 + 4] = a0.transpose(0, 3, 1, 2).reshape(L, 128, 4)
    pv[:, :, PV_KK:PV_KK + 2] = fm(inp["rwkv_k_k"][:L], 2)
    pv[:, :, PV_KA:PV_KA + 2] = fm(inp["rwkv_k_a"][:L], 2)
    pv[:, :, PV_RK:PV_RK + 2] = fm(np.asarray(inp["rwkv_r_k"][:L]).reshape(L, 256), 2)
    pv[:, :, PV_LNW:PV_LNW + 2] = fm(inp["rwkv_lnx_w"][:L], 2)
    pv[:, :, PV_LNB:PV_LNB + 2] = fm(inp["rwkv_lnx_b"][:L], 2)
    pv[:, :, PV_QN] = np.tile(np.asarray(inp["gqa_q_norm"][:L], np.float32), (1, 2))
    pv[:, :, PV_KN] = np.tile(np.asarray(inp["gqa_k_norm"][:L], np.float32), (1, 2))
    pv[:, :, PV_PS:PV_PS + 2] = fm(inp["pool_scale"][:L], 2)
    w["pvec"] = pv
    wi = f(inp["w_in"])[:L].copy()
    wi[:, :, 1152:1408] = wi[:, :, 1152 + GQ_PERM]
    w["w_in"] = wi
    wo = f(inp["w_out"])[:L].copy()
    wo[:, 256:512, :] = wo[:, 256 + GQ_PERM, :]
    w["w_out"] = wo
    w["rwkv_w2"] = f(inp["rwkv_w2"])[:L].reshape(L, 128, 256)
    w["rwkv_a2"] = f(inp["rwkv_a2"])[:L].reshape(L, 128, 256)
    w["rwkv_g2"] = f(inp["rwkv_g2"])[:L]
    rp = np.zeros((L, 2048), np.float32)
    rp[:, 48:48 + 1860] = np.asarray(inp["nat_rpb"][:L], np.float32).reshape(L, 1860)
    w["rpb_pad"] = rp
    w["pool_w"] = f(inp["pool_w"])[:L]
    if full:
        nd, nm = (L + 1) // 2, L // 2
        for k in ("ffn_w_gate", "ffn_w_up", "ffn_w_down"):
            w[k] = f(inp[k])[:nd]
        if nm:
            for k in ("moe_router", "moe_w_gate", "moe_w_up", "moe_w_down"):
                w[k] = f(inp[k])[:nm]
    w["consts"] = make_consts()
    w["consts"][:, C_GFIN:C_GFIN + 8] = np.asarray(inp["final_norm_g"], np.float32).reshape(8, 128).T
    w["cs_tab"] = make_rope_tab()
    w["invc"] = make_invc(sorted(set(seq_lens), reverse=True))
    return w


def ph_pool(C, l):
    P = C.P
    P.begin()
    W = TT + 16
    pwf = P.sbuf("pwf", [128, 2, 128], F32); r_pwf = P.res()
    pwb = P.sbuf("pwb", [128, 2, 128], BF16); r_pwb = P.res()
    P.dve(lambda e: e.memset(pwf[:], 0.0), W=[r_pwf])
    for g in range(4):
        j, hp = g // 2, g % 2
        P.dma("sp", pwf[64 * hp:64 * hp + 64, j, 64 * hp:64 * hp + 64], C.pool_w[l, g], R=[r_pwf], W=[r_pwf])
    P.dve(lambda e: e.tensor_copy(out=pwb[:], in_=pwf[:]), R=[r_pwf], W=[r_pwb])
    X = [P.sbuf(f"X{i}", [128, 2, W], F32) for i in range(2)]; r_X = [P.res() for _ in range(2)]
    IV = [P.sbuf(f"IV{i}", [128, 2, TT], F32) for i in range(2)]; r_IV = [P.res() for _ in range(2)]
    A2 = P.sbuf("A2", [128, 2, W], F32); r_A2 = P.res()
    B4 = P.sbuf("B4", [128, 2, W], F32); r_B4 = P.res()
    B8 = P.sbuf("B8", [128, W], F32); r_B8 = P.res()
    S = P.sbuf("S", [128, 2, TT], F32); r_S = P.res()
    db = P.sbuf("db", [128, 2, TT], BF16); r_db = P.res()
    yo = [P.sbuf(f"yo{i}", [128, 2, TT], BF16) for i in range(2)]; r_yo = [P.res() for _ in range(2)]
    ps = [P.psum(f"pps{i}", [128, TT]) for i in range(2)]; r_ps = [P.pres() for _ in range(2)]
    it = 0
    for s, T in enumerate(C.seq_lens):
        tsel = C.Tsel.index(T)
        for i in range(T // TT):
            t0 = i * TT
            g0 = C.offs[s] + t0
            b = it % 2
            it += 1
            lo = max(t0 - 8, 0)
            hi = min(t0 + TT + 8, T)
            if lo > t0 - 8:
                P.pool(lambda e, b=b: e.memset(X[b][:, :, 0:8], 0.0), W=[r_X[b]])
            if hi < t0 + TT + 8:
                P.pool(lambda e, b=b: e.memset(X[b][:, :, W - 8:W], 0.0), W=[r_X[b]])
            u0 = lo - (t0 - 8)
            P.dma("sp", X[b][:, :, u0:u0 + (hi - lo)],
                  C.pd[:, C.offs[s] + lo:C.offs[s] + hi].rearrange("(c p) t -> p c t", p=128), R=[r_X[b]], W=[r_X[b]])
            P.dma("sp", IV[b][:], C.invc[tsel, :, :, t0:t0 + TT], W=[r_IV[b]])
            x = X[b]
            P.pool(lambda e, x=x: e.tensor_tensor(out=A2[:, :, 1:W], in0=x[:, :, 1:W], in1=x[:, :, 0:W - 1], op=ALU.add),
                   R=[r_X[b]], W=[r_A2])
            P.pool(lambda e: e.tensor_tensor(out=B4[:, :, 2:W - 1], in0=A2[:, :, 1:W - 2], in1=A2[:, :, 3:W], op=ALU.add),
                   R=[r_A2], W=[r_B4])
            P.dve(lambda e: e.tensor_tensor(out=B8[:, 4:W - 3], in0=B4[:, 1, 2:W - 5], in1=B4[:, 1, 6:W - 1], op=ALU.add),
                  R=[r_B4], W=[r_B8])
            P.dve(lambda e: e.tensor_copy(out=S[0:64, 0, :], in_=A2[0:64, 0, 8:8 + TT]), R=[r_A2], W=[r_S])
            P.dve(lambda e: e.tensor_copy(out=S[64:128, 0, :], in_=B4[64:128, 0, 8:8 + TT]), R=[r_B4], W=[r_S])
            P.dve(lambda e: e.tensor_copy(out=S[0:64, 1, :], in_=B8[0:64, 8:8 + TT]), R=[r_B8], W=[r_S])
            P.dve(lambda e: e.tensor_tensor(out=S[64:128, 1, :], in0=B8[64:128, 4:4 + TT], in1=B8[64:128, 12:12 + TT],
                                            op=ALU.add), R=[r_B8], W=[r_S])
            P.dve(lambda e, b=b: e.tensor_tensor(out=S[:], in0=S[:], in1=IV[b][:], op=ALU.mult), R=[r_S, r_IV[b]], W=[r_S])
            P.dve(lambda e, x=x: e.tensor_tensor(out=db[:], in0=S[:], in1=x[:, :, 8:8 + TT], op=ALU.subtract),
                  R=[r_S, r_X[b]], W=[r_db])
            for j in range(2):
                P.pe(lambda e, j=j: e.matmul(ps[j][:], pwb[:, j, :], db[:, j, :], start=True, stop=True),
                     R=[r_pwb, r_db], W=[r_ps[j]])
                P.act(lambda e, j=j, b=b: e.activation(out=yo[b][:, j, :], in_=ps[j][:], func=AF.Copy,
                                                       scale=C.pv[:, l, PV_PS + j:PV_PS + j + 1]),
                      R=[r_ps[j]], W=[r_yo[b]])
            P.dma("sp", C.ymix[768:1024, g0:g0 + TT].rearrange("(c p) t -> p c t", p=128), yo[b][:], R=[r_yo[b]])
    P.end()


def ph_nat(C, l):
    P = C.P
    Tm = C.Tmax
    P.begin()
    ones_b = C.cstb[:, 384:512]
    Tq = P.sbuf("Tq", [64, 60, 64], F32); r_Tq = P.res()
    P.dma("sp", Tq[:], bass.AP(C.rpb.tensor, l * 2048, [[1, 64], [31, 60], [1, 64]]), W=[r_Tq])
    EE = P.sbuf("EE", [128, 2, 15, 64], F32); r_EE = P.res()
    ps_s = [P.psum(f"nps{i}", [128, 512]) for i in range(2)]; r_pss = [P.pres() for _ in range(2)]
    ps_n = [P.psum(f"npn{i}", [128, 512]) for i in range(2)]; r_psn = [P.pres() for _ in range(2)]
    ps_d = [P.psum(f"npd{i}", [128, 512]) for i in range(2)]; r_psd = [P.pres() for _ in range(2)]
    anti = C.cst[0:64, C_ANTI:C_ANTI + 64]
    k = 0
    for j in range(2):
        for (d0, d1) in ((0, 8), (8, 15)):
            b = k % 2
            k += 1
            for hp in range(2):
                for dr in range(d0, d1):
                    P.pe(lambda e, b=b, hp=hp, dr=dr, d0=d0, j=j: e.matmul(
                        ps_s[b][64 * hp:64 * hp + 64, (dr - d0) * 64:(dr - d0 + 1) * 64],
                        Tq[:, (2 * j + hp) * 15 + dr, :], anti, start=True, stop=True), R=[r_Tq], W=[r_pss[b]])
            n = (d1 - d0) * 64
            P.act(lambda e, b=b, j=j, d0=d0, d1=d1, n=n: e.activation(
                out=EE[:, j, d0:d1, :], in_=ps_s[b][:, 0:n].rearrange("p (a c) -> p a c", c=64), func=AF.Exp),
                R=[r_pss[b]], W=[r_EE])
    P.dve(lambda e: e.tensor_tensor(out=EE[:].rearrange("p j a c -> p (j a) c"), in0=EE[:].rearrange("p j a c -> p (j a) c"),
                                    in1=bcast(C.cst[:, C_CMASK:C_CMASK + 64].unsqueeze(1), [128, 30, 64]), op=ALU.mult),
          R=[r_EE], W=[r_EE])
    qk = P.sbuf("nqk", [128, 4, Tm], BF16); r_qk = P.res()
    vr = P.sbuf("nvr", [128, Tm // 64, 256], BF16); r_vr = P.res()
    yn = P.sbuf("nyn", [128, 2, Tm], BF16); r_yn = P.res()
    pe_ = [P.sbuf(f"npe{i}", [128, 512], F32) for i in range(2)]; r_pe = [P.res() for _ in range(2)]
    pm = [P.sbuf(f"npm{i}", [128, 512], BF16) for i in range(2)]; r_pm = [P.res() for _ in range(2)]
    rd = [P.sbuf(f"nrd{i}", [128, 64], F32) for i in range(2)]; r_rd = [P.res() for _ in range(2)]
    it = 0
    for s, T in enumerate(C.seq_lens):
        g0 = C.offs[s]
        R = T // 64
        P.dma("sp", qk[:, :, 0:T], C.ntqk[:, g0:g0 + T].rearrange("(c p) t -> p c t", p=128), R=[r_qk], W=[r_qk])
        for hp in range(2):
            P.dma("sp", vr[64 * hp:64 * hp + 64, 0:R, :], C.ntv[g0:g0 + T, :].rearrange("(r p) c -> p r c", p=64),
                  R=[r_vr], W=[r_vr])
        for r in range(R):
            rs = min(max(r - 4, 0), R - 8)
            dl = r - rs
            for j in range(2):
                b = it % 2
                it += 1
                for hp in range(2):
                    sl = slice(64 * hp, 64 * hp + 64)
                    for kr in range(8):
                        P.pe(lambda e, b=b, sl=sl, kr=kr, j=j, rs=rs, r=r: e.matmul(
                            ps_s[b][sl, kr * 64:(kr + 1) * 64], qk[sl, 2 + j, (rs + kr) * 64:(rs + kr + 1) * 64],
                            qk[sl, j, r * 64:(r + 1) * 64], start=True, stop=True), R=[r_qk], W=[r_pss[b]])
                P.act(lambda e, b=b: e.activation(out=pe_[b][:], in_=ps_s[b][:], func=AF.Exp), R=[r_pss[b]], W=[r_pe[b]])
                P.dve(lambda e, b=b, j=j, dl=dl: e.tensor_tensor(
                    out=pm[b][:].rearrange("p (a c) -> p a c", c=64), in0=pe_[b][:].rearrange("p (a c) -> p a c", c=64),
                    in1=EE[:, j, 7 - dl:15 - dl, :], op=ALU.mult), R=[r_pe[b], r_EE], W=[r_pm[b]])
                for hp in range(2):
                    sl = slice(64 * hp, 64 * hp + 64)
                    for kr in range(8):
                        P.pe(lambda e, b=b, sl=sl, kr=kr, j=j, hp=hp, rs=rs: e.matmul(
                            ps_n[b][sl, 0:64], vr[sl, rs + kr, (2 * j + hp) * 64:(2 * j + hp + 1) * 64],
                            pm[b][sl, kr * 64:(kr + 1) * 64], start=(kr == 0), stop=(kr == 7)),
                            R=[r_vr, r_pm[b]], W=[r_psn[b]])
                    for kr in range(8):
                        P.pe(lambda e, b=b, sl=sl, kr=kr: e.matmul(
                            ps_d[b][sl, 0:64], ones_b[sl, 0:64], pm[b][sl, kr * 64:(kr + 1) * 64],
                            start=(kr == 0), stop=(kr == 7)), R=[r_pm[b]], W=[r_psd[b]])
                P.dve(lambda e, b=b: e.reciprocal(out=rd[b][:], in_=ps_d[b][:, 0:64]), R=[r_psd[b]], W=[r_rd[b]])
                P.dve(lambda e, b=b, j=j, r=r: e.tensor_tensor(out=yn[:, j, r * 64:(r + 1) * 64], in0=ps_n[b][:, 0:64],
                                                              in1=rd[b][:], op=ALU.mult),
                      R=[r_psn[b], r_rd[b]], W=[r_yn])
        P.dma("sp", C.ymix[512:768, g0:g0 + T].rearrange("(c p) t -> p c t", p=128), yn[:, :, 0:T], R=[r_yn])
    P.end()


def ph_gqa(C, l):
    P = C.P
    Tm = C.Tmax
    P.begin()
    ones_b = C.cstb[:, 384:512]
    qk = P.sbuf("gqk", [128, 3, Tm], BF16); r_qk = P.res()
    v = P.sbuf("gv", [128, Tm // 128, 128], BF16); r_v = P.res()
    yg = P.sbuf("gyg", [128, 2, Tm], BF16); r_yg = P.res()
    pt = [P.sbuf(f"gpt{i}", [128, 512], BF16) for i in range(3)]; r_pt = [P.res() for _ in range(3)]
    rd = P.sbuf("grd", [128, 512], F32); r_rd = P.res()
    ps_s = [P.psum(f"gps{i}", [128, 512]) for i in range(3)]; r_pss = [P.pres() for _ in range(3)]
    ps_n = [P.psum(f"gpn{i}", [128, 512]) for i in range(2)]; r_psn = [P.pres() for _ in range(2)]
    ps_d = [P.psum(f"gpd{i}", [128, 512]) for i in range(2)]; r_psd = [P.pres() for _ in range(2)]
    it = 0
    ob = 0
    for s, T in enumerate(C.seq_lens):
        g0 = C.offs[s]
        P.dma("sp", qk[:, :, 0:T], C.gqqk[:, g0:g0 + T].rearrange("(c p) t -> p c t", p=128), R=[r_qk], W=[r_qk])
        P.dma("sp", v[:, 0:T // 128, :], C.gqv[g0:g0 + T, :].rearrange("(b p) c -> p b c", p=128), R=[r_v], W=[r_v])
        nkb = T // 128
        for j in range(2):
            for qb in range(T // 512):
                o = ob % 2
                ob += 1
                for kb in range(nkb):
                    for hp in range(2):
                        sl = slice(64 * hp, 64 * hp + 64)
                        b = it % 3
                        it += 1
                        P.pe(lambda e, b=b, sl=sl, kb=kb, j=j, qb=qb: e.matmul(
                            ps_s[b][:], qk[sl, 2, kb * 128:(kb + 1) * 128], qk[sl, j, qb * 512:(qb + 1) * 512],
                            start=True, stop=True), R=[r_qk], W=[r_pss[b]])
                        P.act(lambda e, b=b: e.activation(out=pt[b][:], in_=ps_s[b][:], func=AF.Exp),
                              R=[r_pss[b]], W=[r_pt[b]])
                        P.pe(lambda e, b=b, sl=sl, kb=kb, o=o: e.matmul(
                            ps_n[o][sl, :], v[:, kb, sl], pt[b][:], start=(kb == 0), stop=(kb == nkb - 1)),
                            R=[r_v, r_pt[b]], W=[r_psn[o]])
                        P.pe(lambda e, b=b, sl=sl, kb=kb, o=o: e.matmul(
                            ps_d[o][sl, :], ones_b[:, 0:64], pt[b][:], start=(kb == 0), stop=(kb == nkb - 1)),
                            R=[r_pt[b]], W=[r_psd[o]])
                P.dve(lambda e, o=o: e.reciprocal(out=rd[:], in_=ps_d[o][:]), R=[r_psd[o]], W=[r_rd])
                P.dve(lambda e, o=o, j=j, qb=qb: e.tensor_tensor(out=yg[:, j, qb * 512:(qb + 1) * 512], in0=ps_n[o][:],
                                                                in1=rd[:], op=ALU.mult), R=[r_psn[o], r_rd], W=[r_yg])
        P.dma("sp", C.ymix[256:512, g0:g0 + T].rearrange("(c p) t -> p c t", p=128), yg[:, :, 0:T], R=[r_yg])
    P.end()


def ph_rwkv(C, l):
    import os
    rw = int(os.environ.get("RWSTOP", "9"))
    ph_RA(C, l)
    if rw >= 2:
        ph_RS(C, l, 0)
    if rw >= 3:
        ph_RS(C, l, 1)
    if rw >= 4:
        ph_RP(C, l)


WSC = -0.6065306597126334


def ph_RA(C, l):
    P = C.P
    P.begin()
    blk_b = C.cstb[:, 128:256]
    pvl = C.pv[:, l, :]
    w2b = P.sbuf("w2b", [128, 256], BF16); a2b = P.sbuf("a2b", [128, 256], BF16); g2b = P.sbuf("g2b", [128, 256], BF16)
    r_wt = P.res()
    P.dma("pool", w2b[:], C.w2[l], W=[r_wt])
    P.dma("pool", a2b[:], C.a2[l], W=[r_wt])
    P.dma("pool", g2b[:], C.g2[l], W=[r_wt])
    sm = P.sbuf("sm", [128, 32], F32); r_sm = P.res()
    P.dve(lambda e: e.tensor_scalar(out=sm[:, 0:9], in0=pvl[:, PV_MU:PV_MU + 9], scalar1=-1.0, scalar2=1.0, op0=ALU.mult,
                                    op1=ALU.add), W=[r_sm])
    P.dve(lambda e: e.tensor_scalar(out=sm[:, 9:18], in0=pvl[:, PV_MU:PV_MU + 9], scalar1=0.5, scalar2=None, op0=ALU.mult),
          W=[r_sm])
    P.dve(lambda e: e.tensor_scalar(out=sm[:, 18:20], in0=pvl[:, PV_KA:PV_KA + 2], scalar1=-1.0, scalar2=1.0, op0=ALU.mult,
                                    op1=ALU.add), W=[r_sm])
    PA = [P.sbuf(f"PA{i}", [128, 9, TT + 2], F32) for i in range(2)]; r_PA = [P.res() for _ in range(2)]
    sft = P.sbuf("sft", [128, 9, TT], F32); r_sft = P.res()
    pp = P.sbuf("pp", [128, 9, TT], F32); r_pp = P.res()
    twl = P.sbuf("twl", [128, TT], BF16); alb = P.sbuf("alb", [128, TT], BF16); sgl = P.sbuf("sgl", [128, TT], BF16)
    r_lo = P.res()
    ST = [P.sbuf(f"ST{i}", [128, 11, 2, TT], F32) for i in range(2)]; r_ST = [P.res() for _ in range(2)]
    kks = P.sbuf("kks", [128, TT], F32); r_kks = P.res()
    sqb = P.sbuf("sqb", [128, TT], BF16); r_sqb = P.res()
    rs_ = P.sbuf("rs_", [128, TT], F32); r_rs = P.res()
    tmp = P.sbuf("tmp", [128, TT], F32); r_tmp = P.res()
    rkb = P.sbuf("rkb", [128, TT], BF16); r_rkb = P.res()
    ps = [P.psum(f"rps{i}", [128, TT]) for i in range(4)]; r_ps = [P.pres() for _ in range(4)]
    pc = [0]

    def nps():
        k = pc[0] % 4
        pc[0] += 1
        return k

    it = 0
    for s, T in enumerate(C.seq_lens):
        for i in range(T // TT):
            t0 = i * TT
            g0 = C.offs[s] + t0
            b = it % 2
            it += 1
            lo, hi = max(t0 - 1, 0), min(t0 + TT + 1, T)
            if lo > t0 - 1:
                P.pool(lambda e, b=b: e.memset(PA[b][:, :, 0:1], 0.0), W=[r_PA[b]])
            if hi < t0 + TT + 1:
                P.pool(lambda e, b=b: e.memset(PA[b][:, :, TT + 1:TT + 2], 0.0), W=[r_PA[b]])
            u0 = lo - (t0 - 1)
            P.dma("sp", PA[b][:, :, u0:u0 + hi - lo],
                  C.pa[:, C.offs[s] + lo:C.offs[s] + hi].rearrange("(c p) t -> p c t", p=128), R=[r_PA[b]], W=[r_PA[b]])
            pa_ = PA[b]
            st = ST[b]
            rst = r_ST[b]
            P.pool(lambda e, pa_=pa_: e.tensor_tensor(out=sft[:], in0=pa_[:, :, 0:TT], in1=pa_[:, :, 2:TT + 2], op=ALU.add),
                   R=[r_PA[b]], W=[r_sft])
            for c in range(9):
                P.dve(lambda e, c=c, pa_=pa_: e.tensor_scalar(out=pp[:, c, :], in0=pa_[:, c, 1:TT + 1], scalar1=sm[:, c:c + 1],
                                                             scalar2=None, op0=ALU.mult), R=[r_PA[b], r_sm], W=[r_pp])
                P.dve(lambda e, c=c: e.scalar_tensor_tensor(out=pp[:, c, :], in0=sft[:, c, :], scalar=sm[:, 9 + c:10 + c],
                                                           in1=pp[:, c, :], op0=ALU.mult, op1=ALU.add),
                      R=[r_sft, r_pp], W=[r_pp])
            P.pool(lambda e, st=st: e.tensor_copy(out=st[:, 0], in_=pp[:, 0:2, :]), R=[r_pp], W=[rst])
            P.pool(lambda e, st=st: e.tensor_copy(out=st[:, 1], in_=pp[:, 4:6, :]), R=[r_pp], W=[rst])
            P.act(lambda e: e.activation(out=twl[:], in_=pp[:, 6, :], func=AF.Tanh), R=[r_pp], W=[r_lo])
            P.act(lambda e: e.activation(out=alb[:], in_=pp[:, 7, :], func=AF.Copy), R=[r_pp], W=[r_lo])
            P.act(lambda e: e.activation(out=sgl[:], in_=pp[:, 8, :], func=AF.Sigmoid), R=[r_pp], W=[r_lo])
            for d in range(2):
                sl = slice(64 * d, 64 * d + 64)
                for c in range(2):
                    k = nps()
                    P.pe(lambda e, k=k, sl=sl, c=c: e.matmul(ps[k][:], w2b[sl, c * 128:(c + 1) * 128], twl[sl, :],
                                                             start=True, stop=True), R=[r_wt, r_lo], W=[r_ps[k]])
                    P.act(lambda e, k=k, d=d, c=c, st=st: e.activation(
                        out=st[:, 7 + d, c, :], in_=ps[k][:], func=AF.Sigmoid,
                        bias=pvl[:, PV_W0 + d * 2 + c:PV_W0 + d * 2 + c + 1]), R=[r_ps[k]], W=[rst])
                    k = nps()
                    P.pe(lambda e, k=k, sl=sl, c=c: e.matmul(ps[k][:], a2b[sl, c * 128:(c + 1) * 128], alb[sl, :],
                                                             start=True, stop=True), R=[r_wt, r_lo], W=[r_ps[k]])
                    P.act(lambda e, k=k, d=d, c=c, st=st: e.activation(
                        out=st[:, 5 + d, c, :], in_=ps[k][:], func=AF.Sigmoid,
                        bias=pvl[:, PV_A0 + d * 2 + c:PV_A0 + d * 2 + c + 1]), R=[r_ps[k]], W=[rst])
            for c in range(2):
                P.dve(lambda e, c=c: e.tensor_scalar(out=kks[:], in0=pp[:, 2 + c, :], scalar1=pvl[:, PV_KK + c:PV_KK + c + 1],
                                                    scalar2=None, op0=ALU.mult), R=[r_pp], W=[r_kks])
                P.act(lambda e: e.activation(out=sqb[:], in_=kks[:], func=AF.Square), R=[r_kks], W=[r_sqb])
                k = nps()
                P.pe(lambda e, k=k: e.matmul(ps[k][:], blk_b, sqb[:], start=True, stop=True), R=[r_sqb], W=[r_ps[k]])
                P.act(lambda e, k=k: e.activation(out=rs_[:], in_=ps[k][:], func=AF.Sqrt), R=[r_ps[k]], W=[r_rs])
                P.dve(lambda e: e.tensor_scalar(out=rs_[:], in0=rs_[:], scalar1=1e-12, scalar2=None, op0=ALU.max),
                      R=[r_rs], W=[r_rs])
                P.dve(lambda e: e.reciprocal(out=rs_[:], in_=rs_[:]), R=[r_rs], W=[r_rs])
                P.dve(lambda e, c=c, st=st: e.tensor_tensor(out=st[:, 2, c, :], in0=kks[:], in1=rs_[:], op=ALU.mult),
                      R=[r_kks, r_rs], W=[rst])
                for d in range(2):
                    P.dve(lambda e, c=c, d=d, st=st: e.tensor_scalar(
                        out=tmp[:], in0=st[:, 5 + d, c, :], scalar1=pvl[:, PV_KA + c:PV_KA + c + 1],
                        scalar2=sm[:, 18 + c:19 + c], op0=ALU.mult, op1=ALU.add), R=[rst, r_sm], W=[r_tmp])
                    P.dve(lambda e, c=c, d=d, st=st: e.tensor_tensor(out=st[:, 3 + d, c, :], in0=tmp[:], in1=pp[:, 2 + c, :],
                                                                    op=ALU.mult), R=[r_tmp, r_pp], W=[rst])
                P.pool(lambda e, c=c, st=st: e.tensor_tensor(out=tmp[:], in0=st[:, 3, c, :], in1=st[:, 4, c, :], op=ALU.add),
                       R=[rst], W=[r_tmp])
                P.dve(lambda e, c=c: e.scalar_tensor_tensor(out=rkb[:], in0=tmp[:], scalar=pvl[:, PV_RK + c:PV_RK + c + 1],
                                                           in1=pp[:, c, :], op0=ALU.mult, op1=ALU.mult),
                      R=[r_tmp, r_pp], W=[r_rkb])
                k = nps()
                P.pe(lambda e, k=k: e.matmul(ps[k][:], blk_b, rkb[:], start=True, stop=True), R=[r_rkb], W=[r_ps[k]])
                P.dve(lambda e, k=k, c=c: e.tensor_tensor(out=tmp[:], in0=ps[k][:], in1=pp[:, 4 + c, :], op=ALU.mult),
                      R=[r_ps[k], r_pp, r_rkb], W=[r_tmp])
                k = nps()
                P.pe(lambda e, k=k, c=c: e.matmul(ps[k][:], g2b[:, c * 128:(c + 1) * 128], sgl[:], start=True, stop=True),
                     R=[r_wt, r_lo], W=[r_ps[k]])
                P.act(lambda e, k=k, c=c, st=st: e.activation(out=st[:, 9, c, :], in_=ps[k][:], func=AF.Copy,
                                                              scale=pvl[:, PV_LNW + c:PV_LNW + c + 1]),
                      R=[r_ps[k]], W=[rst])
                P.dve(lambda e, k=k, c=c, st=st: e.scalar_tensor_tensor(
                    out=st[:, 10, c, :], in0=tmp[:], scalar=pvl[:, PV_LNB + c:PV_LNB + c + 1], in1=ps[k][:],
                    op0=ALU.add, op1=ALU.mult), R=[r_tmp, r_ps[k]], W=[rst])
            for a in range(9):
                P.dma("sp", C.rkv[a, :, g0:g0 + TT].rearrange("(c p) t -> p c t", p=128), st[:, a], R=[rst])
            for a in range(2):
                P.dma("sp", C.g23[a, :, g0:g0 + TT].rearrange("(c p) t -> p c t", p=128), st[:, 9 + a], R=[rst])
    P.end()
RT = 256


def ph_RS(C, l, d):
    P = C.P
    P.begin()
    NCH = RT // 64
    ident64 = C.cst[0:64, C_ID:C_ID + 64]
    mk = C.cst[0:64, (C_MU if d == 0 else C_ML):(C_MU if d == 0 else C_ML) + 128]
    mkT = C.cst[0:64, (C_ML if d == 0 else C_MU):(C_ML if d == 0 else C_MU) + 64]
    rstm = C.cst[0:64, C_RST:C_RST + RT]
    IN = [P.sbuf(f"IN{i}", [64, 6, 4, RT], F32) for i in range(2)]; r_IN = [P.res() for _ in range(2)]
    mkt = lambda n: P.sbuf(n, [64, 4, RT], F32)
    Lg, pre, cum, exl, rem = mkt("Lg"), mkt("pre"), mkt("cum"), mkt("exl"), mkt("rem")
    e_in, e_ex, e_ng, e_rm, kb = mkt("e_in"), mkt("e_ex"), mkt("e_ng"), mkt("e_rm"), mkt("kb")
    gC = P.sbuf("gC", [64, 4, NCH], F32)
    r_el = P.res()
    AR = P.sbuf("AR", [64, 4, NCH, 128], F32); r_AR = P.res()
    BbT, KbT = mkt("BbT"), mkt("KbT"); r_bk = P.res()
    Btl, Ktl = mkt("Btl"), mkt("Ktl"); r_tl = P.res()
    dG = P.sbuf("dG", [64, 4, NCH, 64], F32); r_dG = P.res()
    NB = NCH * 4
    MS = P.sbuf("MS", [64, NB, 256], F32); r_MS = [P.res() for _ in range(NCH)]
    XP = P.sbuf("XP", [64, NB, 128], F32); r_XP = [P.res() for _ in range(NCH)]
    PT = P.sbuf("PT", [64, NB, 64], F32); r_PT = [P.res() for _ in range(NCH)]
    TK = P.sbuf("TK", [64, NCH, 3, 256], F32); r_TK = [P.res() for _ in range(NCH)]
    S = P.sbuf("S", [64, 4, 64], F32); r_S = P.res()
    Wt = P.sbuf("Wt", [64, 4, 64], F32); r_Wt = P.res()
    Ut = P.sbuf("Ut", [64, 4, 64], F32); r_Ut = P.res()
    Yt = [P.sbuf(f"Yt{i}", [64, NCH, 256], F32) for i in range(2)]; r_Yt = [P.res() for _ in range(2)]
    pA = [P.psum(f"spA{i}", [128, 512]) for i in range(2)]; r_pA = [P.pres() for _ in range(2)]
    pB = [P.psum(f"spB{i}", [128, 512]) for i in range(2)]; r_pB = [P.pres() for _ in range(2)]
    pW = P.psum("spW", [128, 512]); r_pW = P.pres()
    pU = P.psum("spU", [128, 512]); r_pU = P.pres()
    pY = P.psum("spY", [128, 512]); r_pY = P.pres()
    pS = P.psum("spS", [128, 512]); r_pS = P.pres()
    arr_idx = (0, 1, 2, 3 + d, 5 + d, 7 + d)
    c4 = lambda t: t.rearrange("p h (c t) -> p h c t", t=64)
    it = 0
    for s, T in enumerate(C.seq_lens):
        ntile = T // RT
        order = list(range(ntile)) if d == 0 else list(range(ntile - 1, -1, -1))
        P.dve(lambda e: e.memset(S[:], 0.0), R=[r_S], W=[r_S])

        def load(ti, b):
            g0 = C.offs[s] + ti * RT
            for a, ai in enumerate(arr_idx):
                P.dma("sp", IN[b][:, a], C.rkv[ai, :, g0:g0 + RT].rearrange("(h k) t -> k h t", k=64),
                      R=[r_IN[b]], W=[r_IN[b]])

        load(order[0], it % 2)
        for oi, ti in enumerate(order):
            b = it % 2
            it += 1
            if oi + 1 < len(order):
                load(order[oi + 1], (b + 1) % 2)
            g0 = C.offs[s] + ti * RT
            X = IN[b]
            rin = r_IN[b]
            r_, v_, kk_, kd_, a_, sg_ = (X[:, a] for a in range(6))
            P.dve(lambda e, sg_=sg_: e.tensor_scalar(out=Lg[:], in0=sg_, scalar1=WSC, scalar2=None, op0=ALU.mult),
                  R=[rin, r_el], W=[r_el])
            for h in range(4):
                P.dve(lambda e, h=h: e.tensor_tensor_scan(out=pre[:, h, :], data0=rstm, data1=Lg[:, h, :], initial=0.0,
                                                          op0=ALU.mult, op1=ALU.add), R=[r_el], W=[r_el])
            tot_b = bcast(c4(pre[:])[:, :, :, 63:64], [64, 4, NCH, 64])
            if d == 0:
                P.pool(lambda e: e.tensor_copy(out=cum[:], in_=pre[:]), R=[r_el], W=[r_el])
                P.dve(lambda e: e.tensor_tensor(out=exl[:], in0=pre[:], in1=Lg[:], op=ALU.subtract), R=[r_el], W=[r_el])
            else:
                P.dve(lambda e: e.tensor_tensor(out=c4(exl[:]), in0=tot_b, in1=c4(pre[:]), op=ALU.subtract),
                      R=[r_el], W=[r_el])
                P.dve(lambda e: e.tensor_tensor(out=cum[:], in0=exl[:], in1=Lg[:], op=ALU.add), R=[r_el], W=[r_el])
            P.dve(lambda e: e.tensor_tensor(out=c4(rem[:]), in0=tot_b, in1=c4(cum[:]), op=ALU.subtract), R=[r_el], W=[r_el])
            P.act(lambda e: e.activation(out=e_in[:], in_=cum[:], func=AF.Exp), R=[r_el], W=[r_el])
            P.act(lambda e: e.activation(out=e_ex[:], in_=exl[:], func=AF.Exp), R=[r_el], W=[r_el])
            P.act(lambda e: e.activation(out=e_ng[:], in_=cum[:], func=AF.Exp, scale=-1.0), R=[r_el], W=[r_el])
            P.act(lambda e: e.activation(out=e_rm[:], in_=rem[:], func=AF.Exp), R=[r_el], W=[r_el])
            P.act(lambda e: e.activation(out=gC[:], in_=c4(pre[:])[:, :, :, 63], func=AF.Exp), R=[r_el], W=[r_el])
            P.dve(lambda e, kk_=kk_: e.scalar_tensor_tensor(out=AR[:, :, :, 0:64], in0=c4(kk_), scalar=-1.0, in1=c4(e_ex[:]),
                                                           op0=ALU.mult, op1=ALU.mult), R=[rin, r_el, r_AR], W=[r_AR])
            P.dve(lambda e, r_=r_: e.tensor_tensor(out=AR[:, :, :, 64:128], in0=c4(r_), in1=c4(e_in[:]), op=ALU.mult),
                  R=[rin, r_el, r_AR], W=[r_AR])
            P.pool(lambda e, kk_=kk_, a_=a_: e.tensor_tensor(out=kb[:], in0=kk_, in1=a_, op=ALU.mult), R=[rin, r_el], W=[r_el])
            P.dve(lambda e: e.tensor_tensor(out=BbT[:], in0=kb[:], in1=e_ng[:], op=ALU.mult), R=[r_el, r_bk], W=[r_bk])
            P.dve(lambda e, kd_=kd_: e.tensor_tensor(out=KbT[:], in0=kd_, in1=e_ng[:], op=ALU.mult), R=[rin, r_el, r_bk], W=[r_bk])
            P.pool(lambda e: e.tensor_tensor(out=Btl[:], in0=kb[:], in1=e_rm[:], op=ALU.mult), R=[r_el, r_tl], W=[r_tl])
            P.pool(lambda e, kd_=kd_: e.tensor_tensor(out=Ktl[:], in0=kd_, in1=e_rm[:], op=ALU.mult), R=[rin, r_el, r_tl], W=[r_tl])
            P.dve(lambda e: e.tensor_tensor(out=dG[:], in0=bcast(gC[:].unsqueeze(3), [64, 4, NCH, 64]),
                                            in1=bcast(ident64.unsqueeze(1).unsqueeze(1), [64, 4, NCH, 64]), op=ALU.mult),
                  R=[r_el, r_dG], W=[r_dG])
            for c in range(NCH):
                q = c % 2
                csl = slice(c * 64, (c + 1) * 64)
                for h in range(4):
                    bank, off = h // 2, (h % 2) * 256
                    P.pe(lambda e, h=h, c=c, csl=csl, bank=bank, off=off: e.matmul(
                        pA[bank][0:64, off:off + 128], BbT[:, h, csl], AR[:, h, c, :], start=True, stop=True),
                        R=[r_bk, r_AR], W=[r_pA[bank]])
                    P.pe(lambda e, h=h, c=c, csl=csl, bank=bank, off=off: e.matmul(
                        pA[bank][0:64, off + 128:off + 256], KbT[:, h, csl], AR[:, h, c, :], start=True, stop=True),
                        R=[r_bk, r_AR], W=[r_pA[bank]])
                    P.pe(lambda e, h=h, c=c, csl=csl, q=q: e.matmul(
                        pB[q][0:64, h * 64:(h + 1) * 64], AR[:, h, c, 0:64], BbT[:, h, csl], start=True, stop=True),
                        R=[r_bk, r_AR], W=[r_pB[q]])
                for bank in range(2):
                    P.dve(lambda e, bank=bank, c=c: e.tensor_tensor(
                        out=MS[:, c * 4 + bank * 2:c * 4 + bank * 2 + 2, :].rearrange("p b (m t) -> p b m t", t=128),
                        in0=pA[bank][0:64, :].rearrange("p (b m t) -> p b m t", b=2, t=128),
                        in1=bcast(mk.unsqueeze(1).unsqueeze(1), [64, 2, 2, 128]), op=ALU.mult),
                        R=[r_pA[bank], r_MS[c]], W=[r_MS[c]])
                bs = slice(c * 4, c * 4 + 4)
                P.dve(lambda e, bs=bs, q=q: e.tensor_tensor(out=PT[:, bs, :],
                                                            in0=pB[q][0:64, 0:256].rearrange("p (b t) -> p b t", t=64),
                                                            in1=bcast(mkT.unsqueeze(1), [64, 4, 64]), op=ALU.mult),
                      R=[r_pB[q], r_PT[c]], W=[r_PT[c]])
                P.dve(lambda e, bs=bs: e.tensor_tensor(out=XP[:, bs, 0:64], in0=MS[:, bs, 0:64],
                                                       in1=bcast(ident64.unsqueeze(1), [64, 4, 64]), op=ALU.add),
                      R=[r_MS[c], r_XP[c]], W=[r_XP[c]])
                P.pool(lambda e, bs=bs: e.tensor_copy(out=XP[:, bs, 64:128], in_=MS[:, bs, 0:64]), R=[r_MS[c], r_XP[c]],
                       W=[r_XP[c]])
                for lev in range(6):
                    a_bank = pA[q]
                    for bi in range(4):
                        blk = c * 4 + bi
                        if lev == 0:
                            P.pe(lambda e, blk=blk, bi=bi, a_bank=a_bank: e.matmul(
                                a_bank[0:64, bi * 128 + 64:bi * 128 + 128], PT[:, blk, :], XP[:, blk, 64:128],
                                start=True, stop=True), R=[r_PT[c], r_XP[c]], W=[r_pA[q]])
                        elif lev < 5:
                            P.pe(lambda e, blk=blk, bi=bi, a_bank=a_bank: e.matmul(
                                a_bank[0:64, bi * 128:bi * 128 + 128], PT[:, blk, :], XP[:, blk, :],
                                start=True, stop=True), R=[r_PT[c], r_XP[c]], W=[r_pA[q]])
                        else:
                            P.pe(lambda e, blk=blk, bi=bi, a_bank=a_bank: e.matmul(
                                a_bank[0:64, bi * 128:bi * 128 + 64], PT[:, blk, :], XP[:, blk, 0:64],
                                start=True, stop=True), R=[r_PT[c], r_XP[c]], W=[r_pA[q]])
                        if lev < 5:
                            P.pe(lambda e, blk=blk, bi=bi, q=q: e.matmul(
                                pB[q][0:64, bi * 64:(bi + 1) * 64], XP[:, blk, 64:128], PT[:, blk, :],
                                start=True, stop=True), R=[r_PT[c], r_XP[c]], W=[r_pB[q]])
                    av = a_bank[0:64, :].rearrange("p (b m t) -> p b m t", b=4, t=64)
                    if lev > 0:
                        P.dve(lambda e, bs=bs, av=av: e.tensor_tensor(out=XP[:, bs, 0:64], in0=XP[:, bs, 0:64],
                                                                      in1=av[:, :, 0, :], op=ALU.add),
                              R=[r_pA[q], r_XP[c]], W=[r_XP[c]])
                    if lev < 5:
                        P.dve(lambda e, bs=bs, av=av: e.tensor_copy(out=XP[:, bs, 64:128], in_=av[:, :, 1, :]),
                              R=[r_pA[q], r_XP[c]], W=[r_XP[c]])
                        P.act(lambda e, bs=bs, q=q: e.activation(out=PT[:, bs, :],
                                                                 in_=pB[q][0:64, 0:256].rearrange("p (b t) -> p b t", t=64),
                                                                 func=AF.Copy), R=[r_pB[q], r_PT[c]], W=[r_PT[c]])
                srcs = (v_, Btl[:], Ktl[:])
                srs = (rin, r_tl, r_tl)
                for kind in range(3):
                    bank = pA[q] if kind < 2 else pB[q]
                    rb = r_pA[q] if kind < 2 else r_pB[q]
                    for h in range(4):
                        o0 = (kind % 2) * 256 + h * 64
                        P.pe(lambda e, kind=kind, h=h, csl=csl, bank=bank, o0=o0, srcs=srcs: e.transpose(
                            bank[0:64, o0:o0 + 64], srcs[kind][:, h, csl], ident64), R=[srs[kind]], W=[rb])
                P.dve(lambda e, c=c, q=q: e.tensor_copy(out=TK[:, c, 0:2, :], in_=pA[q][0:64, :].rearrange("p (k f) -> p k f", f=256)),
                      R=[r_pA[q], r_TK[c]], W=[r_TK[c]])
                P.act(lambda e, c=c, q=q: e.activation(out=TK[:, c, 2, :], in_=pB[q][0:64, 0:256], func=AF.Copy),
                      R=[r_pB[q], r_TK[c]], W=[r_TK[c]])
            yb = it % 2
            corder = list(range(NCH)) if d == 0 else list(range(NCH - 1, -1, -1))
            for c in corder:
                rr = [r_MS[c], r_XP[c], r_TK[c]]
                for h in range(4):
                    blk = c * 4 + h
                    hs = slice(h * 64, (h + 1) * 64)
                    P.pe(lambda e, h=h, c=c, hs=hs: e.matmul(pW[0:64, hs], AR[:, h, c, 0:64], S[:, h, :],
                                                           start=True, stop=False), R=[r_AR, r_S], W=[r_pW])
                    P.pe(lambda e, blk=blk, c=c, hs=hs: e.matmul(pW[0:64, hs], MS[:, blk, 128:192], TK[:, c, 0, hs],
                                                                start=False, stop=True), R=rr, W=[r_pW])
                P.act(lambda e: e.activation(out=Wt[:], in_=pW[0:64, 0:256].rearrange("p (h v) -> p h v", v=64), func=AF.Copy),
                      R=[r_pW, r_Wt], W=[r_Wt])
                for h in range(4):
                    blk = c * 4 + h
                    hs = slice(h * 64, (h + 1) * 64)
                    P.pe(lambda e, blk=blk, h=h, hs=hs: e.matmul(pU[0:64, hs], XP[:, blk, 0:64], Wt[:, h, :],
                                                                start=True, stop=True), R=rr + [r_Wt], W=[r_pU])
                P.dve(lambda e: e.tensor_copy(out=Ut[:], in_=pU[0:64, 0:256].rearrange("p (h v) -> p h v", v=64)),
                      R=[r_pU, r_Ut], W=[r_Ut])
                for h in range(4):
                    blk = c * 4 + h
                    hs = slice(h * 64, (h + 1) * 64)
                    P.pe(lambda e, h=h, c=c, hs=hs: e.matmul(pY[0:64, hs], AR[:, h, c, 64:128], S[:, h, :],
                                                           start=True, stop=False), R=[r_AR, r_S], W=[r_pY])
                    P.pe(lambda e, blk=blk, h=h, hs=hs: e.matmul(pY[0:64, hs], MS[:, blk, 64:128], Ut[:, h, :],
                                                                start=False, stop=False), R=rr + [r_Ut], W=[r_pY])
                    P.pe(lambda e, blk=blk, c=c, hs=hs: e.matmul(pY[0:64, hs], MS[:, blk, 192:256], TK[:, c, 0, hs],
                                                                start=False, stop=True), R=rr, W=[r_pY])
                P.act(lambda e, c=c, yb=yb: e.activation(out=Yt[yb][:, c, :], in_=pY[0:64, 0:256], func=AF.Copy),
                      R=[r_pY, r_Yt[yb]], W=[r_Yt[yb]])
                for h in range(4):
                    hs = slice(h * 64, (h + 1) * 64)
                    P.pe(lambda e, c=c, hs=hs, h=h: e.matmul(pS[0:64, hs], TK[:, c, 1, hs], Ut[:, h, :],
                                                           start=True, stop=False), R=rr + [r_Ut], W=[r_pS])
                    P.pe(lambda e, c=c, hs=hs: e.matmul(pS[0:64, hs], TK[:, c, 2, hs], TK[:, c, 0, hs],
                                                       start=False, stop=False), R=rr, W=[r_pS])
                    P.pe(lambda e, h=h, c=c, hs=hs: e.matmul(pS[0:64, hs], dG[:, h, c, :], S[:, h, :],
                                                           start=False, stop=True), R=[r_dG, r_S], W=[r_pS])
                P.dve(lambda e: e.tensor_copy(out=S[:], in_=pS[0:64, 0:256].rearrange("p (h v) -> p h v", v=64)),
                      R=[r_pS, r_S], W=[r_S])
            P.dma("sp", C.yscan[d, g0:g0 + RT, :].rearrange("(c p) f -> p c f", p=64), Yt[yb][:], R=[r_Yt[yb]])
    P.end()


def ph_RP(C, l):
    P = C.P
    P.begin()
    ident_f = C.cst[:, C_ID:C_ID + 128]
    gne = C.cst[:, C_EPS + 1:C_EPS + 2]
    y0 = [P.sbuf(f"y0{i}", [128, 4, 256], F32) for i in range(2)]
    y1 = [P.sbuf(f"y1{i}", [128, 4, 256], F32) for i in range(2)]
    G = [P.sbuf(f"G{i}", [128, 2, 2, TT], F32) for i in range(2)]
    r_in = [P.res() for _ in range(2)]
    sqv = P.sbuf("sqv", [128, 16, 64], F32)
    mean = P.sbuf("mean", [128, 16], F32)
    var = P.sbuf("var", [128, 16], F32)
    r_w = P.res()
    yo = [P.sbuf(f"ryo{i}", [128, 2, TT], BF16) for i in range(2)]; r_yo = [P.res() for _ in range(2)]
    tmpf = P.sbuf("tmpf", [128, 2, TT], F32); r_tf = P.res()
    ps = [P.psum(f"pps{i}", [128, TT]) for i in range(2)]; r_ps = [P.pres() for _ in range(2)]
    it = 0
    for s, T in enumerate(C.seq_lens):
        for i in range(T // TT):
            g0 = C.offs[s] + i * TT
            b = it % 2
            it += 1
            ya, yb_, Gb = y0[b], y1[b], G[b]
            P.dma("sp", ya[:], C.yscan[0, g0:g0 + TT, :].rearrange("(b p) f -> p b f", p=128), R=[r_in[b]], W=[r_in[b]])
            P.dma("sp", yb_[:], C.yscan[1, g0:g0 + TT, :].rearrange("(b p) f -> p b f", p=128), R=[r_in[b]], W=[r_in[b]])
            for a in range(2):
                P.dma("sp", Gb[:, a], C.g23[a, :, g0:g0 + TT].rearrange("(c p) t -> p c t", p=128), R=[r_in[b]], W=[r_in[b]])
            yv = ya[:].rearrange("p b (h k) -> p (b h) k", k=64)
            P.dve(lambda e, ya=ya, yb_=yb_: e.tensor_tensor(out=ya[:], in0=ya[:], in1=yb_[:], op=ALU.add), R=[r_in[b]], W=[r_in[b]])
            P.dve(lambda e, yv=yv: e.tensor_reduce(out=mean[:], in_=yv, axis=AX.X, op=ALU.add), R=[r_in[b], r_w], W=[r_w])
            P.dve(lambda e: e.tensor_scalar(out=mean[:], in0=mean[:], scalar1=1.0 / 64, scalar2=None, op0=ALU.mult), R=[r_w], W=[r_w])
            P.dve(lambda e, yv=yv: e.tensor_tensor(out=yv, in0=yv, in1=bcast(mean[:].unsqueeze(2), [128, 16, 64]), op=ALU.subtract),
                  R=[r_in[b], r_w], W=[r_in[b]])
            P.pool(lambda e, yv=yv: e.tensor_tensor(out=sqv[:], in0=yv, in1=yv, op=ALU.mult), R=[r_in[b], r_w], W=[r_w])
            P.dve(lambda e: e.tensor_reduce(out=var[:], in_=sqv[:], axis=AX.X, op=ALU.add), R=[r_w], W=[r_w])
            P.act(lambda e: e.activation(out=var[:], in_=var[:], func=AF.Sqrt, bias=gne, scale=1.0 / 64), R=[r_w], W=[r_w])
            P.dve(lambda e: e.reciprocal(out=var[:], in_=var[:]), R=[r_w], W=[r_w])
            P.dve(lambda e, yv=yv: e.tensor_tensor(out=yv, in0=yv, in1=bcast(var[:].unsqueeze(2), [128, 16, 64]), op=ALU.mult),
                  R=[r_in[b], r_w], W=[r_in[b]])
            for j in range(2):
                for tb in range(4):
                    P.pe(lambda e, j=j, tb=tb, ya=ya: e.transpose(ps[j][:, tb * 128:(tb + 1) * 128], ya[:, tb, j * 128:(j + 1) * 128],
                                                                 ident_f), R=[r_in[b]], W=[r_ps[j]])
                P.dve(lambda e, j=j, Gb=Gb: e.tensor_tensor(out=tmpf[:, j, :], in0=ps[j][:], in1=Gb[:, 0, j, :], op=ALU.mult),
                      R=[r_ps[j], r_in[b], r_tf], W=[r_tf])
                P.pool(lambda e, j=j, Gb=Gb, b=b: e.tensor_tensor(out=yo[b][:, j, :], in0=tmpf[:, j, :], in1=Gb[:, 1, j, :], op=ALU.add),
                       R=[r_tf, r_in[b], r_yo[b]], W=[r_yo[b]])
            P.dma("sp", C.ymix[0:256, g0:g0 + TT].rearrange("(c p) t -> p c t", p=128), yo[b][:], R=[r_yo[b]])
    P.end()


def ph_C(C, l):
    ph_C1(C, l)
    ph_C2(C, l)


def ph_C1(C, l):
    P = C.P
    is_moe = (l % 2 == 1)
    X = C.xT if l == 0 else C.yT
    P.begin()
    ones_b = C.cstb[:, 384:512]
    eps_ap = C.cst[:, C_EPS:C_EPS + 1]
    wo = P.sbuf("wo", [128, 8, D], BF16); r_wo = P.res()
    P.dma("pool", wo[:], C.w_out[l].rearrange("(c p) n -> p c n", p=128), W=[r_wo])
    if is_moe:
        rw = P.sbuf("rw", [128, 8, NEXP], F32); r_rw = P.res()
        P.dma("sp", rw[:], C.router[l // 2].rearrange("(c p) e -> p c e", p=128), W=[r_rw])
    xt = [P.sbuf(f"cx{i}", [128, 8, TT], F32) for i in range(2)]; r_xt = [P.res() for _ in range(2)]
    ym = [P.sbuf(f"cym{i}", [128, 8, TT], BF16) for i in range(2)]; r_ym = [P.res() for _ in range(2)]
    sq = P.sbuf("csq", [128, 8, TT], BF16); r_sq = P.res()
    rt = P.sbuf("crt", [128, TT], F32); r_rt = P.res()
    hf = P.sbuf("chf", [128, 8, TT], F32); r_hf = P.res()
    hb = [P.sbuf(f"chb{i}", [128, 8, TT], BF16) for i in range(2)]; r_hb = [P.res() for _ in range(2)]
    ps_o = [P.psum(f"cps{i}", [128, TT]) for i in range(4)]; r_pso = [P.pres() for _ in range(4)]
    ps_ss = P.psum("cpss", [128, TT]); r_pss = P.pres()
    if is_moe:
        ps_r = P.psum("cpsr", [128, 4, NEXP]); r_psr = P.pres()
        lg = P.sbuf("lg", [128, 4, NEXP], F32); r_lg = P.res()
        lg2 = P.sbuf("lg2", [128, 4, NEXP], F32)
        eq1 = P.sbuf("eq1", [128, 4, NEXP], F32)
        eq2 = P.sbuf("eq2", [128, 4, NEXP], F32)
        m1 = P.sbuf("m1", [128, 4], F32)
        m2 = P.sbuf("m2", [128, 4], F32)
        g1 = P.sbuf("g1", [128, 4], F32)
        g2 = P.sbuf("g2", [128, 4], F32)
        cmb = [P.sbuf(f"cmb{i}", [128, 4, NEXP], F32) for i in range(2)]; r_cmb = [P.res() for _ in range(2)]
    tiles = []
    for s, T in enumerate(C.seq_lens):
        for i in range(T // TT):
            tiles.append((s, i * TT, C.offs[s] + i * TT))

    def load(ti):
        s, t0, g0 = tiles[ti]
        b = ti % 2
        P.dma("sp", xt[b][:], X[:, g0:g0 + TT].rearrange("(c p) t -> p c t", p=128), R=[r_xt[b]], W=[r_xt[b]])
        P.dma("sp", ym[b][:], C.ymix[:, g0:g0 + TT].rearrange("(c p) t -> p c t", p=128), R=[r_ym[b]], W=[r_ym[b]])

    pi_ = [0]
    load(0)
    for ti in range(len(tiles)):
        if ti + 1 < len(tiles):
            load(ti + 1)
        s, t0, g0 = tiles[ti]
        b = ti % 2
        for oc in range(8):
            pi = pi_[0] % 4
            pi_[0] += 1
            for kc in range(8):
                P.pe(lambda e, pi=pi, kc=kc, oc=oc, b=b: e.matmul(ps_o[pi][:], wo[:, kc, oc * 128:(oc + 1) * 128],
                                                             ym[b][:, kc, :], start=(kc == 0), stop=(kc == 7)),
                     R=[r_wo, r_ym[b]], W=[r_pso[pi]])
            P.dve(lambda e, pi=pi, oc=oc, b=b, s=s: e.scalar_tensor_tensor(
                out=xt[b][:, oc, :], in0=ps_o[pi][:], scalar=C.modT[:, l, 2 * 8 + oc, s:s + 1], in1=xt[b][:, oc, :],
                op0=ALU.mult, op1=ALU.add), R=[r_pso[pi], r_xt[b]], W=[r_xt[b]])
        P.dma("sp", C.yT[:, g0:g0 + TT].rearrange("(c p) t -> p c t", p=128), xt[b][:], R=[r_xt[b]])
        P.act(lambda e, b=b: e.activation(out=sq[:], in_=xt[b][:], func=AF.Square), R=[r_xt[b]], W=[r_sq])
        for c in range(8):
            P.pe(lambda e, c=c: e.matmul(ps_ss[:], ones_b, sq[:, c, :], start=(c == 0), stop=(c == 7)),
                 R=[r_sq], W=[r_pss])
        P.act(lambda e: e.activation(out=rt[:], in_=ps_ss[:], func=AF.Sqrt, bias=eps_ap, scale=1.0 / D),
              R=[r_pss], W=[r_rt])
        P.dve(lambda e: e.reciprocal(out=rt[:], in_=rt[:]), R=[r_rt], W=[r_rt])
        P.dve(lambda e, b=b: e.tensor_tensor(out=hf[:], in0=xt[b][:], in1=bcast(rt[:].unsqueeze(1), [128, 8, TT]),
                                             op=ALU.mult), R=[r_xt[b], r_rt], W=[r_hf])
        for c in range(8):
            if c % 2 == 0:
                P.act(lambda e, c=c, s=s: e.activation(out=hf[:, c, :], in_=hf[:, c, :], func=AF.Identity,
                                                       bias=C.modT[:, l, 3 * 8 + c, s:s + 1],
                                                       scale=C.modA[:, l, 1, c, s:s + 1]), R=[r_hf], W=[r_hf])
            else:
                P.dve(lambda e, c=c, s=s: e.tensor_scalar(out=hf[:, c, :], in0=hf[:, c, :],
                                                          scalar1=C.modA[:, l, 1, c, s:s + 1],
                                                          scalar2=C.modT[:, l, 3 * 8 + c, s:s + 1],
                                                          op0=ALU.mult, op1=ALU.add), R=[r_hf], W=[r_hf])
        P.pool(lambda e, b=b: e.tensor_copy(out=hb[b][:], in_=hf[:]), R=[r_hf], W=[r_hb[b]])
        P.dma("sp", C.h2s[:, g0:g0 + TT].rearrange("(c p) t -> p c t", p=128), hb[b][:], R=[r_hb[b]])
        if is_moe:
            for tb in range(4):
                for kc in range(8):
                    P.pe(lambda e, tb=tb, kc=kc: e.matmul(ps_r[:, tb, :], hf[:, kc, tb * 128:(tb + 1) * 128], rw[:, kc, :],
                                                          start=(kc == 0), stop=(kc == 7)), R=[r_hf, r_rw], W=[r_psr])
            cb = cmb[b]
            sh = [128, 4, NEXP]
            P.dve(lambda e: e.tensor_copy(out=lg[:], in_=ps_r[:]), R=[r_psr, r_lg], W=[r_lg])
            P.dve(lambda e: e.tensor_reduce(out=m1[:], in_=lg[:], axis=AX.X, op=ALU.max), R=[r_lg], W=[r_lg])
            P.dve(lambda e: e.tensor_tensor(out=eq1[:], in0=lg[:], in1=bcast(m1[:].unsqueeze(2), sh), op=ALU.is_equal),
                  R=[r_lg], W=[r_lg])
            P.dve(lambda e: e.scalar_tensor_tensor(out=lg2[:], in0=eq1[:], scalar=-1e30, in1=lg[:], op0=ALU.mult,
                                                   op1=ALU.add), R=[r_lg], W=[r_lg])
            P.dve(lambda e: e.tensor_reduce(out=m2[:], in_=lg2[:], axis=AX.X, op=ALU.max), R=[r_lg], W=[r_lg])
            P.dve(lambda e: e.tensor_tensor(out=eq2[:], in0=lg2[:], in1=bcast(m2[:].unsqueeze(2), sh), op=ALU.is_equal),
                  R=[r_lg], W=[r_lg])
            P.dve(lambda e: e.tensor_tensor(out=g1[:], in0=m1[:], in1=m2[:], op=ALU.subtract), R=[r_lg], W=[r_lg])
            P.act(lambda e: e.activation(out=g1[:], in_=g1[:], func=AF.Sigmoid), R=[r_lg], W=[r_lg])
            P.dve(lambda e: e.tensor_scalar(out=g2[:], in0=g1[:], scalar1=-1.0, scalar2=1.0, op0=ALU.mult, op1=ALU.add),
                  R=[r_lg], W=[r_lg])
            P.dve(lambda e: e.tensor_tensor(out=eq1[:], in0=eq1[:], in1=bcast(g1[:].unsqueeze(2), sh), op=ALU.mult),
                  R=[r_lg], W=[r_lg])
            P.dve(lambda e: e.tensor_tensor(out=eq2[:], in0=eq2[:], in1=bcast(g2[:].unsqueeze(2), sh), op=ALU.mult),
                  R=[r_lg], W=[r_lg])
            P.dve(lambda e, cb=cb: e.tensor_tensor(out=cb[:], in0=eq1[:], in1=eq2[:], op=ALU.add),
                  R=[r_lg, r_cmb[b]], W=[r_cmb[b]])
            P.dma("sp", C.comb[g0:g0 + TT, :].rearrange("(b p) e -> p b e", p=128), cb[:], R=[r_cmb[b]])
    P.end()


def ph_C2(C, l):
    P = C.P
    is_moe = (l % 2 == 1)
    last = (l == C.L - 1)
    li = l // 2
    G = min(2048, min(C.seq_lens))
    NSUB = G // TT
    P.begin()
    ones_b = C.cstb[:, 384:512]
    eps_ap = C.cst[:, C_EPS:C_EPS + 1]
    acc = P.sbuf("acc", [128, 8, G], F32); r_acc = P.res()
    h2 = P.sbuf("h2", [128, 8, G], BF16); r_h2 = P.res()
    wg = [P.sbuf(f"wg{i}", [128, 8, 512], BF16) for i in range(2)]
    wu = [P.sbuf(f"wu{i}", [128, 8, 512], BF16) for i in range(2)]
    wd = [P.sbuf(f"wd{i}", [128, 4, D], BF16) for i in range(2)]
    r_w = [P.res() for _ in range(2)]
    actb = [P.sbuf(f"actb{i}", [128, 4, TT], BF16) for i in range(2)]; r_act = [P.res() for _ in range(2)]
    sg = [P.sbuf(f"sg{i}", [128, TT], F32) for i in range(2)]; r_sg = [P.res() for _ in range(2)]
    tt_ = [P.sbuf(f"tt{i}", [128, TT], F32) for i in range(2)]; r_tt = [P.res() for _ in range(2)]
    ps_g = [P.psum(f"fpg{i}", [128, TT]) for i in range(2)]; r_pg = [P.pres() for _ in range(2)]
    ps_u = [P.psum(f"fpu{i}", [128, TT]) for i in range(2)]; r_pu = [P.pres() for _ in range(2)]
    ps_d = [P.psum(f"fpd{i}", [128, TT]) for i in range(2)]; r_pd = [P.pres() for _ in range(2)]
    ps_x = P.psum("fpx", [128, TT]); r_px = P.pres()
    if is_moe:
        cmt = P.sbuf("cmt", [128, G // 128, NEXP], F32); r_cmt = P.res()
        cwb = P.sbuf("cwb", [128, G // 128, 128], F32); r_cwb = P.res()
        cw = P.sbuf("cw", [128, NSUB, TT], F32); r_cw = P.res()
    if last:
        sq = P.sbuf("fsq", [128, 8, TT], BF16); r_sq = P.res()
        rt = P.sbuf("frt", [128, TT], F32); r_rt = P.res()
    ident_f = C.cst[:, C_ID:C_ID + 128]
    dff = DFF_E if is_moe else DFF_D
    nch = dff // 128
    fgs = [(c0, min(4, nch - c0)) for c0 in range(0, nch, 4)]
    wi_ = [0]
    ai_ = [0]
    for s, T in enumerate(C.seq_lens):
        for gi in range(T // G):
            g0 = C.offs[s] + gi * G
            P.dma("sp", acc[:], C.yT[:, g0:g0 + G].rearrange("(c p) t -> p c t", p=128), R=[r_acc], W=[r_acc])
            P.dma("sp", h2[:], C.h2s[:, g0:g0 + G].rearrange("(c p) t -> p c t", p=128), R=[r_h2], W=[r_h2])
            if is_moe:
                P.dma("sp", cmt[:], C.comb[g0:g0 + G, :].rearrange("(b p) e -> p b e", p=128), R=[r_cmt], W=[r_cmt])
            for ex in range(NEXP if is_moe else 1):
                if is_moe:
                    P.dve(lambda e, ex=ex: e.tensor_copy(out=cwb[:], in_=bcast(cmt[:, :, ex:ex + 1], [128, G // 128, 128])),
                          R=[r_cmt, r_cwb], W=[r_cwb])
                    for sub in range(NSUB):
                        for tb in range(4):
                            P.pe(lambda e, sub=sub, tb=tb: e.matmul(ps_x[:, tb * 128:(tb + 1) * 128], cwb[:, sub * 4 + tb, :],
                                                                   ident_f, start=True, stop=True), R=[r_cwb], W=[r_px])
                        P.act(lambda e, sub=sub: e.activation(out=cw[:, sub, :], in_=ps_x[:], func=AF.Copy),
                              R=[r_px, r_cw], W=[r_cw])
                    Wg, Wu, Wd = C.moe_g[li, ex], C.moe_u[li, ex], C.moe_d[li, ex]
                else:
                    Wg, Wu, Wd = C.ffn_g[li], C.ffn_u[li], C.ffn_d[li]
                for (c0, nc4) in fgs:
                    wb = wi_[0] % 2
                    wi_[0] += 1
                    f0, fw = c0 * 128, nc4 * 128
                    P.dma("pool", wg[wb][:, :, 0:fw], Wg[:, f0:f0 + fw].rearrange("(c p) f -> p c f", p=128),
                          R=[r_w[wb]], W=[r_w[wb]])
                    P.dma("pool", wu[wb][:, :, 0:fw], Wu[:, f0:f0 + fw].rearrange("(c p) f -> p c f", p=128),
                          R=[r_w[wb]], W=[r_w[wb]])
                    P.dma("pool", wd[wb][:, 0:nc4, :], Wd[f0:f0 + fw, :].rearrange("(c p) n -> p c n", p=128),
                          R=[r_w[wb]], W=[r_w[wb]])
                    for sub in range(NSUB):
                        ab = ai_[0] % 2
                        ai_[0] += 1
                        tsl = slice(sub * TT, (sub + 1) * TT)
                        for c4 in range(nc4):
                            pb = c4 % 2
                            for kc in range(8):
                                P.pe(lambda e, pb=pb, wb=wb, kc=kc, c4=c4, tsl=tsl: e.matmul(
                                    ps_g[pb][:], wg[wb][:, kc, c4 * 128:(c4 + 1) * 128], h2[:, kc, tsl],
                                    start=(kc == 0), stop=(kc == 7)), R=[r_w[wb], r_h2], W=[r_pg[pb]])
                            for kc in range(8):
                                P.pe(lambda e, pb=pb, wb=wb, kc=kc, c4=c4, tsl=tsl: e.matmul(
                                    ps_u[pb][:], wu[wb][:, kc, c4 * 128:(c4 + 1) * 128], h2[:, kc, tsl],
                                    start=(kc == 0), stop=(kc == 7)), R=[r_w[wb], r_h2], W=[r_pu[pb]])
                            P.act(lambda e, pb=pb: e.activation(out=sg[pb][:], in_=ps_g[pb][:], func=AF.Silu),
                                  R=[r_pg[pb]], W=[r_sg[pb]])
                            if is_moe:
                                P.dve(lambda e, pb=pb: e.tensor_tensor(out=tt_[pb][:], in0=sg[pb][:], in1=ps_u[pb][:],
                                                                       op=ALU.mult), R=[r_sg[pb], r_pu[pb]], W=[r_tt[pb]])
                                P.pool(lambda e, pb=pb, ab=ab, c4=c4, sub=sub: e.tensor_tensor(
                                    out=actb[ab][:, c4, :], in0=tt_[pb][:], in1=cw[:, sub, :], op=ALU.mult),
                                    R=[r_tt[pb], r_cw], W=[r_act[ab]])
                            else:
                                P.dve(lambda e, pb=pb, ab=ab, c4=c4: e.tensor_tensor(
                                    out=actb[ab][:, c4, :], in0=sg[pb][:], in1=ps_u[pb][:], op=ALU.mult),
                                    R=[r_sg[pb], r_pu[pb]], W=[r_act[ab]])
                        for oc in range(8):
                            db_ = oc % 2
                            for c4 in range(nc4):
                                P.pe(lambda e, db_=db_, wb=wb, c4=c4, oc=oc, ab=ab, nc4=nc4: e.matmul(
                                    ps_d[db_][:], wd[wb][:, c4, oc * 128:(oc + 1) * 128], actb[ab][:, c4, :],
                                    start=(c4 == 0), stop=(c4 == nc4 - 1)), R=[r_w[wb], r_act[ab]], W=[r_pd[db_]])
                            P.dve(lambda e, db_=db_, oc=oc, tsl=tsl, s=s: e.scalar_tensor_tensor(
                                out=acc[:, oc, tsl], in0=ps_d[db_][:], scalar=C.modT[:, l, 5 * 8 + oc, s:s + 1],
                                in1=acc[:, oc, tsl], op0=ALU.mult, op1=ALU.add), R=[r_pd[db_], r_acc], W=[r_acc])
            if last:
                for sub in range(NSUB):
                    tsl = slice(sub * TT, (sub + 1) * TT)
                    P.act(lambda e, tsl=tsl: e.activation(out=sq[:], in_=acc[:, :, tsl], func=AF.Square),
                          R=[r_acc, r_sq], W=[r_sq])
                    for c in range(8):
                        P.pe(lambda e, c=c: e.matmul(ps_x[:], ones_b, sq[:, c, :], start=(c == 0), stop=(c == 7)),
                             R=[r_sq], W=[r_px])
                    P.act(lambda e: e.activation(out=rt[:], in_=ps_x[:], func=AF.Sqrt, bias=eps_ap, scale=1.0 / D),
                          R=[r_px, r_rt], W=[r_rt])
                    P.dve(lambda e: e.reciprocal(out=rt[:], in_=rt[:]), R=[r_rt], W=[r_rt])
                    P.dve(lambda e, tsl=tsl: e.tensor_tensor(out=acc[:, :, tsl], in0=acc[:, :, tsl],
                                                             in1=bcast(rt[:].unsqueeze(1), [128, 8, TT]), op=ALU.mult),
                          R=[r_acc, r_rt], W=[r_acc])
                P.dve(lambda e: e.tensor_tensor(out=acc[:], in0=acc[:],
                                                in1=bcast(C.cst[:, C_GFIN:C_GFIN + 8].unsqueeze(2), [128, 8, G]),
                                                op=ALU.mult), R=[r_acc], W=[r_acc])
            P.dma("sp", C.yT[:, g0:g0 + G].rearrange("(c p) t -> p c t", p=128), acc[:], R=[r_acc], W=[r_acc])
    P.end()


SEQ_LENS = [4096, 2048, 2048, 2048, 2048]
DEPTH = 4
N_CORES = 8


def kernel(**inputs):
    inp = {k: np.asarray(v) for k, v in inputs.items()}
    xp, xs_, cp, cs_ = inp["x_prompt"], inp["x_sample"], inp["c_prompt"], inp["c_sample"]
    w = pack_weights(inp, DEPTH, SEQ_LENS)
    nc = build_program(SEQ_LENS, DEPTH)
    in_maps = []
    for core in range(N_CORES):
        xs = [xp[core]] + [xs_[4 * core + i] for i in range(4)]
        cc = np.stack([cp[core]] + [cs_[4 * core + i] for i in range(4)], axis=0).astype(np.float32)
        m = dict(w)
        m["xT"] = np.ascontiguousarray(np.concatenate([np.asarray(a, np.float32).T for a in xs], axis=1))
        m["cT"] = np.ascontiguousarray(cc.T.reshape(8, 128, len(xs)).transpose(1, 0, 2))
        in_maps.append(m)
    res = run_bass_kernel_spmd(nc, in_maps, core_ids=list(range(N_CORES)))
    y_prompt = np.empty(xp.shape, np.float32)
    y_sample = np.empty(xs_.shape, np.float32)
    for core in range(N_CORES):
        yT = np.asarray(res.results[core]["yT"])
        y_prompt[core] = yT[:, 0:4096].T
        for i in range(4):
            o = 4096 + 2048 * i
            y_sample[4 * core + i] = yT[:, o:o + 2048].T
    return (y_prompt, y_sample)
```

```python
import contextlib
import numpy as np
import concourse.bass as bass
import concourse.mybir as mybir

F32 = mybir.dt.float32
BF16 = mybir.dt.bfloat16
I32 = mybir.dt.int32
AF = mybir.ActivationFunctionType
ALU = mybir.AluOpType
AX = mybir.AxisListType

ENGS = ("pe", "dve", "act", "pool", "sp")
MAXC = 30000
DMA_SLOTS = 8
MAXD_USES = 1800


class Res:
    __slots__ = ("name", "writers", "readers", "excl")

    def __init__(self, name="", excl=False):
        self.name = name
        self.writers = {}
        self.readers = {}
        self.excl = excl


class Op:
    __slots__ = ("eng", "idx", "fn", "deps", "is_dma", "signal", "sig", "slot", "use")

    def __init__(self, eng, fn, is_dma):
        self.eng = eng
        self.fn = fn
        self.is_dma = is_dma
        self.deps = {}
        self.signal = False
        self.sig = None
        self.slot = None
        self.use = None


class Prog:
    def __init__(self, nc, same_engine_sync=True):
        self.nc = nc
        self.same_engine_sync = same_engine_sync
        self.gstack = contextlib.ExitStack()
        self.pstack = None
        self.ops = None
        self.ndma = {e: 0 for e in ENGS}
        self.ccount = {e: 0 for e in ENGS}
        self.sems = {}
        self.all_res = []
        self.pending_barrier = {}
        self.n_inst = 0
        self.n_wait = 0
        self.eng_obj = {"pe": nc.tensor, "dve": nc.vector, "act": nc.scalar,
                        "pool": nc.gpsimd, "sp": nc.sync}

    def gsbuf(self, name, shape, dtype):
        return self.gstack.enter_context(self.nc.sbuf_tensor(name, list(shape), dtype))

    def sbuf(self, name, shape, dtype):
        return self.pstack.enter_context(self.nc.sbuf_tensor(f"{name}_p{self.phase_id}", list(shape), dtype))

    def psum(self, name, shape, dtype=F32):
        return self.pstack.enter_context(self.nc.psum_tensor(f"{name}_p{self.phase_id}", list(shape), dtype))

    def res(self, name="", excl=False):
        r = Res(name, excl)
        self.all_res.append(r)
        return r

    def pres(self, name=""):
        return self.res(name, True)

    def get_sem(self, key):
        if key not in self.sems:
            nm = "s_" + "_".join(str(x) for x in key)
            self.sems[key] = self.gstack.enter_context(self.nc.semaphore(nm))
        return self.sems[key]

    def begin(self):
        assert self.ops is None
        self.phase_id = getattr(self, "phase_id", 0) + 1
        self.pstack = contextlib.ExitStack()
        self.ops = {e: [] for e in ENGS}

    def _key(self, op):
        if op.is_dma:
            return ("dma", op.eng, op.slot)
        return ("c", op.eng)

    def _record(self, op, reads, writes):
        deps = op.deps

        def add(p):
            k = self._key(p)
            q = deps.get(k)
            if q is None or p.idx > q.idx:
                deps[k] = p

        for r in reads:
            for p in r.writers.values():
                add(p)
            if r.excl:
                for p in r.readers.values():
                    if p.eng != op.eng:
                        add(p)
        for w in writes:
            for p in w.writers.values():
                add(p)
            for p in w.readers.values():
                add(p)
        for k in list(deps.keys()):
            p = deps[k]
            if p is op:
                del deps[k]
                continue
            if (not p.is_dma) and p.eng == op.eng and not op.is_dma:
                if (not self.same_engine_sync) or op.eng == "pe":
                    del deps[k]
                    continue
            p.signal = True
        me = self._key(op)
        for r in reads:
            r.readers[me] = op
        for w in writes:
            w.writers = {me: op}
            w.readers = {}

    def op(self, eng, fn, R=(), W=()):
        o = Op(eng, fn, False)
        o.idx = len(self.ops[eng])
        self.ops[eng].append(o)
        self._record(o, R, W)
        return o

    def dma(self, q, out, in_, R=(), W=(), **kw):
        o = Op(q, (lambda e: e.dma_start(out=out, in_=in_, **kw)), True)
        o.idx = len(self.ops[q])
        n = self.ndma[q]
        self.ndma[q] = n + 1
        o.slot = n % DMA_SLOTS
        o.use = n // DMA_SLOTS
        o.signal = True
        self.ops[q].append(o)
        self._record(o, R, W)
        return o

    def pe(self, fn, R=(), W=()):
        return self.op("pe", fn, R, W)

    def dve(self, fn, R=(), W=()):
        return self.op("dve", fn, R, W)

    def act(self, fn, R=(), W=()):
        return self.op("act", fn, R, W)

    def pool(self, fn, R=(), W=()):
        return self.op("pool", fn, R, W)

    def end(self, final=False):
        nc = self.nc
        bar = {}
        for e in ENGS:
            last = None
            for o in self.ops[e]:
                if not o.is_dma:
                    last = o
            if last is not None:
                last.signal = True
                bar[e] = last
        for e in ENGS:
            for o in self.ops[e]:
                if o.is_dma:
                    ep = o.use // MAXD_USES
                    u = o.use % MAXD_USES
                    o.sig = (("d", e, o.slot, ep), 16 * (u + 1), 16)
                elif o.signal:
                    cnt = self.ccount[e]
                    self.ccount[e] = cnt + 1
                    o.sig = (("c", e, cnt // MAXC), cnt % MAXC + 1, 1)
                if o.sig is not None:
                    self.get_sem(o.sig[0])
        new_barrier = {}
        for e in ENGS:
            if e in bar:
                new_barrier[bar[e].sig[0]] = bar[e].sig[1]
            for o in self.ops[e]:
                if o.is_dma:
                    k, v, _ = o.sig
                    if new_barrier.get(k, 0) < v:
                        new_barrier[k] = v
        start_waits = dict(self.pending_barrier)
        sems = self.sems
        ops = self.ops
        stats = self

        with nc.Block() as block:
            def run(e):
                def body(eng):
                    waited = {}
                    for (k, v) in start_waits.items():
                        if k[0] == "c" and k[1] == e:
                            continue
                        eng.wait_ge(sems[k], v)
                        waited[k] = v
                        stats.n_wait += 1
                    for o in ops[e]:
                        waits = []
                        for p in o.deps.values():
                            waits.append((p.sig[0], p.sig[1]))
                        if o.is_dma:
                            k = o.sig[0]
                            if o.sig[1] > 16:
                                waits.append((k, o.sig[1] - 16))
                            elif k[3] > 0:
                                waits.append(((k[0], k[1], k[2], k[3] - 1), 16 * MAXD_USES))
                        for (k, v) in waits:
                            if waited.get(k, 0) >= v:
                                continue
                            waited[k] = v
                            eng.wait_ge(sems[k], v)
                            stats.n_wait += 1
                        inst = o.fn(eng)
                        stats.n_inst += 1
                        if o.sig is not None:
                            inst.then_inc(sems[o.sig[0]], o.sig[2])
                    if final:
                        for (k, v) in new_barrier.items():
                            if waited.get(k, 0) >= v:
                                continue
                            if k[0] == "c" and k[1] == e:
                                continue
                            eng.wait_ge(sems[k], v)
                return body
            block.tensor(run("pe"))
            block.vector(run("dve"))
            block.scalar(run("act"))
            block.gpsimd(run("pool"))
            block.sync(run("sp"))
        self.pending_barrier = new_barrier
        for r in self.all_res:
            r.writers = {}
            r.readers = {}
        self.ops = None
        self.pstack.close()
        self.pstack = None

    def finish(self):
        self.gstack.close()


from concourse.bass_utils import run_bass_kernel_spmd

D = 1024
IN_COLS = 2688
NCH_IN = 21
DFF_D = 2816
DFF_E = 3584
NEXP = 8
EPS = 1e-6
GN_EPS = 64e-5
TT = 512
PV_GMIX, PV_GFFN, PV_BADA, PV_MU, PV_W0, PV_A0 = 0, 8, 16, 64, 73, 77
PV_KK, PV_KA, PV_RK, PV_LNW, PV_LNB, PV_QN, PV_KN, PV_PS = 81, 83, 85, 87, 89, 91, 92, 93
PV_N = 95
C_ID, C_BLK, C_SWAP, C_ANTI, C_CMASK, C_EXPO, C_EPS, C_MU, C_ML, C_GFIN = 0, 128, 256, 384, 448, 512, 513, 516, 644, 772
C_RST, C_IP = 780, 1036
NCONST = 1100


def bcast(ap, shape):
    return ap.to_broadcast(list(shape))


class Ctx:
    pass


def build_program(seq_lens, depth, dbg=()):
    nc = bass.Bass("TRN2", target_bir_lowering=False)
    P = Prog(nc)
    C = Ctx()
    C.nc, C.P = nc, P
    C.seq_lens = list(seq_lens)
    C.NS = len(seq_lens)
    C.NT = sum(seq_lens)
    C.offs = [sum(seq_lens[:i]) for i in range(len(seq_lens))]
    C.L = depth
    C.dbg = set(dbg)
    NT, NS, L = C.NT, C.NS, C.L
    Tmax = max(seq_lens)
    C.Tmax = Tmax
    n_dense = (depth + 1) // 2
    n_moe = depth // 2

    def din(name, shape, dt=F32):
        return nc.dram_tensor(name, list(shape), dt, kind="ExternalInput").ap()

    def dscr(name, shape, dt=F32):
        kind = "ExternalOutput" if name in C.dbg else "Internal"
        return nc.dram_tensor(name, list(shape), dt, kind=kind).ap()

    C.xT = din("xT", [D, NT])
    C.cT = din("cT", [128, 8, NS])
    C.w_ada = din("w_ada", [L, D, 6 * D])
    C.pvec = din("pvec", [L, 128, PV_N])
    C.w_in = din("w_in", [L, D, IN_COLS])
    C.w_out = din("w_out", [L, D, D])
    C.w2 = din("rwkv_w2", [L, 128, 256])
    C.a2 = din("rwkv_a2", [L, 128, 256])
    C.g2 = din("rwkv_g2", [L, 128, 256])
    C.rpb = din("rpb_pad", [L, 2048])
    C.pool_w = din("pool_w", [L, 4, 64, 64])
    C.full = "stopS" not in C.dbg and "stopA" not in C.dbg and "stopM" not in C.dbg
    if C.full:
        C.ffn_g = din("ffn_w_gate", [n_dense, D, DFF_D])
        C.ffn_u = din("ffn_w_up", [n_dense, D, DFF_D])
        C.ffn_d = din("ffn_w_down", [n_dense, DFF_D, D])
        if n_moe:
            C.router = din("moe_router", [n_moe, D, NEXP])
            C.moe_g = din("moe_w_gate", [n_moe, NEXP, D, DFF_E])
            C.moe_u = din("moe_w_up", [n_moe, NEXP, D, DFF_E])
            C.moe_d = din("moe_w_down", [n_moe, NEXP, DFF_E, D])
    C.consts = din("consts", [128, NCONST])
    C.Tsel = sorted(set(seq_lens), reverse=True)
    C.invc = din("invc", [len(C.Tsel), 128, 2, 4096])
    C.yT = nc.dram_tensor("yT", [D, NT], F32, kind="ExternalOutput").ap()

    C.pa = dscr("s_pa", [1152, NT])
    C.gqqk = dscr("s_gqqk", [384, NT], BF16)
    C.gqv = dscr("s_gqv", [NT, 128], BF16)
    C.ntqk = dscr("s_ntqk", [512, NT], BF16)
    C.ntv = dscr("s_ntv", [NT, 256], BF16)
    C.pd = dscr("s_pd", [256, NT])
    C.ymix = dscr("s_ymix", [D, NT], BF16)
    C.cs = din("cs_tab", [2, 128, 4096])
    C.g23 = dscr("s_g23", [2, 256, NT])
    C.yscan = dscr("s_yscan", [2, NT, 256])
    C.rkv = dscr("s_rkv", [10, 256, NT])
    C.h2s = dscr("s_h2", [D, NT], BF16)
    C.comb = dscr("s_comb", [NT, NEXP])
    C.dscr = dscr

    C.cst = P.gsbuf("cst", [128, NCONST], F32)
    C.cstb = P.gsbuf("cstb", [128, 512], BF16)
    C.pv = P.gsbuf("pv", [128, L, PV_N], F32)
    C.modT = P.gsbuf("modT", [128, L, 48, NS], F32)
    C.modA = P.gsbuf("modA", [128, L, 2, 8, NS], F32)
    C.qkg = P.gsbuf("qkg", [128, L, 2], F32)
    C.rnull = P.res("null")

    ph_setup(C)
    for l in range(L):
        if "stopS" in C.dbg:
            break
        ph_A(C, l)
        if "stopA" in C.dbg:
            break
        ph_pool(C, l)
        ph_nat(C, l)
        ph_gqa(C, l)
        ph_rwkv(C, l)
        if "stopM" in C.dbg:
            break
        ph_C(C, l)
    P.begin()
    P.end(final=True)
    P.finish()
    return nc


def ph_setup(C):
    P, nc = C.P, C.nc
    L, NS = C.L, C.NS
    rn = C.rnull
    P.begin()
    r_c = P.res()
    P.dma("sp", C.cst[:], C.consts, W=[r_c])
    r_cb = P.res()
    P.dma("pool", C.cstb[:, 0:384], C.consts[:, 0:384], W=[r_cb])
    P.pool(lambda e: e.memset(C.cstb[:, 384:512], 1.0), W=[r_cb])
    r_pv = P.res()
    P.dma("sp", C.pv[:], C.pvec.rearrange("l p n -> p l n"), W=[r_pv])
    P.dve(lambda e: e.tensor_scalar(out=C.qkg[:, :, 0:1], in0=C.pv[:, :, PV_QN:PV_QN + 1], scalar1=0.125,
                                    scalar2=None, op0=ALU.mult), R=[r_pv], W=[rn])
    P.dve(lambda e: e.tensor_copy(out=C.qkg[:, :, 1:2], in_=C.pv[:, :, PV_KN:PV_KN + 1]), R=[r_pv], W=[rn])
    C.r_csd = [P.res(), P.res()]
    ct = P.sbuf("ct", [128, 8, NS], F32)
    r_ct = P.res()
    P.dma("sp", ct[:], C.cT, W=[r_ct])
    sc = P.sbuf("sc", [128, 8, NS], BF16)
    r_sc = P.res()
    P.act(lambda e: e.activation(out=sc[:], in_=ct[:], func=AF.Silu), R=[r_ct], W=[r_sc])
    wts = [P.sbuf(f"wada{i}", [128, 8, 768], BF16) for i in range(2)]
    r_w = [P.res(), P.res()]
    pss = [P.psum(f"psada{i}", [128, 6, NS], F32) for i in range(2)]
    r_ps = [P.res(), P.res()]
    it = 0
    for l in range(L):
        for j in range(8):
            b = it % 2
            it += 1
            wt, ps = wts[b], pss[b]
            P.dma("pool", wt[:], C.w_ada[l, :, j * 768:(j + 1) * 768].rearrange("(c p) f -> p c f", p=128),
                  W=[r_w[b]])
            for f in range(6):
                for kc in range(8):
                    P.pe(lambda e, ps=ps, wt=wt, f=f, kc=kc: e.matmul(
                        ps[:, f, :], wt[:, kc, f * 128:(f + 1) * 128], sc[:, kc, :], start=(kc == 0), stop=(kc == 7)),
                        R=[r_w[b], r_sc], W=[r_ps[b]])
            P.dve(lambda e, ps=ps, l=l, j=j: e.tensor_tensor(
                out=C.modT[:, l, j * 6:(j + 1) * 6, :], in0=ps[:],
                in1=bcast(C.pv[:, l, PV_BADA + j * 6:PV_BADA + (j + 1) * 6].unsqueeze(2), [128, 6, NS]), op=ALU.add),
                R=[r_ps[b], r_pv], W=[rn])
    for l in range(L):
        for i, (gcol, sidx) in enumerate(((PV_GMIX, 1), (PV_GFFN, 4))):
            P.dve(lambda e, l=l, i=i, gcol=gcol, sidx=sidx: e.scalar_tensor_tensor(
                out=C.modA[:, l, i], in0=C.modT[:, l, sidx * 8:(sidx + 1) * 8, :], scalar=1.0,
                in1=bcast(C.pv[:, l, gcol:gcol + 8].unsqueeze(2), [128, 8, NS]), op0=ALU.add, op1=ALU.mult),
                R=[rn, r_pv], W=[rn])
    P.end()


def ph_A(C, l):
    P = C.P
    NT = C.NT
    rn = C.rnull
    X = C.xT if l == 0 else C.yT
    P.begin()
    wi = P.sbuf("wi", [128, 8, IN_COLS], BF16)
    r_wi = [P.res() for _ in range(8)]
    for kc in range(8):
        P.dma("pool", wi[:, kc, :], C.w_in[l, kc * 128:(kc + 1) * 128, :], W=[r_wi[kc]])
    xt = [P.sbuf(f"xt{i}", [128, 8, TT], F32) for i in range(2)]
    r_xt = [P.res() for _ in range(2)]
    hb = [P.sbuf(f"hb{i}", [128, 8, TT], BF16) for i in range(2)]
    r_hb = [P.res() for _ in range(2)]
    sq = P.sbuf("sq", [128, 8, TT], BF16)
    r_sq = P.res()
    rt = P.sbuf("rt", [128, TT], F32)
    r_rt = P.res()
    cst = [P.sbuf(f"cosb{i}", [128, 2, TT], F32) for i in range(2)]
    r_cst = [P.res() for _ in range(2)]
    st_pa = [P.sbuf(f"st_pa{i}", [128, 9, TT], F32) for i in range(2)]
    st_gq = [P.sbuf(f"st_gq{i}", [128, 3, TT], BF16) for i in range(2)]
    st_gv = [P.sbuf(f"st_gv{i}", [128, 4, 128], BF16) for i in range(2)]
    st_nq = [P.sbuf(f"st_nq{i}", [128, 4, TT], BF16) for i in range(2)]
    st_nv = [P.sbuf(f"st_nv{i}", [128, 4, 256], BF16) for i in range(2)]
    st_pd = [P.sbuf(f"st_pd{i}", [128, 2, TT], F32) for i in range(2)]
    r_st = [[P.res() for _ in range(6)] for _ in range(2)]
    sq2 = P.sbuf("sq2", [128, TT], BF16); r_sq2 = P.res()
    qf = P.sbuf("qf", [128, TT], F32); r_qf = P.res()
    rq = P.sbuf("rq", [128, TT], F32); r_rq = P.res()
    qnb = P.sbuf("qnb", [128, TT], BF16); r_qnb = P.res()
    t1 = P.sbuf("t1", [128, TT], F32); r_t1 = P.res()
    t2 = P.sbuf("t2", [128, TT], F32); r_t2 = P.res()
    ps_ss = P.psum("ps_ss", [128, TT]); r_pss = P.res()
    ps_o = [P.psum(f"ps_o{i}", [128, TT]) for i in range(4)]
    r_pso = [P.res() for _ in range(4)]
    ps_n = P.psum("ps_n", [128, TT]); r_psn = P.res()
    ps_w = P.psum("ps_w", [128, TT]); r_psw = P.res()
    ones_b = C.cstb[:, 384:512]
    blk_b = C.cstb[:, 128:256]
    swap_b = C.cstb[:, 256:384]
    eps_ap = C.cst[:, C_EPS:C_EPS + 1]

    tiles = []
    for s, T in enumerate(C.seq_lens):
        for i in range(T // TT):
            tiles.append((s, i * TT, C.offs[s] + i * TT))

    def prep(ti):
        s, t0, g0 = tiles[ti]
        b = ti % 2
        P.dma("sp", xt[b][:], X[:, g0:g0 + TT].rearrange("(c p) t -> p c t", p=128), W=[r_xt[b]])
        P.dma("sp", cst[b][:, 0, :], C.cs[0, :, t0:t0 + TT], R=[C.r_csd[0]], W=[r_cst[b]])
        P.dma("sp", cst[b][:, 1, :], C.cs[1, :, t0:t0 + TT], R=[C.r_csd[1]], W=[r_cst[b]])
        P.act(lambda e: e.activation(out=sq[:], in_=xt[b][:], func=AF.Square), R=[r_xt[b]], W=[r_sq])
        for c in range(8):
            P.pe(lambda e, c=c: e.matmul(ps_ss[:], ones_b, sq[:, c, :], start=(c == 0), stop=(c == 7)),
                 R=[r_sq], W=[r_pss])
        P.act(lambda e: e.activation(out=rt[:], in_=ps_ss[:], func=AF.Sqrt, bias=eps_ap, scale=1.0 / D),
              R=[r_pss], W=[r_rt])
        P.dve(lambda e: e.reciprocal(out=rt[:], in_=rt[:]), R=[r_rt], W=[r_rt])
        P.dve(lambda e: e.tensor_tensor(out=xt[b][:], in0=xt[b][:], in1=bcast(rt[:].unsqueeze(1), [128, 8, TT]),
                                        op=ALU.mult), R=[r_xt[b], r_rt], W=[r_xt[b]])
        for c in range(8):
            if c % 2 == 0:
                P.act(lambda e, c=c: e.activation(out=hb[b][:, c, :], in_=xt[b][:, c, :], func=AF.Identity,
                                                  bias=C.modT[:, l, 0 * 8 + c, s:s + 1],
                                                  scale=C.modA[:, l, 0, c, s:s + 1]), R=[r_xt[b]], W=[r_hb[b]])
            else:
                P.dve(lambda e, c=c: e.tensor_scalar(out=hb[b][:, c, :], in0=xt[b][:, c, :],
                                                     scalar1=C.modA[:, l, 0, c, s:s + 1],
                                                     scalar2=C.modT[:, l, 0 * 8 + c, s:s + 1],
                                                     op0=ALU.mult, op1=ALU.add), R=[r_xt[b]], W=[r_hb[b]])

    state = {"pi": 0}

    def mm_fm(ch, b):
        pi = state["pi"] % 4
        state["pi"] += 1
        for kc in range(8):
            P.pe(lambda e, kc=kc: e.matmul(ps_o[pi][:], wi[:, kc, ch * 128:(ch + 1) * 128], hb[b][:, kc, :],
                                           start=(kc == 0), stop=(kc == 7)),
                 R=[r_wi[kc], r_hb[b]], W=[r_pso[pi]])
        return pi

    import os
    ASTOP = 99

    def main(ti):
        s, t0, g0 = tiles[ti]
        b = ti % 2
        rs = r_st[b]
        if ASTOP < 2:
            return
        for ch in range(9):
            pi = mm_fm(ch, b)
            if ch % 2 == 0:
                P.act(lambda e, pi=pi, ch=ch: e.activation(out=st_pa[b][:, ch, :], in_=ps_o[pi][:], func=AF.Copy),
                      R=[r_pso[pi]], W=[rs[0]])
            else:
                P.dve(lambda e, pi=pi, ch=ch: e.tensor_copy(out=st_pa[b][:, ch, :], in_=ps_o[pi][:]),
                      R=[r_pso[pi]], W=[rs[0]])
        P.dma("sp", C.pa[:, g0:g0 + TT].rearrange("(c p) t -> p c t", p=128), st_pa[b][:], R=[rs[0]])
        if ASTOP < 3:
            return
        GS = 99
        for j in range(3):
            ch = 9 + j
            pi = mm_fm(ch, b)
            gi = 0 if j < 2 else 1
            P.dve(lambda e, pi=pi: e.tensor_copy(out=qf[:], in_=ps_o[pi][:]), R=[r_pso[pi]], W=[r_qf])
            P.act(lambda e: e.activation(out=sq2[:], in_=qf[:], func=AF.Square), R=[r_qf], W=[r_sq2])
            if GS < 1: continue
            P.pe(lambda e: e.matmul(ps_n[:], blk_b, sq2[:], start=True, stop=True), R=[r_sq2], W=[r_psn])
            P.act(lambda e: e.activation(out=rq[:], in_=ps_n[:], func=AF.Sqrt, bias=eps_ap, scale=1.0 / 64),
                  R=[r_psn], W=[r_rq])
            P.dve(lambda e: e.reciprocal(out=rq[:], in_=rq[:]), R=[r_rq], W=[r_rq])
            if GS < 2: continue
            P.dve(lambda e, gi=gi: e.scalar_tensor_tensor(out=qnb[:], in0=qf[:], scalar=C.qkg[:, l, gi:gi + 1],
                                                          in1=rq[:], op0=ALU.mult, op1=ALU.mult),
                  R=[r_qf, r_rq], W=[r_qnb])
            if GS < 3: continue
            P.pe(lambda e: e.matmul(ps_w[:], swap_b, qnb[:], start=True, stop=True), R=[r_qnb], W=[r_psw])
            P.dve(lambda e: e.tensor_tensor(out=t1[:], in0=qnb[:], in1=cst[b][:, 0, :], op=ALU.mult),
                  R=[r_qnb, r_cst[b]], W=[r_t1])
            if GS < 4: continue
            P.dve(lambda e: e.tensor_tensor(out=t2[:], in0=ps_w[:], in1=cst[b][:, 1, :], op=ALU.mult),
                  R=[r_psw, r_cst[b]], W=[r_t2])
            if GS < 5: continue
            P.dve(lambda e, j=j: e.tensor_tensor(out=st_gq[b][:, j, :], in0=t1[:], in1=t2[:], op=ALU.add),
                  R=[r_t1, r_t2], W=[rs[1]])
        if GS < 6:
            return
        P.dma("sp", C.gqqk[:, g0:g0 + TT].rearrange("(c p) t -> p c t", p=128), st_gq[b][:], R=[rs[1]])
        if ASTOP < 4:
            return
        pi = state["pi"] % 4
        state["pi"] += 1
        for tb in range(4):
            for kc in range(8):
                P.pe(lambda e, tb=tb, kc=kc, pi=pi: e.matmul(ps_o[pi][:, tb * 128:(tb + 1) * 128],
                                                      hb[b][:, kc, tb * 128:(tb + 1) * 128],
                                                      wi[:, kc, 12 * 128:13 * 128], start=(kc == 0), stop=(kc == 7)),
                     R=[r_wi[kc], r_hb[b]], W=[r_pso[pi]])
        P.dve(lambda e, pi=pi: e.tensor_copy(out=st_gv[b][:], in_=ps_o[pi][:].rearrange("p (b c) -> p b c", c=128)),
              R=[r_pso[pi]], W=[rs[2]])
        P.dma("sp", C.gqv[g0:g0 + TT, :].rearrange("(b p) c -> p b c", p=128), st_gv[b][:], R=[rs[2]])
        if ASTOP < 5:
            return
        for j in range(4):
            ch = 13 + j
            pi = mm_fm(ch, b)
            if j < 2:
                P.act(lambda e, pi=pi, j=j: e.activation(out=st_nq[b][:, j, :], in_=ps_o[pi][:], func=AF.Copy,
                                                         scale=0.125), R=[r_pso[pi]], W=[rs[3]])
            else:
                P.dve(lambda e, pi=pi, j=j: e.tensor_copy(out=st_nq[b][:, j, :], in_=ps_o[pi][:]),
                      R=[r_pso[pi]], W=[rs[3]])
        P.dma("sp", C.ntqk[:, g0:g0 + TT].rearrange("(c p) t -> p c t", p=128), st_nq[b][:], R=[rs[3]])
        if ASTOP < 6:
            return
        for half in range(2):
            pi = state["pi"] % 4
            state["pi"] += 1
            for tb2 in range(2):
                tb = half * 2 + tb2
                for kc in range(8):
                    P.pe(lambda e, tb=tb, tb2=tb2, kc=kc, pi=pi: e.matmul(ps_o[pi][:, tb2 * 256:(tb2 + 1) * 256],
                                                                   hb[b][:, kc, tb * 128:(tb + 1) * 128],
                                                                   wi[:, kc, 17 * 128:19 * 128],
                                                                   start=(kc == 0), stop=(kc == 7)),
                         R=[r_wi[kc], r_hb[b]], W=[r_pso[pi]])
            P.dve(lambda e, pi=pi, half=half: e.tensor_copy(
                out=st_nv[b][:, half * 2:half * 2 + 2, :], in_=ps_o[pi][:].rearrange("p (b c) -> p b c", c=256)),
                R=[r_pso[pi]], W=[rs[4]])
        P.dma("sp", C.ntv[g0:g0 + TT, :].rearrange("(b p) c -> p b c", p=128), st_nv[b][:], R=[rs[4]])
        if ASTOP < 7:
            return
        for j in range(2):
            pi = mm_fm(19 + j, b)
            P.dve(lambda e, pi=pi, j=j: e.tensor_copy(out=st_pd[b][:, j, :], in_=ps_o[pi][:]),
                  R=[r_pso[pi]], W=[rs[5]])
        P.dma("sp", C.pd[:, g0:g0 + TT].rearrange("(c p) t -> p c t", p=128), st_pd[b][:], R=[rs[5]])

    prep(0)
    for ti in range(len(tiles)):
        if ti + 1 < len(tiles):
            prep(ti + 1)
        main(ti)
    P.end()


GQ_PERM = np.concatenate([np.arange(0, 64), np.arange(128, 192), np.arange(64, 128), np.arange(192, 256)])


def make_consts():
    c = np.zeros((128, NCONST), np.float32)
    c[:, C_ID:C_ID + 128] = np.eye(128)
    blk = np.zeros((128, 128), np.float32)
    blk[:64, :64] = 1.0
    blk[64:, 64:] = 1.0
    c[:, C_BLK:C_BLK + 128] = blk
    sw = np.zeros((128, 128), np.float32)
    for i in range(64):
        sw[2 * i + 1, 2 * i] = -1.0
        sw[2 * i, 2 * i + 1] = 1.0
    c[:, C_SWAP:C_SWAP + 128] = sw
    anti = np.zeros((64, 64), np.float32)
    for i in range(64):
        anti[i, 63 - i] = 1.0
    c[0:64, C_ANTI:C_ANTI + 64] = anti
    c[64:128, C_ANTI:C_ANTI + 64] = anti
    cols = np.arange(64)
    cs = np.clip(cols - 8, 0, 48)
    cm = ((cols[:, None] >= cs[None, :]) & (cols[:, None] < cs[None, :] + 16)).astype(np.float32)
    c[0:64, C_CMASK:C_CMASK + 64] = cm
    c[64:128, C_CMASK:C_CMASK + 64] = cm
    p = np.arange(128)
    c[:, C_EXPO] = (((p % 64) // 2) % 16) / 16.0
    c[:, C_EPS] = EPS
    c[:, C_EPS + 1] = GN_EPS
    c[:, C_EPS + 2] = 1e-24
    s_ = np.arange(64)[:, None]
    t_ = np.arange(64)[None, :]
    mu = np.zeros((128, 128), np.float32)
    ml = np.zeros((128, 128), np.float32)
    for h in range(2):
        mu[h * 64:(h + 1) * 64, 0:64] = (s_ < t_)
        mu[h * 64:(h + 1) * 64, 64:128] = (s_ <= t_)
        ml[h * 64:(h + 1) * 64, 0:64] = (s_ > t_)
        ml[h * 64:(h + 1) * 64, 64:128] = (s_ >= t_)
    c[:, C_MU:C_MU + 128] = mu
    c[:, C_ML:C_ML + 128] = ml
    rst = np.ones((128, 256), np.float32)
    rst[:, ::64] = 0.0
    c[:, C_RST:C_RST + 256] = rst
    c[:, C_IP:C_IP + 64] = np.concatenate([np.eye(64), np.eye(64)], axis=0)
    return c


def make_rope_tab():
    t = np.arange(4096)
    row = (t // 64).astype(np.float32)
    col = (t % 64).astype(np.float32)
    p = np.arange(128)
    i = (p % 64) // 2
    inv = (10000.0 ** (-((i % 16).astype(np.float32)) / 16.0)).astype(np.float32)
    pos = np.where((i < 16)[:, None], row[None, :], col[None, :]).astype(np.float32)
    ang = pos * inv[:, None]
    return np.stack([np.cos(ang), np.sin(ang)]).astype(np.float32)


def make_invc(Ts):
    out = np.zeros((len(Ts), 128, 2, 4096), np.float32)
    wins = (2, 4, 8, 16)
    for ti, T in enumerate(Ts):
        t = np.arange(T)
        for g, w in enumerate(wins):
            lo = np.clip(t - w // 2, 0, T)
            hi = np.clip(t + w - w // 2, 0, T)
            inv = (1.0 / (hi - lo).astype(np.float32)).astype(np.float32)
            j, hp = g // 2, g % 2
            out[ti, hp * 64:(hp + 1) * 64, j, :T] = inv[None, :]
    return out


def pack_weights(inp, L, seq_lens, full=True):
    f = lambda a: np.ascontiguousarray(np.asarray(a, dtype=np.float32))
    w = {}
    w["w_ada"] = f(inp["w_ada"])[:L]
    pv = np.zeros((L, 128, PV_N), np.float32)

    def fm(v, n):
        return np.asarray(v, np.float32).reshape(L, n, 128).transpose(0, 2, 1)

    pv[:, :, PV_GMIX:PV_GMIX + 8] = fm(inp["norm_mix_g"][:L], 8)
    pv[:, :, PV_GFFN:PV_GFFN + 8] = fm(inp["norm_ffn_g"][:L], 8)
    pv[:, :, PV_BADA:PV_BADA + 48] = fm(inp["b_ada"][:L], 48)
    pv[:, :, PV_MU:PV_MU + 9] = fm(inp["rwkv_mu"][:L], 9)
    w0 = np.asarray(inp["rwkv_w0"][:L], np.float32).reshape(L, 2, 2, 128)
    pv[:, :, PV_W0:PV_W0 + 4] = w0.transpose(0, 3, 1, 2).reshape(L, 128, 4)
    a0 = np.asarray(inp["rwkv_a0"][:L], np.float32).reshape(L, 2, 2, 128)
    pv[:, :, PV_A0:PV_A0 + 4] = a0.transpose(0, 3, 1, 2).reshape(L, 128, 4)
    pv[:, :, PV_KK:PV_KK + 2] = fm(inp["rwkv_k_k"][:L], 2)
    pv[:, :, PV_KA:PV_KA + 2] = fm(inp["rwkv_k_a"][:L], 2)
    pv[:, :, PV_RK:PV_RK + 2] = fm(np.asarray(inp["rwkv_r_k"][:L]).reshape(L, 256), 2)
    pv[:, :, PV_LNW:PV_LNW + 2] = fm(inp["rwkv_lnx_w"][:L], 2)
    pv[:, :, PV_LNB:PV_LNB + 2] = fm(inp["rwkv_lnx_b"][:L], 2)
    pv[:, :, PV_QN] = np.tile(np.asarray(inp["gqa_q_norm"][:L], np.float32), (1, 2))
    pv[:, :, PV_KN] = np.tile(np.asarray(inp["gqa_k_norm"][:L], np.float32), (1, 2))
    pv[:, :, PV_PS:PV_PS + 2] = fm(inp["pool_scale"][:L], 2)
    w["pvec"] = pv
    wi = f(inp["w_in"])[:L].copy()
    wi[:, :, 1152:1408] = wi[:, :, 1152 + GQ_PERM]
    w["w_in"] = wi
    wo = f(inp["w_out"])[:L].copy()
    wo[:, 256:512, :] = wo[:, 256 + GQ_PERM, :]
    w["w_out"] = wo
    w["rwkv_w2"] = f(inp["rwkv_w2"])[:L].reshape(L, 128, 256)
    w["rwkv_a2"] = f(inp["rwkv_a2"])[:L].reshape(L, 128, 256)
    w["rwkv_g2"] = f(inp["rwkv_g2"])[:L]
    rp = np.zeros((L, 2048), np.float32)
    rp[:, 48:48 + 1860] = np.asarray(inp["nat_rpb"][:L], np.float32).reshape(L, 1860)
    w["rpb_pad"] = rp
    w["pool_w"] = f(inp["pool_w"])[:L]
    if full:
        nd, nm = (L + 1) // 2, L // 2
        for k in ("ffn_w_gate", "ffn_w_up", "ffn_w_down"):
            w[k] = f(inp[k])[:nd]
        if nm:
            for k in ("moe_router", "moe_w_gate", "moe_w_up", "moe_w_down"):
                w[k] = f(inp[k])[:nm]
    w["consts"] = make_consts()
    w["consts"][:, C_GFIN:C_GFIN + 8] = np.asarray(inp["final_norm_g"], np.float32).reshape(8, 128).T
    w["cs_tab"] = make_rope_tab()
    w["invc"] = make_invc(sorted(set(seq_lens), reverse=True))
    return w


def ph_pool(C, l):
    P = C.P
    P.begin()
    W = TT + 16
    pwf = P.sbuf("pwf", [128, 2, 128], F32); r_pwf = P.res()
    pwb = P.sbuf("pwb", [128, 2, 128], BF16); r_pwb = P.res()
    P.dve(lambda e: e.memset(pwf[:], 0.0), W=[r_pwf])
    for g in range(4):
        j, hp = g // 2, g % 2
        P.dma("sp", pwf[64 * hp:64 * hp + 64, j, 64 * hp:64 * hp + 64], C.pool_w[l, g], R=[r_pwf], W=[r_pwf])
    P.dve(lambda e: e.tensor_copy(out=pwb[:], in_=pwf[:]), R=[r_pwf], W=[r_pwb])
    X = [P.sbuf(f"X{i}", [128, 2, W], F32) for i in range(2)]; r_X = [P.res() for _ in range(2)]
    IV = [P.sbuf(f"IV{i}", [128, 2, TT], F32) for i in range(2)]; r_IV = [P.res() for _ in range(2)]
    A2 = P.sbuf("A2", [128, 2, W], F32); r_A2 = P.res()
    B4 = P.sbuf("B4", [128, 2, W], F32); r_B4 = P.res()
    B8 = P.sbuf("B8", [128, W], F32); r_B8 = P.res()
    S = P.sbuf("S", [128, 2, TT], F32); r_S = P.res()
    db = P.sbuf("db", [128, 2, TT], BF16); r_db = P.res()
    yo = [P.sbuf(f"yo{i}", [128, 2, TT], BF16) for i in range(2)]; r_yo = [P.res() for _ in range(2)]
    ps = [P.psum(f"pps{i}", [128, TT]) for i in range(2)]; r_ps = [P.pres() for _ in range(2)]
    it = 0
    for s, T in enumerate(C.seq_lens):
        tsel = C.Tsel.index(T)
        for i in range(T // TT):
            t0 = i * TT
            g0 = C.offs[s] + t0
            b = it % 2
            it += 1
            lo = max(t0 - 8, 0)
            hi = min(t0 + TT + 8, T)
            if lo > t0 - 8:
                P.pool(lambda e, b=b: e.memset(X[b][:, :, 0:8], 0.0), W=[r_X[b]])
            if hi < t0 + TT + 8:
                P.pool(lambda e, b=b: e.memset(X[b][:, :, W - 8:W], 0.0), W=[r_X[b]])
            u0 = lo - (t0 - 8)
            P.dma("sp", X[b][:, :, u0:u0 + (hi - lo)],
                  C.pd[:, C.offs[s] + lo:C.offs[s] + hi].rearrange("(c p) t -> p c t", p=128), R=[r_X[b]], W=[r_X[b]])
            P.dma("sp", IV[b][:], C.invc[tsel, :, :, t0:t0 + TT], W=[r_IV[b]])
            x = X[b]
            P.pool(lambda e, x=x: e.tensor_tensor(out=A2[:, :, 1:W], in0=x[:, :, 1:W], in1=x[:, :, 0:W - 1], op=ALU.add),
                   R=[r_X[b]], W=[r_A2])
            P.pool(lambda e: e.tensor_tensor(out=B4[:, :, 2:W - 1], in0=A2[:, :, 1:W - 2], in1=A2[:, :, 3:W], op=ALU.add),
                   R=[r_A2], W=[r_B4])
            P.dve(lambda e: e.tensor_tensor(out=B8[:, 4:W - 3], in0=B4[:, 1, 2:W - 5], in1=B4[:, 1, 6:W - 1], op=ALU.add),
                  R=[r_B4], W=[r_B8])
            P.dve(lambda e: e.tensor_copy(out=S[0:64, 0, :], in_=A2[0:64, 0, 8:8 + TT]), R=[r_A2], W=[r_S])
            P.dve(lambda e: e.tensor_copy(out=S[64:128, 0, :], in_=B4[64:128, 0, 8:8 + TT]), R=[r_B4], W=[r_S])
            P.dve(lambda e: e.tensor_copy(out=S[0:64, 1, :], in_=B8[0:64, 8:8 + TT]), R=[r_B8], W=[r_S])
            P.dve(lambda e: e.tensor_tensor(out=S[64:128, 1, :], in0=B8[64:128, 4:4 + TT], in1=B8[64:128, 12:12 + TT],
                                            op=ALU.add), R=[r_B8], W=[r_S])
            P.dve(lambda e, b=b: e.tensor_tensor(out=S[:], in0=S[:], in1=IV[b][:], op=ALU.mult), R=[r_S, r_IV[b]], W=[r_S])
            P.dve(lambda e, x=x: e.tensor_tensor(out=db[:], in0=S[:], in1=x[:, :, 8:8 + TT], op=ALU.subtract),
                  R=[r_S, r_X[b]], W=[r_db])
            for j in range(2):
                P.pe(lambda e, j=j: e.matmul(ps[j][:], pwb[:, j, :], db[:, j, :], start=True, stop=True),
                     R=[r_pwb, r_db], W=[r_ps[j]])
                P.act(lambda e, j=j, b=b: e.activation(out=yo[b][:, j, :], in_=ps[j][:], func=AF.Copy,
                                                       scale=C.pv[:, l, PV_PS + j:PV_PS + j + 1]),
                      R=[r_ps[j]], W=[r_yo[b]])
            P.dma("sp", C.ymix[768:1024, g0:g0 + TT].rearrange("(c p) t -> p c t", p=128), yo[b][:], R=[r_yo[b]])
    P.end()


def ph_nat(C, l):
    P = C.P
    Tm = C.Tmax
    P.begin()
    ones_b = C.cstb[:, 384:512]
    Tq = P.sbuf("Tq", [64, 60, 64], F32); r_Tq = P.res()
    P.dma("sp", Tq[:], bass.AP(C.rpb.tensor, l * 2048, [[1, 64], [31, 60], [1, 64]]), W=[r_Tq])
    EE = P.sbuf("EE", [128, 2, 15, 64], F32); r_EE = P.res()
    ps_s = [P.psum(f"nps{i}", [128, 512]) for i in range(2)]; r_pss = [P.pres() for _ in range(2)]
    ps_n = [P.psum(f"npn{i}", [128, 512]) for i in range(2)]; r_psn = [P.pres() for _ in range(2)]
    ps_d = [P.psum(f"npd{i}", [128, 512]) for i in range(2)]; r_psd = [P.pres() for _ in range(2)]
    anti = C.cst[0:64, C_ANTI:C_ANTI + 64]
    k = 0
    for j in range(2):
        for (d0, d1) in ((0, 8), (8, 15)):
            b = k % 2
            k += 1
            for hp in range(2):
                for dr in range(d0, d1):
                    P.pe(lambda e, b=b, hp=hp, dr=dr, d0=d0, j=j: e.matmul(
                        ps_s[b][64 * hp:64 * hp + 64, (dr - d0) * 64:(dr - d0 + 1) * 64],
                        Tq[:, (2 * j + hp) * 15 + dr, :], anti, start=True, stop=True), R=[r_Tq], W=[r_pss[b]])
            n = (d1 - d0) * 64
            P.act(lambda e, b=b, j=j, d0=d0, d1=d1, n=n: e.activation(
                out=EE[:, j, d0:d1, :], in_=ps_s[b][:, 0:n].rearrange("p (a c) -> p a c", c=64), func=AF.Exp),
                R=[r_pss[b]], W=[r_EE])
    P.dve(lambda e: e.tensor_tensor(out=EE[:].rearrange("p j a c -> p (j a) c"), in0=EE[:].rearrange("p j a c -> p (j a) c"),
                                    in1=bcast(C.cst[:, C_CMASK:C_CMASK + 64].unsqueeze(1), [128, 30, 64]), op=ALU.mult),
          R=[r_EE], W=[r_EE])
    qk = P.sbuf("nqk", [128, 4, Tm], BF16); r_qk = P.res()
    vr = P.sbuf("nvr", [128, Tm // 64, 256], BF16); r_vr = P.res()
    yn = P.sbuf("nyn", [128, 2, Tm], BF16); r_yn = P.res()
    pe_ = [P.sbuf(f"npe{i}", [128, 512], F32) for i in range(2)]; r_pe = [P.res() for _ in range(2)]
    pm = [P.sbuf(f"npm{i}", [128, 512], BF16) for i in range(2)]; r_pm = [P.res() for _ in range(2)]
    rd = [P.sbuf(f"nrd{i}", [128, 64], F32) for i in range(2)]; r_rd = [P.res() for _ in range(2)]
    it = 0
    for s, T in enumerate(C.seq_lens):
        g0 = C.offs[s]
        R = T // 64
        P.dma("sp", qk[:, :, 0:T], C.ntqk[:, g0:g0 + T].rearrange("(c p) t -> p c t", p=128), R=[r_qk], W=[r_qk])
        for hp in range(2):
            P.dma("sp", vr[64 * hp:64 * hp + 64, 0:R, :], C.ntv[g0:g0 + T, :].rearrange("(r p) c -> p r c", p=64),
                  R=[r_vr], W=[r_vr])
        for r in range(R):
            rs = min(max(r - 4, 0), R - 8)
            dl = r - rs
            for j in range(2):
                b = it % 2
                it += 1
                for hp in range(2):
                    sl = slice(64 * hp, 64 * hp + 64)
                    for kr in range(8):
                        P.pe(lambda e, b=b, sl=sl, kr=kr, j=j, rs=rs, r=r: e.matmul(
                            ps_s[b][sl, kr * 64:(kr + 1) * 64], qk[sl, 2 + j, (rs + kr) * 64:(rs + kr + 1) * 64],
                            qk[sl, j, r * 64:(r + 1) * 64], start=True, stop=True), R=[r_qk], W=[r_pss[b]])
                P.act(lambda e, b=b: e.activation(out=pe_[b][:], in_=ps_s[b][:], func=AF.Exp), R=[r_pss[b]], W=[r_pe[b]])
                P.dve(lambda e, b=b, j=j, dl=dl: e.tensor_tensor(
                    out=pm[b][:].rearrange("p (a c) -> p a c", c=64), in0=pe_[b][:].rearrange("p (a c) -> p a c", c=64),
                    in1=EE[:, j, 7 - dl:15 - dl, :], op=ALU.mult), R=[r_pe[b], r_EE], W=[r_pm[b]])
                for hp in range(2):
                    sl = slice(64 * hp, 64 * hp + 64)
                    for kr in range(8):
                        P.pe(lambda e, b=b, sl=sl, kr=kr, j=j, hp=hp, rs=rs: e.matmul(
                            ps_n[b][sl, 0:64], vr[sl, rs + kr, (2 * j + hp) * 64:(2 * j + hp + 1) * 64],
                            pm[b][sl, kr * 64:(kr + 1) * 64], start=(kr == 0), stop=(kr == 7)),
                            R=[r_vr, r_pm[b]], W=[r_psn[b]])
                    for kr in range(8):
                        P.pe(lambda e, b=b, sl=sl, kr=kr: e.matmul(
                            ps_d[b][sl, 0:64], ones_b[sl, 0:64], pm[b][sl, kr * 64:(kr + 1) * 64],
                            start=(kr == 0), stop=(kr == 7)), R=[r_pm[b]], W=[r_psd[b]])
                P.dve(lambda e, b=b: e.reciprocal(out=rd[b][:], in_=ps_d[b][:, 0:64]), R=[r_psd[b]], W=[r_rd[b]])
                P.dve(lambda e, b=b, j=j, r=r: e.tensor_tensor(out=yn[:, j, r * 64:(r + 1) * 64], in0=ps_n[b][:, 0:64],
                                                              in1=rd[b][:], op=ALU.mult),
                      R=[r_psn[b], r_rd[b]], W=[r_yn])
        P.dma("sp", C.ymix[512:768, g0:g0 + T].rearrange("(c p) t -> p c t", p=128), yn[:, :, 0:T], R=[r_yn])
    P.end()


def ph_gqa(C, l):
    P = C.P
    Tm = C.Tmax
    P.begin()
    ones_b = C.cstb[:, 384:512]
    qk = P.sbuf("gqk", [128, 3, Tm], BF16); r_qk = P.res()
    v = P.sbuf("gv", [128, Tm // 128, 128], BF16); r_v = P.res()
    yg = P.sbuf("gyg", [128, 2, Tm], BF16); r_yg = P.res()
    pt = [P.sbuf(f"gpt{i}", [128, 512], BF16) for i in range(3)]; r_pt = [P.res() for _ in range(3)]
    rd = P.sbuf("grd", [128, 512], F32); r_rd = P.res()
    ps_s = [P.psum(f"gps{i}", [128, 512]) for i in range(3)]; r_pss = [P.pres() for _ in range(3)]
    ps_n = [P.psum(f"gpn{i}", [128, 512]) for i in range(2)]; r_psn = [P.pres() for _ in range(2)]
    ps_d = [P.psum(f"gpd{i}", [128, 512]) for i in range(2)]; r_psd = [P.pres() for _ in range(2)]
    it = 0
    ob = 0
    for s, T in enumerate(C.seq_lens):
        g0 = C.offs[s]
        P.dma("sp", qk[:, :, 0:T], C.gqqk[:, g0:g0 + T].rearrange("(c p) t -> p c t", p=128), R=[r_qk], W=[r_qk])
        P.dma("sp", v[:, 0:T // 128, :], C.gqv[g0:g0 + T, :].rearrange("(b p) c -> p b c", p=128), R=[r_v], W=[r_v])
        nkb = T // 128
        for j in range(2):
            for qb in range(T // 512):
                o = ob % 2
                ob += 1
                for kb in range(nkb):
                    for hp in range(2):
                        sl = slice(64 * hp, 64 * hp + 64)
                        b = it % 3
                        it += 1
                        P.pe(lambda e, b=b, sl=sl, kb=kb, j=j, qb=qb: e.matmul(
                            ps_s[b][:], qk[sl, 2, kb * 128:(kb + 1) * 128], qk[sl, j, qb * 512:(qb + 1) * 512],
                            start=True, stop=True), R=[r_qk], W=[r_pss[b]])
                        P.act(lambda e, b=b: e.activation(out=pt[b][:], in_=ps_s[b][:], func=AF.Exp),
                              R=[r_pss[b]], W=[r_pt[b]])
                        P.pe(lambda e, b=b, sl=sl, kb=kb, o=o: e.matmul(
                            ps_n[o][sl, :], v[:, kb, sl], pt[b][:], start=(kb == 0), stop=(kb == nkb - 1)),
                            R=[r_v, r_pt[b]], W=[r_psn[o]])
                        P.pe(lambda e, b=b, sl=sl, kb=kb, o=o: e.matmul(
                            ps_d[o][sl, :], ones_b[:, 0:64], pt[b][:], start=(kb == 0), stop=(kb == nkb - 1)),
                            R=[r_pt[b]], W=[r_psd[o]])
                P.dve(lambda e, o=o: e.reciprocal(out=rd[:], in_=ps_d[o][:]), R=[r_psd[o]], W=[r_rd])
                P.dve(lambda e, o=o, j=j, qb=qb: e.tensor_tensor(out=yg[:, j, qb * 512:(qb + 1) * 512], in0=ps_n[o][:],
                                                                in1=rd[:], op=ALU.mult), R=[r_psn[o], r_rd], W=[r_yg])
        P.dma("sp", C.ymix[256:512, g0:g0 + T].rearrange("(c p) t -> p c t", p=128), yg[:, :, 0:T], R=[r_yg])
    P.end()


def ph_rwkv(C, l):
    import os
    rw = 9
    ph_RA(C, l)
    if rw >= 2:
        ph_RS(C, l, 0)
    if rw >= 3:
        ph_RS(C, l, 1)
    if rw >= 4:
        ph_RP(C, l)


WSC = -0.6065306597126334


def ph_RA(C, l):
    P = C.P
    P.begin()
    blk_b = C.cstb[:, 128:256]
    pvl = C.pv[:, l, :]
    w2b = P.sbuf("w2b", [128, 256], BF16); a2b = P.sbuf("a2b", [128, 256], BF16); g2b = P.sbuf("g2b", [128, 256], BF16)
    r_wt = P.res()
    P.dma("pool", w2b[:], C.w2[l], W=[r_wt])
    P.dma("pool", a2b[:], C.a2[l], W=[r_wt])
    P.dma("pool", g2b[:], C.g2[l], W=[r_wt])
    sm = P.sbuf("sm", [128, 32], F32); r_sm = P.res()
    P.dve(lambda e: e.tensor_scalar(out=sm[:, 0:9], in0=pvl[:, PV_MU:PV_MU + 9], scalar1=-1.0, scalar2=1.0, op0=ALU.mult,
                                    op1=ALU.add), W=[r_sm])
    P.dve(lambda e: e.tensor_scalar(out=sm[:, 9:18], in0=pvl[:, PV_MU:PV_MU + 9], scalar1=0.5, scalar2=None, op0=ALU.mult),
          W=[r_sm])
    P.dve(lambda e: e.tensor_scalar(out=sm[:, 18:20], in0=pvl[:, PV_KA:PV_KA + 2], scalar1=-1.0, scalar2=1.0, op0=ALU.mult,
                                    op1=ALU.add), W=[r_sm])
    PA = [P.sbuf(f"PA{i}", [128, 9, TT + 2], F32) for i in range(2)]; r_PA = [P.res() for _ in range(2)]
    sft = P.sbuf("sft", [128, 9, TT], F32); r_sft = P.res()
    pp = P.sbuf("pp", [128, 9, TT], F32); r_pp = P.res()
    twl = P.sbuf("twl", [128, TT], BF16); alb = P.sbuf("alb", [128, TT], BF16); sgl = P.sbuf("sgl", [128, TT], BF16)
    r_lo = P.res()
    ST = [P.sbuf(f"ST{i}", [128, 11, 2, TT], F32) for i in range(2)]; r_ST = [P.res() for _ in range(2)]
    kks = P.sbuf("kks", [128, TT], F32); r_kks = P.res()
    sqb = P.sbuf("sqb", [128, TT], BF16); r_sqb = P.res()
    rs_ = P.sbuf("rs_", [128, TT], F32); r_rs = P.res()
    tmp = P.sbuf("tmp", [128, TT], F32); r_tmp = P.res()
    rkb = P.sbuf("rkb", [128, TT], BF16); r_rkb = P.res()
    ps = [P.psum(f"rps{i}", [128, TT]) for i in range(4)]; r_ps = [P.pres() for _ in range(4)]
    pc = [0]

    def nps():
        k = pc[0] % 4
        pc[0] += 1
        return k

    it = 0
    for s, T in enumerate(C.seq_lens):
        for i in range(T // TT):
            t0 = i * TT
            g0 = C.offs[s] + t0
            b = it % 2
            it += 1
            lo, hi = max(t0 - 1, 0), min(t0 + TT + 1, T)
            if lo > t0 - 1:
                P.pool(lambda e, b=b: e.memset(PA[b][:, :, 0:1], 0.0), W=[r_PA[b]])
            if hi < t0 + TT + 1:
                P.pool(lambda e, b=b: e.memset(PA[b][:, :, TT + 1:TT + 2], 0.0), W=[r_PA[b]])
            u0 = lo - (t0 - 1)
            P.dma("sp", PA[b][:, :, u0:u0 + hi - lo],
                  C.pa[:, C.offs[s] + lo:C.offs[s] + hi].rearrange("(c p) t -> p c t", p=128), R=[r_PA[b]], W=[r_PA[b]])
            pa_ = PA[b]
            st = ST[b]
            rst = r_ST[b]
            P.pool(lambda e, pa_=pa_: e.tensor_tensor(out=sft[:], in0=pa_[:, :, 0:TT], in1=pa_[:, :, 2:TT + 2], op=ALU.add),
                   R=[r_PA[b]], W=[r_sft])
            for c in range(9):
                P.dve(lambda e, c=c, pa_=pa_: e.tensor_scalar(out=pp[:, c, :], in0=pa_[:, c, 1:TT + 1], scalar1=sm[:, c:c + 1],
                                                             scalar2=None, op0=ALU.mult), R=[r_PA[b], r_sm], W=[r_pp])
                P.dve(lambda e, c=c: e.scalar_tensor_tensor(out=pp[:, c, :], in0=sft[:, c, :], scalar=sm[:, 9 + c:10 + c],
                                                           in1=pp[:, c, :], op0=ALU.mult, op1=ALU.add),
                      R=[r_sft, r_pp], W=[r_pp])
            P.pool(lambda e, st=st: e.tensor_copy(out=st[:, 0], in_=pp[:, 0:2, :]), R=[r_pp], W=[rst])
            P.pool(lambda e, st=st: e.tensor_copy(out=st[:, 1], in_=pp[:, 4:6, :]), R=[r_pp], W=[rst])
            P.act(lambda e: e.activation(out=twl[:], in_=pp[:, 6, :], func=AF.Tanh), R=[r_pp], W=[r_lo])
            P.act(lambda e: e.activation(out=alb[:], in_=pp[:, 7, :], func=AF.Copy), R=[r_pp], W=[r_lo])
            P.act(lambda e: e.activation(out=sgl[:], in_=pp[:, 8, :], func=AF.Sigmoid), R=[r_pp], W=[r_lo])
            for d in range(2):
                sl = slice(64 * d, 64 * d + 64)
                for c in range(2):
                    k = nps()
                    P.pe(lambda e, k=k, sl=sl, c=c: e.matmul(ps[k][:], w2b[sl, c * 128:(c + 1) * 128], twl[sl, :],
                                                             start=True, stop=True), R=[r_wt, r_lo], W=[r_ps[k]])
                    P.act(lambda e, k=k, d=d, c=c, st=st: e.activation(
                        out=st[:, 7 + d, c, :], in_=ps[k][:], func=AF.Sigmoid,
                        bias=pvl[:, PV_W0 + d * 2 + c:PV_W0 + d * 2 + c + 1]), R=[r_ps[k]], W=[rst])
                    k = nps()
                    P.pe(lambda e, k=k, sl=sl, c=c: e.matmul(ps[k][:], a2b[sl, c * 128:(c + 1) * 128], alb[sl, :],
                                                             start=True, stop=True), R=[r_wt, r_lo], W=[r_ps[k]])
                    P.act(lambda e, k=k, d=d, c=c, st=st: e.activation(
                        out=st[:, 5 + d, c, :], in_=ps[k][:], func=AF.Sigmoid,
                        bias=pvl[:, PV_A0 + d * 2 + c:PV_A0 + d * 2 + c + 1]), R=[r_ps[k]], W=[rst])
            for c in range(2):
                P.dve(lambda e, c=c: e.tensor_scalar(out=kks[:], in0=pp[:, 2 + c, :], scalar1=pvl[:, PV_KK + c:PV_KK + c + 1],
                                                    scalar2=None, op0=ALU.mult), R=[r_pp], W=[r_kks])
                P.act(lambda e: e.activation(out=sqb[:], in_=kks[:], func=AF.Square), R=[r_kks], W=[r_sqb])
                k = nps()
                P.pe(lambda e, k=k: e.matmul(ps[k][:], blk_b, sqb[:], start=True, stop=True), R=[r_sqb], W=[r_ps[k]])
                P.act(lambda e, k=k: e.activation(out=rs_[:], in_=ps[k][:], func=AF.Sqrt), R=[r_ps[k]], W=[r_rs])
                P.dve(lambda e: e.tensor_scalar(out=rs_[:], in0=rs_[:], scalar1=1e-12, scalar2=None, op0=ALU.max),
                      R=[r_rs], W=[r_rs])
                P.dve(lambda e: e.reciprocal(out=rs_[:], in_=rs_[:]), R=[r_rs], W=[r_rs])
                P.dve(lambda e, c=c, st=st: e.tensor_tensor(out=st[:, 2, c, :], in0=kks[:], in1=rs_[:], op=ALU.mult),
                      R=[r_kks, r_rs], W=[rst])
                for d in range(2):
                    P.dve(lambda e, c=c, d=d, st=st: e.tensor_scalar(
                        out=tmp[:], in0=st[:, 5 + d, c, :], scalar1=pvl[:, PV_KA + c:PV_KA + c + 1],
                        scalar2=sm[:, 18 + c:19 + c], op0=ALU.mult, op1=ALU.add), R=[rst, r_sm], W=[r_tmp])
                    P.dve(lambda e, c=c, d=d, st=st: e.tensor_tensor(out=st[:, 3 + d, c, :], in0=tmp[:], in1=pp[:, 2 + c, :],
                                                                    op=ALU.mult), R=[r_tmp, r_pp], W=[rst])
                P.pool(lambda e, c=c, st=st: e.tensor_tensor(out=tmp[:], in0=st[:, 3, c, :], in1=st[:, 4, c, :], op=ALU.add),
                       R=[rst], W=[r_tmp])
                P.dve(lambda e, c=c: e.scalar_tensor_tensor(out=rkb[:], in0=tmp[:], scalar=pvl[:, PV_RK + c:PV_RK + c + 1],
                                                           in1=pp[:, c, :], op0=ALU.mult, op1=ALU.mult),
                      R=[r_tmp, r_pp], W=[r_rkb])
                k = nps()
                P.pe(lambda e, k=k: e.matmul(ps[k][:], blk_b, rkb[:], start=True, stop=True), R=[r_rkb], W=[r_ps[k]])
                P.dve(lambda e, k=k, c=c: e.tensor_tensor(out=tmp[:], in0=ps[k][:], in1=pp[:, 4 + c, :], op=ALU.mult),
                      R=[r_ps[k], r_pp, r_rkb], W=[r_tmp])
                k = nps()
                P.pe(lambda e, k=k, c=c: e.matmul(ps[k][:], g2b[:, c * 128:(c + 1) * 128], sgl[:], start=True, stop=True),
                     R=[r_wt, r_lo], W=[r_ps[k]])
                P.act(lambda e, k=k, c=c, st=st: e.activation(out=st[:, 9, c, :], in_=ps[k][:], func=AF.Copy,
                                                              scale=pvl[:, PV_LNW + c:PV_LNW + c + 1]),
                      R=[r_ps[k]], W=[rst])
                P.dve(lambda e, k=k, c=c, st=st: e.scalar_tensor_tensor(
                    out=st[:, 10, c, :], in0=tmp[:], scalar=pvl[:, PV_LNB + c:PV_LNB + c + 1], in1=ps[k][:],
                    op0=ALU.add, op1=ALU.mult), R=[r_tmp, r_ps[k]], W=[rst])
            for a in range(9):
                P.dma("sp", C.rkv[a, :, g0:g0 + TT].rearrange("(c p) t -> p c t", p=128), st[:, a], R=[rst])
            for a in range(2):
                P.dma("sp", C.g23[a, :, g0:g0 + TT].rearrange("(c p) t -> p c t", p=128), st[:, 9 + a], R=[rst])
    P.end()
RT = 256


def ph_RS(C, l, d):
    P = C.P
    P.begin()
    NCH = RT // 64
    ident64 = C.cst[0:64, C_ID:C_ID + 64]
    mk = C.cst[0:64, (C_MU if d == 0 else C_ML):(C_MU if d == 0 else C_ML) + 128]
    mkT = C.cst[0:64, (C_ML if d == 0 else C_MU):(C_ML if d == 0 else C_MU) + 64]
    rstm = C.cst[0:64, C_RST:C_RST + RT]
    IN = [P.sbuf(f"IN{i}", [64, 6, 4, RT], F32) for i in range(2)]; r_IN = [P.res() for _ in range(2)]
    mkt = lambda n: P.sbuf(n, [64, 4, RT], F32)
    Lg, pre, cum, exl, rem = mkt("Lg"), mkt("pre"), mkt("cum"), mkt("exl"), mkt("rem")
    e_in, e_ex, e_ng, e_rm, kb = mkt("e_in"), mkt("e_ex"), mkt("e_ng"), mkt("e_rm"), mkt("kb")
    gC = P.sbuf("gC", [64, 4, NCH], F32)
    r_el = P.res()
    AR = P.sbuf("AR", [64, 4, NCH, 128], F32); r_AR = P.res()
    BbT, KbT = mkt("BbT"), mkt("KbT"); r_bk = P.res()
    Btl, Ktl = mkt("Btl"), mkt("Ktl"); r_tl = P.res()
    dG = P.sbuf("dG", [64, 4, NCH, 64], F32); r_dG = P.res()
    NB = NCH * 4
    MS = P.sbuf("MS", [64, NB, 256], F32); r_MS = [P.res() for _ in range(NCH)]
    XP = P.sbuf("XP", [64, NB, 128], F32); r_XP = [P.res() for _ in range(NCH)]
    PT = P.sbuf("PT", [64, NB, 64], F32); r_PT = [P.res() for _ in range(NCH)]
    TK = P.sbuf("TK", [64, NCH, 3, 256], F32); r_TK = [P.res() for _ in range(NCH)]
    S = P.sbuf("S", [64, 4, 64], F32); r_S = P.res()
    Wt = P.sbuf("Wt", [64, 4, 64], F32); r_Wt = P.res()
    Ut = P.sbuf("Ut", [64, 4, 64], F32); r_Ut = P.res()
    Yt = [P.sbuf(f"Yt{i}", [64, NCH, 256], F32) for i in range(2)]; r_Yt = [P.res() for _ in range(2)]
    pA = [P.psum(f"spA{i}", [128, 512]) for i in range(2)]; r_pA = [P.pres() for _ in range(2)]
    pB = [P.psum(f"spB{i}", [128, 512]) for i in range(2)]; r_pB = [P.pres() for _ in range(2)]
    pW = P.psum("spW", [128, 512]); r_pW = P.pres()
    pU = P.psum("spU", [128, 512]); r_pU = P.pres()
    pY = P.psum("spY", [128, 512]); r_pY = P.pres()
    pS = P.psum("spS", [128, 512]); r_pS = P.pres()
    arr_idx = (0, 1, 2, 3 + d, 5 + d, 7 + d)
    c4 = lambda t: t.rearrange("p h (c t) -> p h c t", t=64)
    it = 0
    for s, T in enumerate(C.seq_lens):
        ntile = T // RT
        order = list(range(ntile)) if d == 0 else list(range(ntile - 1, -1, -1))
        P.dve(lambda e: e.memset(S[:], 0.0), R=[r_S], W=[r_S])

        def load(ti, b):
            g0 = C.offs[s] + ti * RT
            for a, ai in enumerate(arr_idx):
                P.dma("sp", IN[b][:, a], C.rkv[ai, :, g0:g0 + RT].rearrange("(h k) t -> k h t", k=64),
                      R=[r_IN[b]], W=[r_IN[b]])

        load(order[0], it % 2)
        for oi, ti in enumerate(order):
            b = it % 2
            it += 1
            if oi + 1 < len(order):
                load(order[oi + 1], (b + 1) % 2)
            g0 = C.offs[s] + ti * RT
            X = IN[b]
            rin = r_IN[b]
            r_, v_, kk_, kd_, a_, sg_ = (X[:, a] for a in range(6))
            P.dve(lambda e, sg_=sg_: e.tensor_scalar(out=Lg[:], in0=sg_, scalar1=WSC, scalar2=None, op0=ALU.mult),
                  R=[rin, r_el], W=[r_el])
            for h in range(4):
                P.dve(lambda e, h=h: e.tensor_tensor_scan(out=pre[:, h, :], data0=rstm, data1=Lg[:, h, :], initial=0.0,
                                                          op0=ALU.mult, op1=ALU.add), R=[r_el], W=[r_el])
            tot_b = bcast(c4(pre[:])[:, :, :, 63:64], [64, 4, NCH, 64])
            if d == 0:
                P.pool(lambda e: e.tensor_copy(out=cum[:], in_=pre[:]), R=[r_el], W=[r_el])
                P.dve(lambda e: e.tensor_tensor(out=exl[:], in0=pre[:], in1=Lg[:], op=ALU.subtract), R=[r_el], W=[r_el])
            else:
                P.dve(lambda e: e.tensor_tensor(out=c4(exl[:]), in0=tot_b, in1=c4(pre[:]), op=ALU.subtract),
                      R=[r_el], W=[r_el])
                P.dve(lambda e: e.tensor_tensor(out=cum[:], in0=exl[:], in1=Lg[:], op=ALU.add), R=[r_el], W=[r_el])
            P.dve(lambda e: e.tensor_tensor(out=c4(rem[:]), in0=tot_b, in1=c4(cum[:]), op=ALU.subtract), R=[r_el], W=[r_el])
            P.act(lambda e: e.activation(out=e_in[:], in_=cum[:], func=AF.Exp), R=[r_el], W=[r_el])
            P.act(lambda e: e.activation(out=e_ex[:], in_=exl[:], func=AF.Exp), R=[r_el], W=[r_el])
            P.act(lambda e: e.activation(out=e_ng[:], in_=cum[:], func=AF.Exp, scale=-1.0), R=[r_el], W=[r_el])
            P.act(lambda e: e.activation(out=e_rm[:], in_=rem[:], func=AF.Exp), R=[r_el], W=[r_el])
            P.act(lambda e: e.activation(out=gC[:], in_=c4(pre[:])[:, :, :, 63], func=AF.Exp), R=[r_el], W=[r_el])
            P.dve(lambda e, kk_=kk_: e.scalar_tensor_tensor(out=AR[:, :, :, 0:64], in0=c4(kk_), scalar=-1.0, in1=c4(e_ex[:]),
                                                           op0=ALU.mult, op1=ALU.mult), R=[rin, r_el, r_AR], W=[r_AR])
            P.dve(lambda e, r_=r_: e.tensor_tensor(out=AR[:, :, :, 64:128], in0=c4(r_), in1=c4(e_in[:]), op=ALU.mult),
                  R=[rin, r_el, r_AR], W=[r_AR])
            P.pool(lambda e, kk_=kk_, a_=a_: e.tensor_tensor(out=kb[:], in0=kk_, in1=a_, op=ALU.mult), R=[rin, r_el], W=[r_el])
            P.dve(lambda e: e.tensor_tensor(out=BbT[:], in0=kb[:], in1=e_ng[:], op=ALU.mult), R=[r_el, r_bk], W=[r_bk])
            P.dve(lambda e, kd_=kd_: e.tensor_tensor(out=KbT[:], in0=kd_, in1=e_ng[:], op=ALU.mult), R=[rin, r_el, r_bk], W=[r_bk])
            P.pool(lambda e: e.tensor_tensor(out=Btl[:], in0=kb[:], in1=e_rm[:], op=ALU.mult), R=[r_el, r_tl], W=[r_tl])
            P.pool(lambda e, kd_=kd_: e.tensor_tensor(out=Ktl[:], in0=kd_, in1=e_rm[:], op=ALU.mult), R=[rin, r_el, r_tl], W=[r_tl])
            P.dve(lambda e: e.tensor_tensor(out=dG[:], in0=bcast(gC[:].unsqueeze(3), [64, 4, NCH, 64]),
                                            in1=bcast(ident64.unsqueeze(1).unsqueeze(1), [64, 4, NCH, 64]), op=ALU.mult),
                  R=[r_el, r_dG], W=[r_dG])
            for c in range(NCH):
                q = c % 2
                csl = slice(c * 64, (c + 1) * 64)
                for h in range(4):
                    bank, off = h // 2, (h % 2) * 256
                    P.pe(lambda e, h=h, c=c, csl=csl, bank=bank, off=off: e.matmul(
                        pA[bank][0:64, off:off + 128], BbT[:, h, csl], AR[:, h, c, :], start=True, stop=True),
                        R=[r_bk, r_AR], W=[r_pA[bank]])
                    P.pe(lambda e, h=h, c=c, csl=csl, bank=bank, off=off: e.matmul(
                        pA[bank][0:64, off + 128:off + 256], KbT[:, h, csl], AR[:, h, c, :], start=True, stop=True),
                        R=[r_bk, r_AR], W=[r_pA[bank]])
                    P.pe(lambda e, h=h, c=c, csl=csl, q=q: e.matmul(
                        pB[q][0:64, h * 64:(h + 1) * 64], AR[:, h, c, 0:64], BbT[:, h, csl], start=True, stop=True),
                        R=[r_bk, r_AR], W=[r_pB[q]])
                for bank in range(2):
                    P.dve(lambda e, bank=bank, c=c: e.tensor_tensor(
                        out=MS[:, c * 4 + bank * 2:c * 4 + bank * 2 + 2, :].rearrange("p b (m t) -> p b m t", t=128),
                        in0=pA[bank][0:64, :].rearrange("p (b m t) -> p b m t", b=2, t=128),
                        in1=bcast(mk.unsqueeze(1).unsqueeze(1), [64, 2, 2, 128]), op=ALU.mult),
                        R=[r_pA[bank], r_MS[c]], W=[r_MS[c]])
                bs = slice(c * 4, c * 4 + 4)
                P.dve(lambda e, bs=bs, q=q: e.tensor_tensor(out=PT[:, bs, :],
                                                            in0=pB[q][0:64, 0:256].rearrange("p (b t) -> p b t", t=64),
                                                            in1=bcast(mkT.unsqueeze(1), [64, 4, 64]), op=ALU.mult),
                      R=[r_pB[q], r_PT[c]], W=[r_PT[c]])
                P.dve(lambda e, bs=bs: e.tensor_tensor(out=XP[:, bs, 0:64], in0=MS[:, bs, 0:64],
                                                       in1=bcast(ident64.unsqueeze(1), [64, 4, 64]), op=ALU.add),
                      R=[r_MS[c], r_XP[c]], W=[r_XP[c]])
                P.pool(lambda e, bs=bs: e.tensor_copy(out=XP[:, bs, 64:128], in_=MS[:, bs, 0:64]), R=[r_MS[c], r_XP[c]],
                       W=[r_XP[c]])
                for lev in range(6):
                    a_bank = pA[q]
                    for bi in range(4):
                        blk = c * 4 + bi
                        if lev == 0:
                            P.pe(lambda e, blk=blk, bi=bi, a_bank=a_bank: e.matmul(
                                a_bank[0:64, bi * 128 + 64:bi * 128 + 128], PT[:, blk, :], XP[:, blk, 64:128],
                                start=True, stop=True), R=[r_PT[c], r_XP[c]], W=[r_pA[q]])
                        elif lev < 5:
                            P.pe(lambda e, blk=blk, bi=bi, a_bank=a_bank: e.matmul(
                                a_bank[0:64, bi * 128:bi * 128 + 128], PT[:, blk, :], XP[:, blk, :],
                                start=True, stop=True), R=[r_PT[c], r_XP[c]], W=[r_pA[q]])
                        else:
                            P.pe(lambda e, blk=blk, bi=bi, a_bank=a_bank: e.matmul(
                                a_bank[0:64, bi * 128:bi * 128 + 64], PT[:, blk, :], XP[:, blk, 0:64],
                                start=True, stop=True), R=[r_PT[c], r_XP[c]], W=[r_pA[q]])
                        if lev < 5:
                            P.pe(lambda e, blk=blk, bi=bi, q=q: e.matmul(
                                pB[q][0:64, bi * 64:(bi + 1) * 64], XP[:, blk, 64:128], PT[:, blk, :],
                                start=True, stop=True), R=[r_PT[c], r_XP[c]], W=[r_pB[q]])
                    av = a_bank[0:64, :].rearrange("p (b m t) -> p b m t", b=4, t=64)
                    if lev > 0:
                        P.dve(lambda e, bs=bs, av=av: e.tensor_tensor(out=XP[:, bs, 0:64], in0=XP[:, bs, 0:64],
                                                                      in1=av[:, :, 0, :], op=ALU.add),
                              R=[r_pA[q], r_XP[c]], W=[r_XP[c]])
                    if lev < 5:
                        P.dve(lambda e, bs=bs, av=av: e.tensor_copy(out=XP[:, bs, 64:128], in_=av[:, :, 1, :]),
                              R=[r_pA[q], r_XP[c]], W=[r_XP[c]])
                        P.act(lambda e, bs=bs, q=q: e.activation(out=PT[:, bs, :],
                                                                 in_=pB[q][0:64, 0:256].rearrange("p (b t) -> p b t", t=64),
                                                                 func=AF.Copy), R=[r_pB[q], r_PT[c]], W=[r_PT[c]])
                srcs = (v_, Btl[:], Ktl[:])
                srs = (rin, r_tl, r_tl)
                for kind in range(3):
                    bank = pA[q] if kind < 2 else pB[q]
                    rb = r_pA[q] if kind < 2 else r_pB[q]
                    for h in range(4):
                        o0 = (kind % 2) * 256 + h * 64
                        P.pe(lambda e, kind=kind, h=h, csl=csl, bank=bank, o0=o0, srcs=srcs: e.transpose(
                            bank[0:64, o0:o0 + 64], srcs[kind][:, h, csl], ident64), R=[srs[kind]], W=[rb])
                P.dve(lambda e, c=c, q=q: e.tensor_copy(out=TK[:, c, 0:2, :], in_=pA[q][0:64, :].rearrange("p (k f) -> p k f", f=256)),
                      R=[r_pA[q], r_TK[c]], W=[r_TK[c]])
                P.act(lambda e, c=c, q=q: e.activation(out=TK[:, c, 2, :], in_=pB[q][0:64, 0:256], func=AF.Copy),
                      R=[r_pB[q], r_TK[c]], W=[r_TK[c]])
            yb = it % 2
            corder = list(range(NCH)) if d == 0 else list(range(NCH - 1, -1, -1))
            for c in corder:
                rr = [r_MS[c], r_XP[c], r_TK[c]]
                for h in range(4):
                    blk = c * 4 + h
                    hs = slice(h * 64, (h + 1) * 64)
                    P.pe(lambda e, h=h, c=c, hs=hs: e.matmul(pW[0:64, hs], AR[:, h, c, 0:64], S[:, h, :],
                                                           start=True, stop=False), R=[r_AR, r_S], W=[r_pW])
                    P.pe(lambda e, blk=blk, c=c, hs=hs: e.matmul(pW[0:64, hs], MS[:, blk, 128:192], TK[:, c, 0, hs],
                                                                start=False, stop=True), R=rr, W=[r_pW])
                P.act(lambda e: e.activation(out=Wt[:], in_=pW[0:64, 0:256].rearrange("p (h v) -> p h v", v=64), func=AF.Copy),
                      R=[r_pW, r_Wt], W=[r_Wt])
                for h in range(4):
                    blk = c * 4 + h
                    hs = slice(h * 64, (h + 1) * 64)
                    P.pe(lambda e, blk=blk, h=h, hs=hs: e.matmul(pU[0:64, hs], XP[:, blk, 0:64], Wt[:, h, :],
                                                                start=True, stop=True), R=rr + [r_Wt], W=[r_pU])
                P.dve(lambda e: e.tensor_copy(out=Ut[:], in_=pU[0:64, 0:256].rearrange("p (h v) -> p h v", v=64)),
                      R=[r_pU, r_Ut], W=[r_Ut])
                for h in range(4):
                    blk = c * 4 + h
                    hs = slice(h * 64, (h + 1) * 64)
                    P.pe(lambda e, h=h, c=c, hs=hs: e.matmul(pY[0:64, hs], AR[:, h, c, 64:128], S[:, h, :],
                                                           start=True, stop=False), R=[r_AR, r_S], W=[r_pY])
                    P.pe(lambda e, blk=blk, h=h, hs=hs: e.matmul(pY[0:64, hs], MS[:, blk, 64:128], Ut[:, h, :],
                                                                start=False, stop=False), R=rr + [r_Ut], W=[r_pY])
                    P.pe(lambda e, blk=blk, c=c, hs=hs: e.matmul(pY[0:64, hs], MS[:, blk, 192:256], TK[:, c, 0, hs],
                                                                start=False, stop=True), R=rr, W=[r_pY])
                P.act(lambda e, c=c, yb=yb: e.activation(out=Yt[yb][:, c, :], in_=pY[0:64, 0:256], func=AF.Copy),
                      R=[r_pY, r_Yt[yb]], W=[r_Yt[yb]])
                for h in range(4):
                    hs = slice(h * 64, (h + 1) * 64)
                    P.pe(lambda e, c=c, hs=hs, h=h: e.matmul(pS[0:64, hs], TK[:, c, 1, hs], Ut[:, h, :],
                                                           start=True, stop=False), R=rr + [r_Ut], W=[r_pS])
                    P.pe(lambda e, c=c, hs=hs: e.matmul(pS[0:64, hs], TK[:, c, 2, hs], TK[:, c, 0, hs],
                                                       start=False, stop=False), R=rr, W=[r_pS])
                    P.pe(lambda e, h=h, c=c, hs=hs: e.matmul(pS[0:64, hs], dG[:, h, c, :], S[:, h, :],
                                                           start=False, stop=True), R=[r_dG, r_S], W=[r_pS])
                P.dve(lambda e: e.tensor_copy(out=S[:], in_=pS[0:64, 0:256].rearrange("p (h v) -> p h v", v=64)),
                      R=[r_pS, r_S], W=[r_S])
            P.dma("sp", C.yscan[d, g0:g0 + RT, :].rearrange("(c p) f -> p c f", p=64), Yt[yb][:], R=[r_Yt[yb]])
    P.end()


def ph_RP(C, l):
    P = C.P
    P.begin()
    ident_f = C.cst[:, C_ID:C_ID + 128]
    gne = C.cst[:, C_EPS + 1:C_EPS + 2]
    y0 = [P.sbuf(f"y0{i}", [128, 4, 256], F32) for i in range(2)]
    y1 = [P.sbuf(f"y1{i}", [128, 4, 256], F32) for i in range(2)]
    G = [P.sbuf(f"G{i}", [128, 2, 2, TT], F32) for i in range(2)]
    r_in = [P.res() for _ in range(2)]
    sqv = P.sbuf("sqv", [128, 16, 64], F32)
    mean = P.sbuf("mean", [128, 16], F32)
    var = P.sbuf("var", [128, 16], F32)
    r_w = P.res()
    yo = [P.sbuf(f"ryo{i}", [128, 2, TT], BF16) for i in range(2)]; r_yo = [P.res() for _ in range(2)]
    tmpf = P.sbuf("tmpf", [128, 2, TT], F32); r_tf = P.res()
    ps = [P.psum(f"pps{i}", [128, TT]) for i in range(2)]; r_ps = [P.pres() for _ in range(2)]
    it = 0
    for s, T in enumerate(C.seq_lens):
        for i in range(T // TT):
            g0 = C.offs[s] + i * TT
            b = it % 2
            it += 1
            ya, yb_, Gb = y0[b], y1[b], G[b]
            P.dma("sp", ya[:], C.yscan[0, g0:g0 + TT, :].rearrange("(b p) f -> p b f", p=128), R=[r_in[b]], W=[r_in[b]])
            P.dma("sp", yb_[:], C.yscan[1, g0:g0 + TT, :].rearrange("(b p) f -> p b f", p=128), R=[r_in[b]], W=[r_in[b]])
            for a in range(2):
                P.dma("sp", Gb[:, a], C.g23[a, :, g0:g0 + TT].rearrange("(c p) t -> p c t", p=128), R=[r_in[b]], W=[r_in[b]])
            yv = ya[:].rearrange("p b (h k) -> p (b h) k", k=64)
            P.dve(lambda e, ya=ya, yb_=yb_: e.tensor_tensor(out=ya[:], in0=ya[:], in1=yb_[:], op=ALU.add), R=[r_in[b]], W=[r_in[b]])
            P.dve(lambda e, yv=yv: e.tensor_reduce(out=mean[:], in_=yv, axis=AX.X, op=ALU.add), R=[r_in[b], r_w], W=[r_w])
            P.dve(lambda e: e.tensor_scalar(out=mean[:], in0=mean[:], scalar1=1.0 / 64, scalar2=None, op0=ALU.mult), R=[r_w], W=[r_w])
            P.dve(lambda e, yv=yv: e.tensor_tensor(out=yv, in0=yv, in1=bcast(mean[:].unsqueeze(2), [128, 16, 64]), op=ALU.subtract),
                  R=[r_in[b], r_w], W=[r_in[b]])
            P.pool(lambda e, yv=yv: e.tensor_tensor(out=sqv[:], in0=yv, in1=yv, op=ALU.mult), R=[r_in[b], r_w], W=[r_w])
            P.dve(lambda e: e.tensor_reduce(out=var[:], in_=sqv[:], axis=AX.X, op=ALU.add), R=[r_w], W=[r_w])
            P.act(lambda e: e.activation(out=var[:], in_=var[:], func=AF.Sqrt, bias=gne, scale=1.0 / 64), R=[r_w], W=[r_w])
            P.dve(lambda e: e.reciprocal(out=var[:], in_=var[:]), R=[r_w], W=[r_w])
            P.dve(lambda e, yv=yv: e.tensor_tensor(out=yv, in0=yv, in1=bcast(var[:].unsqueeze(2), [128, 16, 64]), op=ALU.mult),
                  R=[r_in[b], r_w], W=[r_in[b]])
            for j in range(2):
                for tb in range(4):
                    P.pe(lambda e, j=j, tb=tb, ya=ya: e.transpose(ps[j][:, tb * 128:(tb + 1) * 128], ya[:, tb, j * 128:(j + 1) * 128],
                                                                 ident_f), R=[r_in[b]], W=[r_ps[j]])
                P.dve(lambda e, j=j, Gb=Gb: e.tensor_tensor(out=tmpf[:, j, :], in0=ps[j][:], in1=Gb[:, 0, j, :], op=ALU.mult),
                      R=[r_ps[j], r_in[b], r_tf], W=[r_tf])
                P.pool(lambda e, j=j, Gb=Gb, b=b: e.tensor_tensor(out=yo[b][:, j, :], in0=tmpf[:, j, :], in1=Gb[:, 1, j, :], op=ALU.add),
                       R=[r_tf, r_in[b], r_yo[b]], W=[r_yo[b]])
            P.dma("sp", C.ymix[0:256, g0:g0 + TT].rearrange("(c p) t -> p c t", p=128), yo[b][:], R=[r_yo[b]])
    P.end()


def ph_C(C, l):
    ph_C1(C, l)
    ph_C2(C, l)


def ph_C1(C, l):
    P = C.P
    is_moe = (l % 2 == 1)
    X = C.xT if l == 0 else C.yT
    P.begin()
    ones_b = C.cstb[:, 384:512]
    eps_ap = C.cst[:, C_EPS:C_EPS + 1]
    wo = P.sbuf("wo", [128, 8, D], BF16); r_wo = P.res()
    P.dma("pool", wo[:], C.w_out[l].rearrange("(c p) n -> p c n", p=128), W=[r_wo])
    if is_moe:
        rw = P.sbuf("rw", [128, 8, NEXP], F32); r_rw = P.res()
        P.dma("sp", rw[:], C.router[l // 2].rearrange("(c p) e -> p c e", p=128), W=[r_rw])
    xt = [P.sbuf(f"cx{i}", [128, 8, TT], F32) for i in range(2)]; r_xt = [P.res() for _ in range(2)]
    ym = [P.sbuf(f"cym{i}", [128, 8, TT], BF16) for i in range(2)]; r_ym = [P.res() for _ in range(2)]
    sq = P.sbuf("csq", [128, 8, TT], BF16); r_sq = P.res()
    rt = P.sbuf("crt", [128, TT], F32); r_rt = P.res()
    hf = P.sbuf("chf", [128, 8, TT], F32); r_hf = P.res()
    hb = [P.sbuf(f"chb{i}", [128, 8, TT], BF16) for i in range(2)]; r_hb = [P.res() for _ in range(2)]
    ps_o = [P.psum(f"cps{i}", [128, TT]) for i in range(4)]; r_pso = [P.pres() for _ in range(4)]
    ps_ss = P.psum("cpss", [128, TT]); r_pss = P.pres()
    if is_moe:
        ps_r = P.psum("cpsr", [128, 4, NEXP]); r_psr = P.pres()
        lg = P.sbuf("lg", [128, 4, NEXP], F32); r_lg = P.res()
        lg2 = P.sbuf("lg2", [128, 4, NEXP], F32)
        eq1 = P.sbuf("eq1", [128, 4, NEXP], F32)
        eq2 = P.sbuf("eq2", [128, 4, NEXP], F32)
        m1 = P.sbuf("m1", [128, 4], F32)
        m2 = P.sbuf("m2", [128, 4], F32)
        g1 = P.sbuf("g1", [128, 4], F32)
        g2 = P.sbuf("g2", [128, 4], F32)
        cmb = [P.sbuf(f"cmb{i}", [128, 4, NEXP], F32) for i in range(2)]; r_cmb = [P.res() for _ in range(2)]
    tiles = []
    for s, T in enumerate(C.seq_lens):
        for i in range(T // TT):
            tiles.append((s, i * TT, C.offs[s] + i * TT))

    def load(ti):
        s, t0, g0 = tiles[ti]
        b = ti % 2
        P.dma("sp", xt[b][:], X[:, g0:g0 + TT].rearrange("(c p) t -> p c t", p=128), R=[r_xt[b]], W=[r_xt[b]])
        P.dma("sp", ym[b][:], C.ymix[:, g0:g0 + TT].rearrange("(c p) t -> p c t", p=128), R=[r_ym[b]], W=[r_ym[b]])

    pi_ = [0]
    load(0)
    for ti in range(len(tiles)):
        if ti + 1 < len(tiles):
            load(ti + 1)
        s, t0, g0 = tiles[ti]
        b = ti % 2
        for oc in range(8):
            pi = pi_[0] % 4
            pi_[0] += 1
            for kc in range(8):
                P.pe(lambda e, pi=pi, kc=kc, oc=oc, b=b: e.matmul(ps_o[pi][:], wo[:, kc, oc * 128:(oc + 1) * 128],
                                                             ym[b][:, kc, :], start=(kc == 0), stop=(kc == 7)),
                     R=[r_wo, r_ym[b]], W=[r_pso[pi]])
            P.dve(lambda e, pi=pi, oc=oc, b=b, s=s: e.scalar_tensor_tensor(
                out=xt[b][:, oc, :], in0=ps_o[pi][:], scalar=C.modT[:, l, 2 * 8 + oc, s:s + 1], in1=xt[b][:, oc, :],
                op0=ALU.mult, op1=ALU.add), R=[r_pso[pi], r_xt[b]], W=[r_xt[b]])
        P.dma("sp", C.yT[:, g0:g0 + TT].rearrange("(c p) t -> p c t", p=128), xt[b][:], R=[r_xt[b]])
        P.act(lambda e, b=b: e.activation(out=sq[:], in_=xt[b][:], func=AF.Square), R=[r_xt[b]], W=[r_sq])
        for c in range(8):
            P.pe(lambda e, c=c: e.matmul(ps_ss[:], ones_b, sq[:, c, :], start=(c == 0), stop=(c == 7)),
                 R=[r_sq], W=[r_pss])
        P.act(lambda e: e.activation(out=rt[:], in_=ps_ss[:], func=AF.Sqrt, bias=eps_ap, scale=1.0 / D),
              R=[r_pss], W=[r_rt])
        P.dve(lambda e: e.reciprocal(out=rt[:], in_=rt[:]), R=[r_rt], W=[r_rt])
        P.dve(lambda e, b=b: e.tensor_tensor(out=hf[:], in0=xt[b][:], in1=bcast(rt[:].unsqueeze(1), [128, 8, TT]),
                                             op=ALU.mult), R=[r_xt[b], r_rt], W=[r_hf])
        for c in range(8):
            if c % 2 == 0:
                P.act(lambda e, c=c, s=s: e.activation(out=hf[:, c, :], in_=hf[:, c, :], func=AF.Identity,
                                                       bias=C.modT[:, l, 3 * 8 + c, s:s + 1],
                                                       scale=C.modA[:, l, 1, c, s:s + 1]), R=[r_hf], W=[r_hf])
            else:
                P.dve(lambda e, c=c, s=s: e.tensor_scalar(out=hf[:, c, :], in0=hf[:, c, :],
                                                          scalar1=C.modA[:, l, 1, c, s:s + 1],
                                                          scalar2=C.modT[:, l, 3 * 8 + c, s:s + 1],
                                                          op0=ALU.mult, op1=ALU.add), R=[r_hf], W=[r_hf])
        P.pool(lambda e, b=b: e.tensor_copy(out=hb[b][:], in_=hf[:]), R=[r_hf], W=[r_hb[b]])
        P.dma("sp", C.h2s[:, g0:g0 + TT].rearrange("(c p) t -> p c t", p=128), hb[b][:], R=[r_hb[b]])
        if is_moe:
            for tb in range(4):
                for kc in range(8):
                    P.pe(lambda e, tb=tb, kc=kc: e.matmul(ps_r[:, tb, :], hf[:, kc, tb * 128:(tb + 1) * 128], rw[:, kc, :],
                                                          start=(kc == 0), stop=(kc == 7)), R=[r_hf, r_rw], W=[r_psr])
            cb = cmb[b]
            sh = [128, 4, NEXP]
            P.dve(lambda e: e.tensor_copy(out=lg[:], in_=ps_r[:]), R=[r_psr, r_lg], W=[r_lg])
            P.dve(lambda e: e.tensor_reduce(out=m1[:], in_=lg[:], axis=AX.X, op=ALU.max), R=[r_lg], W=[r_lg])
            P.dve(lambda e: e.tensor_tensor(out=eq1[:], in0=lg[:], in1=bcast(m1[:].unsqueeze(2), sh), op=ALU.is_equal),
                  R=[r_lg], W=[r_lg])
            P.dve(lambda e: e.scalar_tensor_tensor(out=lg2[:], in0=eq1[:], scalar=-1e30, in1=lg[:], op0=ALU.mult,
                                                   op1=ALU.add), R=[r_lg], W=[r_lg])
            P.dve(lambda e: e.tensor_reduce(out=m2[:], in_=lg2[:], axis=AX.X, op=ALU.max), R=[r_lg], W=[r_lg])
            P.dve(lambda e: e.tensor_tensor(out=eq2[:], in0=lg2[:], in1=bcast(m2[:].unsqueeze(2), sh), op=ALU.is_equal),
                  R=[r_lg], W=[r_lg])
            P.dve(lambda e: e.tensor_tensor(out=g1[:], in0=m1[:], in1=m2[:], op=ALU.subtract), R=[r_lg], W=[r_lg])
            P.act(lambda e: e.activation(out=g1[:], in_=g1[:], func=AF.Sigmoid), R=[r_lg], W=[r_lg])
            P.dve(lambda e: e.tensor_scalar(out=g2[:], in0=g1[:], scalar1=-1.0, scalar2=1.0, op0=ALU.mult, op1=ALU.add),
                  R=[r_lg], W=[r_lg])
            P.dve(lambda e: e.tensor_tensor(out=eq1[:], in0=eq1[:], in1=bcast(g1[:].unsqueeze(2), sh), op=ALU.mult),
                  R=[r_lg], W=[r_lg])
            P.dve(lambda e: e.tensor_tensor(out=eq2[:], in0=eq2[:], in1=bcast(g2[:].unsqueeze(2), sh), op=ALU.mult),
                  R=[r_lg], W=[r_lg])
            P.dve(lambda e, cb=cb: e.tensor_tensor(out=cb[:], in0=eq1[:], in1=eq2[:], op=ALU.add),
                  R=[r_lg, r_cmb[b]], W=[r_cmb[b]])
            P.dma("sp", C.comb[g0:g0 + TT, :].rearrange("(b p) e -> p b e", p=128), cb[:], R=[r_cmb[b]])
    P.end()


def ph_C2(C, l):
    P = C.P
    is_moe = (l % 2 == 1)
    last = (l == C.L - 1)
    li = l // 2
    G = min(2048, min(C.seq_lens))
    NSUB = G // TT
    P.begin()
    ones_b = C.cstb[:, 384:512]
    eps_ap = C.cst[:, C_EPS:C_EPS + 1]
    acc = P.sbuf("acc", [128, 8, G], F32); r_acc = P.res()
    h2 = P.sbuf("h2", [128, 8, G], BF16); r_h2 = P.res()
    wg = [P.sbuf(f"wg{i}", [128, 8, 512], BF16) for i in range(2)]
    wu = [P.sbuf(f"wu{i}", [128, 8, 512], BF16) for i in range(2)]
    wd = [P.sbuf(f"wd{i}", [128, 4, D], BF16) for i in range(2)]
    r_w = [P.res() for _ in range(2)]
    actb = [P.sbuf(f"actb{i}", [128, 4, TT], BF16) for i in range(2)]; r_act = [P.res() for _ in range(2)]
    sg = [P.sbuf(f"sg{i}", [128, TT], F32) for i in range(2)]; r_sg = [P.res() for _ in range(2)]
    tt_ = [P.sbuf(f"tt{i}", [128, TT], F32) for i in range(2)]; r_tt = [P.res() for _ in range(2)]
    ps_g = [P.psum(f"fpg{i}", [128, TT]) for i in range(2)]; r_pg = [P.pres() for _ in range(2)]
    ps_u = [P.psum(f"fpu{i}", [128, TT]) for i in range(2)]; r_pu = [P.pres() for _ in range(2)]
    ps_d = [P.psum(f"fpd{i}", [128, TT]) for i in range(2)]; r_pd = [P.pres() for _ in range(2)]
    ps_x = P.psum("fpx", [128, TT]); r_px = P.pres()
    if is_moe:
        cmt = P.sbuf("cmt", [128, G // 128, NEXP], F32); r_cmt = P.res()
        cwb = P.sbuf("cwb", [128, G // 128, 128], F32); r_cwb = P.res()
        cw = P.sbuf("cw", [128, NSUB, TT], F32); r_cw = P.res()
    if last:
        sq = P.sbuf("fsq", [128, 8, TT], BF16); r_sq = P.res()
        rt = P.sbuf("frt", [128, TT], F32); r_rt = P.res()
    ident_f = C.cst[:, C_ID:C_ID + 128]
    dff = DFF_E if is_moe else DFF_D
    nch = dff // 128
    fgs = [(c0, min(4, nch - c0)) for c0 in range(0, nch, 4)]
    wi_ = [0]
    ai_ = [0]
    for s, T in enumerate(C.seq_lens):
        for gi in range(T // G):
            g0 = C.offs[s] + gi * G
            P.dma("sp", acc[:], C.yT[:, g0:g0 + G].rearrange("(c p) t -> p c t", p=128), R=[r_acc], W=[r_acc])
            P.dma("sp", h2[:], C.h2s[:, g0:g0 + G].rearrange("(c p) t -> p c t", p=128), R=[r_h2], W=[r_h2])
            if is_moe:
                P.dma("sp", cmt[:], C.comb[g0:g0 + G, :].rearrange("(b p) e -> p b e", p=128), R=[r_cmt], W=[r_cmt])
            for ex in range(NEXP if is_moe else 1):
                if is_moe:
                    P.dve(lambda e, ex=ex: e.tensor_copy(out=cwb[:], in_=bcast(cmt[:, :, ex:ex + 1], [128, G // 128, 128])),
                          R=[r_cmt, r_cwb], W=[r_cwb])
                    for sub in range(NSUB):
                        for tb in range(4):
                            P.pe(lambda e, sub=sub, tb=tb: e.matmul(ps_x[:, tb * 128:(tb + 1) * 128], cwb[:, sub * 4 + tb, :],
                                                                   ident_f, start=True, stop=True), R=[r_cwb], W=[r_px])
                        P.act(lambda e, sub=sub: e.activation(out=cw[:, sub, :], in_=ps_x[:], func=AF.Copy),
                              R=[r_px, r_cw], W=[r_cw])
                    Wg, Wu, Wd = C.moe_g[li, ex], C.moe_u[li, ex], C.moe_d[li, ex]
                else:
                    Wg, Wu, Wd = C.ffn_g[li], C.ffn_u[li], C.ffn_d[li]
                for (c0, nc4) in fgs:
                    wb = wi_[0] % 2
                    wi_[0] += 1
                    f0, fw = c0 * 128, nc4 * 128
                    P.dma("pool", wg[wb][:, :, 0:fw], Wg[:, f0:f0 + fw].rearrange("(c p) f -> p c f", p=128),
                          R=[r_w[wb]], W=[r_w[wb]])
                    P.dma("pool", wu[wb][:, :, 0:fw], Wu[:, f0:f0 + fw].rearrange("(c p) f -> p c f", p=128),
                          R=[r_w[wb]], W=[r_w[wb]])
                    P.dma("pool", wd[wb][:, 0:nc4, :], Wd[f0:f0 + fw, :].rearrange("(c p) n -> p c n", p=128),
                          R=[r_w[wb]], W=[r_w[wb]])
                    for sub in range(NSUB):
                        ab = ai_[0] % 2
                        ai_[0] += 1
                        tsl = slice(sub * TT, (sub + 1) * TT)
                        for c4 in range(nc4):
                            pb = c4 % 2
                            for kc in range(8):
                                P.pe(lambda e, pb=pb, wb=wb, kc=kc, c4=c4, tsl=tsl: e.matmul(
                                    ps_g[pb][:], wg[wb][:, kc, c4 * 128:(c4 + 1) * 128], h2[:, kc, tsl],
                                    start=(kc == 0), stop=(kc == 7)), R=[r_w[wb], r_h2], W=[r_pg[pb]])
                            for kc in range(8):
                                P.pe(lambda e, pb=pb, wb=wb, kc=kc, c4=c4, tsl=tsl: e.matmul(
                                    ps_u[pb][:], wu[wb][:, kc, c4 * 128:(c4 + 1) * 128], h2[:, kc, tsl],
                                    start=(kc == 0), stop=(kc == 7)), R=[r_w[wb], r_h2], W=[r_pu[pb]])
                            P.act(lambda e, pb=pb: e.activation(out=sg[pb][:], in_=ps_g[pb][:], func=AF.Silu),
                                  R=[r_pg[pb]], W=[r_sg[pb]])
                            if is_moe:
                                P.dve(lambda e, pb=pb: e.tensor_tensor(out=tt_[pb][:], in0=sg[pb][:], in1=ps_u[pb][:],
                                                                       op=ALU.mult), R=[r_sg[pb], r_pu[pb]], W=[r_tt[pb]])
                                P.pool(lambda e, pb=pb, ab=ab, c4=c4, sub=sub: e.tensor_tensor(
                                    out=actb[ab][:, c4, :], in0=tt_[pb][:], in1=cw[:, sub, :], op=ALU.mult),
                                    R=[r_tt[pb], r_cw], W=[r_act[ab]])
                            else:
                                P.dve(lambda e, pb=pb, ab=ab, c4=c4: e.tensor_tensor(
                                    out=actb[ab][:, c4, :], in0=sg[pb][:], in1=ps_u[pb][:], op=ALU.mult),
                                    R=[r_sg[pb], r_pu[pb]], W=[r_act[ab]])
                        for oc in range(8):
                            db_ = oc % 2
                            for c4 in range(nc4):
                                P.pe(lambda e, db_=db_, wb=wb, c4=c4, oc=oc, ab=ab, nc4=nc4: e.matmul(
                                    ps_d[db_][:], wd[wb][:, c4, oc * 128:(oc + 1) * 128], actb[ab][:, c4, :],
                                    start=(c4 == 0), stop=(c4 == nc4 - 1)), R=[r_w[wb], r_act[ab]], W=[r_pd[db_]])
                            P.dve(lambda e, db_=db_, oc=oc, tsl=tsl, s=s: e.scalar_tensor_tensor(
                                out=acc[:, oc, tsl], in0=ps_d[db_][:], scalar=C.modT[:, l, 5 * 8 + oc, s:s + 1],
                                in1=acc[:, oc, tsl], op0=ALU.mult, op1=ALU.add), R=[r_pd[db_], r_acc], W=[r_acc])
            if last:
                for sub in range(NSUB):
                    tsl = slice(sub * TT, (sub + 1) * TT)
                    P.act(lambda e, tsl=tsl: e.activation(out=sq[:], in_=acc[:, :, tsl], func=AF.Square),
                          R=[r_acc, r_sq], W=[r_sq])
                    for c in range(8):
                        P.pe(lambda e, c=c: e.matmul(ps_x[:], ones_b, sq[:, c, :], start=(c == 0), stop=(c == 7)),
                             R=[r_sq], W=[r_px])
                    P.act(lambda e: e.activation(out=rt[:], in_=ps_x[:], func=AF.Sqrt, bias=eps_ap, scale=1.0 / D),
                          R=[r_px, r_rt], W=[r_rt])
                    P.dve(lambda e: e.reciprocal(out=rt[:], in_=rt[:]), R=[r_rt], W=[r_rt])
                    P.dve(lambda e, tsl=tsl: e.tensor_tensor(out=acc[:, :, tsl], in0=acc[:, :, tsl],
                                                             in1=bcast(rt[:].unsqueeze(1), [128, 8, TT]), op=ALU.mult),
                          R=[r_acc, r_rt], W=[r_acc])
                P.dve(lambda e: e.tensor_tensor(out=acc[:], in0=acc[:],
                                                in1=bcast(C.cst[:, C_GFIN:C_GFIN + 8].unsqueeze(2), [128, 8, G]),
                                                op=ALU.mult), R=[r_acc], W=[r_acc])
            P.dma("sp", C.yT[:, g0:g0 + G].rearrange("(c p) t -> p c t", p=128), acc[:], R=[r_acc], W=[r_acc])
    P.end()


SEQ_LENS = [4096, 2048, 2048, 2048, 2048]
DEPTH = 4
N_CORES = 8


def kernel(**inputs):
    inp = {k: np.asarray(v) for k, v in inputs.items()}
    xp, xs_, cp, cs_ = inp["x_prompt"], inp["x_sample"], inp["c_prompt"], inp["c_sample"]
    w = pack_weights(inp, DEPTH, SEQ_LENS)
    nc = build_program(SEQ_LENS, DEPTH)
    in_maps = []
    for core in range(N_CORES):
        xs = [xp[core]] + [xs_[4 * core + i] for i in range(4)]
        cc = np.stack([cp[core]] + [cs_[4 * core + i] for i in range(4)], axis=0).astype(np.float32)
        m = dict(w)
        m["xT"] = np.ascontiguousarray(np.concatenate([np.asarray(a, np.float32).T for a in xs], axis=1))
        m["cT"] = np.ascontiguousarray(cc.T.reshape(8, 128, len(xs)).transpose(1, 0, 2))
        in_maps.append(m)
    res = run_bass_kernel_spmd(nc, in_maps, core_ids=list(range(N_CORES)))
    y_prompt = np.empty(xp.shape, np.float32)
    y_sample = np.empty(xs_.shape, np.float32)
    for core in range(N_CORES):
        yT = np.asarray(res.results[core]["yT"])
        y_prompt[core] = yT[:, 0:4096].T
        for i in range(4):
            o = 4096 + 2048 * i
            y_sample[4 * core + i] = yT[:, o:o + 2048].T
    return (y_prompt, y_sample)
```
